# Optimizing a Trainium2 kernel written in Bass

```python
import math
import jax
import jax.numpy as jnp
from jax import lax
import numpy as np

D_MODEL = 1024
BATCH = 8
SEQ = 2048
DEPTH = 4

GRID_W = 64
CTX_LEN = 256
HEAD_DIM = 64
D_MIX = D_MODEL
GDN_WIDTH = D_MIX // 4
GDN_HEADS = GDN_WIDTH // HEAD_DIM
GDN_CHUNK = 64
SHORT_CONV = 4
LRU_WIDTH = D_MIX // 4
LRU_BLOCKS = 4
LRU_BLOCK_W = LRU_WIDTH // LRU_BLOCKS
LRU_C = 8.0
ATT_WIDTH = D_MIX - GDN_WIDTH - LRU_WIDTH
ATT_Q_HEADS = ATT_WIDTH // HEAD_DIM
ATT_KV_HEADS = 2
ATT_GROUP = ATT_Q_HEADS // ATT_KV_HEADS
ATT_KV_WIDTH = ATT_KV_HEADS * HEAD_DIM
Q_BLOCK = 128
ROPE_THETA = 10000.0
ROPE_AXIS_DIM = HEAD_DIM // 2
GDN_PROJ = 4 * GDN_WIDTH + 4 * GDN_HEADS
LRU_PROJ = 2 * LRU_WIDTH
ATT_PROJ = ATT_WIDTH + 2 * ATT_KV_WIDTH
D_IN_PROJ = GDN_PROJ + LRU_PROJ + ATT_PROJ
MOE_GROUPS = 8
MOE_EXPERTS_PER_GROUP = 8
MOE_EXPERTS = MOE_GROUPS * MOE_EXPERTS_PER_GROUP
MOE_TOP_K = 2
MOE_HIDDEN = 512
MOE_BLOCK = 128
EPS = 1e-6

kernel_name = 'hybrid_prefix_dit_gdn_rglru_gqa_hmoe'


def rms_norm(x, g):
    xf = x.astype(jnp.float32)
    y = xf * lax.rsqrt(jnp.mean(xf * xf, axis=-1, keepdims=True) + EPS)
    return (y * g.astype(jnp.float32)).astype(x.dtype)


def l2_norm(x):
    return x * lax.rsqrt(jnp.sum(x * x, axis=-1, keepdims=True) + EPS)


def modulate(x, g, shift, scale):
    return rms_norm(x, g) * (1 + scale) + shift


def depthwise_conv(x, w, b=None):
    k = w.shape[0]
    lo = (k - 1) // 2
    y = lax.conv_general_dilated(x, w[:, None, :].astype(x.dtype), (1,), [(lo, k - 1 - lo)],
                                 dimension_numbers=('NWC', 'WIO', 'NWC'),
                                 feature_group_count=x.shape[-1])
    if b is not None:
        y = y + b
    return y


def axial_rope_tables(rows):
    row = jnp.repeat(jnp.arange(rows, dtype=jnp.float32), GRID_W)
    col = jnp.tile(jnp.arange(GRID_W, dtype=jnp.float32), rows)
    inv_freq = ROPE_THETA ** (-jnp.arange(0, ROPE_AXIS_DIM, 2, dtype=jnp.float32) / ROPE_AXIS_DIM)
    ang = jnp.concatenate([row[:, None] * inv_freq, col[:, None] * inv_freq], axis=-1)
    return jnp.cos(ang), jnp.sin(ang)


def rotate_pairs(x, cos, sin):
    x1, x2 = jnp.split(x, 2, axis=-1)
    return jnp.concatenate([x1 * cos - x2 * sin, x1 * sin + x2 * cos], axis=-1)


def apply_axial_rope(x, cos, sin):
    n = ROPE_AXIS_DIM // 2
    cos = cos[:, None, :].astype(x.dtype)
    sin = sin[:, None, :].astype(x.dtype)
    xr = rotate_pairs(x[..., :ROPE_AXIS_DIM], cos[..., :n], sin[..., :n])
    xc = rotate_pairs(x[..., ROPE_AXIS_DIM:], cos[..., n:], sin[..., n:])
    return jnp.concatenate([xr, xc], axis=-1)


def gated_delta_chunked(q, k, v, beta, g, state):
    b, t, h, dk = q.shape
    dv = v.shape[-1]
    n = t // GDN_CHUNK

    def chunks(a):
        a = a.reshape((b, n, GDN_CHUNK, h) + a.shape[3:])
        return jnp.moveaxis(a, (1, 3), (0, 2))

    qc, kc, vc, bc, gc = (chunks(a) for a in (q, k, v, beta, g))
    gcum = jnp.cumsum(gc, axis=-1)
    lower = jnp.tril(jnp.ones((GDN_CHUNK, GDN_CHUNK), dtype=bool))
    strict = jnp.tril(jnp.ones((GDN_CHUNK, GDN_CHUNK), dtype=bool), k=-1)
    decay = jnp.exp(jnp.where(lower, gcum[..., :, None] - gcum[..., None, :], -jnp.inf))
    kb = kc * bc[..., None]
    a_mat = jnp.where(strict, jnp.einsum('nbhid,nbhjd->nbhij', kb, kc) * decay, 0.0)
    eye = jnp.eye(GDN_CHUNK, dtype=q.dtype)
    rhs = jnp.concatenate([vc * bc[..., None], kb * jnp.exp(gcum)[..., None]], axis=-1)
    sol = lax.linalg.triangular_solve(eye + a_mat, rhs, left_side=True, lower=True)
    u, w = sol[..., :dv], sol[..., dv:]
    qk = jnp.where(lower, jnp.einsum('nbhid,nbhjd->nbhij', qc, kc) * decay, 0.0)

    def step(s, inp):
        q_i, k_i, u_i, w_i, g_i, qk_i = inp
        v_new = u_i - jnp.einsum('bhck,bhkv->bhcv', w_i, s)
        o = (jnp.einsum('bhck,bhkv->bhcv', q_i * jnp.exp(g_i)[..., None], s)
             + jnp.einsum('bhij,bhjv->bhiv', qk_i, v_new))
        g_last = g_i[..., -1:]
        s = (s * jnp.exp(g_last)[..., None]
             + jnp.einsum('bhck,bhcv->bhkv', k_i * jnp.exp(g_last - g_i)[..., None], v_new))
        return s, o

    state, o = lax.scan(step, state, (qc, kc, u, w, gcum, qk))
    o = jnp.moveaxis(o, (0, 2), (1, 3)).reshape(b, t, h, dv)
    return o, state


def gdn_mixer(p_lat, p_ctx, conv_w, a_log, dt_bias, norm_g, need_ctx):
    f32 = jnp.float32

    def prep(p):
        bsz, t, _ = p.shape
        qkv = jax.nn.silu(depthwise_conv(p[..., :3 * GDN_WIDTH], conv_w)).astype(f32)
        qkv = qkv.reshape(bsz, t, 3, GDN_HEADS, HEAD_DIM)
        q = l2_norm(qkv[:, :, 0]) * HEAD_DIM ** -0.5
        k = l2_norm(qkv[:, :, 1])
        v = qkv[:, :, 2]
        gate = p[..., 3 * GDN_WIDTH:4 * GDN_WIDTH]
        ab = p[..., 4 * GDN_WIDTH:].astype(f32).reshape(bsz, t, 2, 2, GDN_HEADS)
        g = -jnp.exp(a_log) * jax.nn.softplus(ab[:, :, 0] + dt_bias)
        beta = jax.nn.sigmoid(ab[:, :, 1])
        return q, k, v, beta, g, gate

    flip = lambda a: jnp.flip(a, axis=1)

    def bidir(q, k, v, beta, g, s0f, s0b):
        of, sf = gated_delta_chunked(q, k, v, beta[:, :, 0], g[:, :, 0], s0f)
        ob, sb = gated_delta_chunked(flip(q), flip(k), flip(v), flip(beta[:, :, 1]), flip(g[:, :, 1]), s0b)
        return of + flip(ob), sf, sb

    def readout(o, gate, dtype):
        bsz, t = o.shape[:2]
        y = rms_norm(o, norm_g).reshape(bsz, t, GDN_WIDTH) * jax.nn.silu(gate.astype(f32))
        return y.astype(dtype)

    qc, kc, vc, bc, gcx, gate_c = prep(p_ctx)
    s0 = jnp.zeros((qc.shape[0], GDN_HEADS, HEAD_DIM, HEAD_DIM), f32)
    oc, sf, sb = bidir(qc, kc, vc, bc, gcx, s0, s0)
    ql, kl, vl, bl, gl, gate_l = prep(p_lat)
    ol, _, _ = bidir(ql, kl, vl, bl, gl, sf, sb)
    out_l = readout(ol, gate_l, p_lat.dtype)
    out_c = readout(oc, gate_c, p_ctx.dtype) if need_ctx else None
    return out_l, out_c


def lru_coefficients(xr, w_r, b_r, w_i, b_i, lam):
    bsz, t, _ = xr.shape
    xb = xr.reshape(bsz, t, LRU_BLOCKS, LRU_BLOCK_W)
    r = jax.nn.sigmoid(jnp.einsum('btnd,nde->btne', xb, w_r).reshape(bsz, t, LRU_WIDTH) + b_r)
    i = jax.nn.sigmoid(jnp.einsum('btnd,nde->btne', xb, w_i).reshape(bsz, t, LRU_WIDTH) + b_i)
    log_a = -LRU_C * r * jax.nn.softplus(-lam)
    return jnp.exp(log_a), jnp.sqrt(-jnp.expm1(2.0 * log_a)) * (i * xr)


def linear_scan(a, bx, h0):
    def combine(e1, e2):
        return e1[0] * e2[0], e2[0] * e1[1] + e2[1]
    a_cum, b_cum = lax.associative_scan(combine, (a, bx), axis=1)
    h = a_cum * h0[:, None, :] + b_cum
    return h, h[:, -1]


def lru_mixer(p_lat, p_ctx, conv_w, conv_b, w_r, b_r, w_i, b_i, lam, need_ctx):
    f32 = jnp.float32

    def branches(p):
        xr = depthwise_conv(p[..., :LRU_WIDTH], conv_w, conv_b).astype(f32)
        y = jax.nn.gelu(p[..., LRU_WIDTH:].astype(f32), approximate=True)
        return xr, y

    def direction(xr, d, h0):
        a, bx = lru_coefficients(xr, w_r[d], b_r[d], w_i[d], b_i[d], lam[d])
        return linear_scan(a, bx, h0)

    flip = lambda a: jnp.flip(a, axis=1)
    xr_c, y_c = branches(p_ctx)
    xr_l, y_l = branches(p_lat)
    h0 = jnp.zeros((xr_c.shape[0], LRU_WIDTH), f32)
    hc_f, s_f = direction(xr_c, 0, h0)
    hc_b, s_b = direction(flip(xr_c), 1, h0)
    hl_f, _ = direction(xr_l, 0, s_f)
    hl_b, _ = direction(flip(xr_l), 1, s_b)
    out_l = ((hl_f + flip(hl_b)) * y_l).astype(p_lat.dtype)
    out_c = ((hc_f + flip(hc_b)) * y_c).astype(p_ctx.dtype) if need_ctx else None
    return out_l, out_c


def blocked_attention(q, k, v):
    bsz, t = q.shape[:2]
    nb = t // Q_BLOCK
    qb = jnp.moveaxis(q.reshape(bsz, nb, Q_BLOCK, ATT_KV_HEADS, ATT_GROUP, HEAD_DIM), 1, 0)
    scale = HEAD_DIM ** -0.5

    def one_block(q_blk):
        s = jnp.einsum('bqkgd,bskd->bkgqs', q_blk, k, preferred_element_type=jnp.float32) * scale
        p = jax.nn.softmax(s, axis=-1)
        return jnp.einsum('bkgqs,bskd->bqkgd', p.astype(v.dtype), v)

    o = lax.map(one_block, qb)
    return jnp.moveaxis(o, 0, 1).reshape(bsz, t, ATT_WIDTH)


def attn_mixer(p_lat, p_ctx, q_norm_g, k_norm_g, cos, sin, need_ctx):
    def heads(p):
        bsz, t, _ = p.shape
        q = p[..., :ATT_WIDTH].reshape(bsz, t, ATT_Q_HEADS, HEAD_DIM)
        k = p[..., ATT_WIDTH:ATT_WIDTH + ATT_KV_WIDTH].reshape(bsz, t, ATT_KV_HEADS, HEAD_DIM)
        v = p[..., ATT_WIDTH + ATT_KV_WIDTH:].reshape(bsz, t, ATT_KV_HEADS, HEAD_DIM)
        return rms_norm(q, q_norm_g), rms_norm(k, k_norm_g), v

    qc, kc, vc = heads(p_ctx)
    ql, kl, vl = heads(p_lat)
    ql = apply_axial_rope(ql, cos, sin)
    kl = apply_axial_rope(kl, cos, sin)
    out_l = blocked_attention(ql, jnp.concatenate([kl, kc], axis=1), jnp.concatenate([vl, vc], axis=1))
    out_c = blocked_attention(qc, kc, vc) if need_ctx else None
    return out_l.astype(p_lat.dtype), (out_c.astype(p_ctx.dtype) if need_ctx else None)


def expert_mlp(xb, w1, w3, w2):
    return (jax.nn.silu(xb @ w1) * (xb @ w3)) @ w2


def hier_moe(tok, w_group, b_group, w_expert, b_expert, w1, w3, w2):
    n, d = tok.shape
    p_group = jax.nn.softmax((tok @ w_group).astype(jnp.float32) + b_group, axis=-1)
    pg_top, g_idx = lax.top_k(p_group, 1)
    le = ((tok @ w_expert).astype(jnp.float32) + b_expert).reshape(n, MOE_GROUPS, MOE_EXPERTS_PER_GROUP)
    le = le[jnp.arange(n), g_idx[:, 0]]
    pe_top, e_local = lax.top_k(jax.nn.softmax(le, axis=-1), MOE_TOP_K)
    gate = pg_top * pe_top / jnp.sum(pe_top, axis=-1, keepdims=True)
    expert = g_idx * MOE_EXPERTS_PER_GROUP + e_local
    nk = n * MOE_TOP_K
    flat_e = expert.reshape(-1)
    order = jnp.argsort(flat_e)
    sorted_e = flat_e[order]
    token_of = order // MOE_TOP_K
    counts = jnp.bincount(flat_e, length=MOE_EXPERTS)
    padded = (counts + MOE_BLOCK - 1) // MOE_BLOCK * MOE_BLOCK
    pad_end = jnp.cumsum(padded)
    start = jnp.cumsum(counts) - counts
    dest = (pad_end - padded)[sorted_e] + jnp.arange(nk) - start[sorted_e]
    n_blocks = -(-nk // MOE_BLOCK) + MOE_EXPERTS
    slot_tok = jnp.full((n_blocks * MOE_BLOCK,), n, dtype=token_of.dtype).at[dest].set(token_of)
    block_e = jnp.minimum(jnp.searchsorted(pad_end, jnp.arange(n_blocks) * MOE_BLOCK, side='right'),
                          MOE_EXPERTS - 1)
    tok_pad = jnp.concatenate([tok, jnp.zeros((1, d), tok.dtype)], axis=0)
    xb = tok_pad[slot_tok].reshape(n_blocks, MOE_BLOCK, d)
    yb = lax.map(lambda a: expert_mlp(a[0], w1[a[1]], w3[a[1]], w2[a[1]]), (xb, block_e))
    y = yb.reshape(-1, d)[dest] * gate.reshape(-1)[order][:, None].astype(tok.dtype)
    return jnp.zeros_like(tok).at[token_of].add(y)


def trunk_layer(x, xc, mod, mod_c, lp, cos, sin, need_ctx):
    sh1, sc1, g1, sh2, sc2, g2 = jnp.split(mod[:, None, :].astype(x.dtype), 6, axis=-1)
    csh1, csc1, cg1, csh2, csc2, cg2 = jnp.split(mod_c.astype(x.dtype), 6, axis=-1)
    proj = modulate(x, lp['norm1_g'], sh1, sc1) @ lp['w_in']
    proj_c = modulate(xc, lp['norm1_g'], csh1, csc1) @ lp['w_in']
    o1, o2 = GDN_PROJ, GDN_PROJ + LRU_PROJ
    a_l, a_c = gdn_mixer(proj[..., :o1], proj_c[..., :o1], lp['gdn_conv_w'], lp['gdn_a_log'],
                         lp['gdn_dt_bias'], lp['gdn_norm_g'], need_ctx)
    b_l, b_c = lru_mixer(proj[..., o1:o2], proj_c[..., o1:o2], lp['lru_conv_w'], lp['lru_conv_b'],
                         lp['lru_w_r'], lp['lru_b_r'], lp['lru_w_i'], lp['lru_b_i'], lp['lru_lambda'], need_ctx)
    c_l, c_c = attn_mixer(proj[..., o2:], proj_c[..., o2:], lp['attn_q_norm_g'], lp['attn_k_norm_g'],
                          cos, sin, need_ctx)
    x = x + g1 * (jnp.concatenate([a_l, b_l, c_l], axis=-1) @ lp['w_out'])
    moe = lambda t: hier_moe(t, lp['moe_w_group'], lp['moe_b_group'], lp['moe_w_expert'],
                             lp['moe_b_expert'], lp['moe_w1'], lp['moe_w3'], lp['moe_w2'])
    h = modulate(x, lp['norm2_g'], sh2, sc2)
    if need_ctx:
        xc = xc + cg1 * (jnp.concatenate([a_c, b_c, c_c], axis=-1) @ lp['w_out'])
        hc = modulate(xc, lp['norm2_g'], csh2, csc2)
        n_lat = h.shape[0] * h.shape[1]
        y = moe(jnp.concatenate([h.reshape(n_lat, -1), hc.reshape(-1, hc.shape[-1])], axis=0))
        x = x + g2 * y[:n_lat].reshape(x.shape)
        xc = xc + cg2 * y[n_lat:].reshape(xc.shape)
    else:
        x = x + g2 * moe(h.reshape(-1, h.shape[-1])).reshape(x.shape)
    return x, xc


def setup_inputs(seed: int = 0) -> dict:
    key = jax.random.key(seed)
    ks = iter(jax.random.split(key, 40))
    f32 = jnp.float32
    L, D = DEPTH, D_MODEL

    def nrm(shape, scale):
        return jax.random.normal(next(ks), shape, f32) * scale

    def gain(shape):
        return 1.0 + nrm(shape, 0.05)

    x = nrm((BATCH, SEQ, D), 1.0)
    c = nrm((BATCH, D), 1.0)
    ctx = nrm((BATCH, CTX_LEN, D), 1.0)
    c_ctx = nrm((D,), 1.0)
    w_ada = nrm((L, D, 6 * D), 0.3 * D ** -0.5)
    b_ada = nrm((L, 6 * D), 0.02)
    norm1_g = gain((L, D))
    norm2_g = gain((L, D))
    w_in = nrm((L, D, D_IN_PROJ), D ** -0.5)
    w_out = nrm((L, D_MIX, D), D_MIX ** -0.5)
    gdn_conv_w = nrm((L, SHORT_CONV, 3 * GDN_WIDTH), SHORT_CONV ** -0.5)
    gdn_a_log = jnp.log(jax.random.uniform(next(ks), (L, 2, GDN_HEADS), f32, minval=1.0, maxval=16.0))
    dt = jnp.exp(jax.random.uniform(next(ks), (L, 2, GDN_HEADS), f32,
                                    minval=math.log(1e-3), maxval=math.log(1e-1)))
    gdn_dt_bias = dt + jnp.log(-jnp.expm1(-dt))
    gdn_norm_g = gain((L, HEAD_DIM))
    lru_conv_w = nrm((L, SHORT_CONV, LRU_WIDTH), SHORT_CONV ** -0.5)
    lru_conv_b = nrm((L, LRU_WIDTH), 0.01)
    lru_w_r = nrm((L, 2, LRU_BLOCKS, LRU_BLOCK_W, LRU_BLOCK_W), LRU_BLOCK_W ** -0.5)
    lru_b_r = nrm((L, 2, LRU_WIDTH), 0.01)
    lru_w_i = nrm((L, 2, LRU_BLOCKS, LRU_BLOCK_W, LRU_BLOCK_W), LRU_BLOCK_W ** -0.5)
    lru_b_i = nrm((L, 2, LRU_WIDTH), 0.01)
    a0 = jax.random.uniform(next(ks), (L, 2, LRU_WIDTH), f32, minval=0.9, maxval=0.999)
    s = a0 ** (1.0 / LRU_C)
    lru_lambda = jnp.log(s) - jnp.log1p(-s)
    attn_q_norm_g = gain((L, HEAD_DIM))
    attn_k_norm_g = gain((L, HEAD_DIM))
    moe_w_group = nrm((L, D, MOE_GROUPS), D ** -0.5)
    moe_b_group = nrm((L, MOE_GROUPS), 0.01)
    moe_w_expert = nrm((L, D, MOE_EXPERTS), D ** -0.5)
    moe_b_expert = nrm((L, MOE_EXPERTS), 0.01)
    moe_w1 = nrm((L, MOE_EXPERTS, D, MOE_HIDDEN), D ** -0.5)
    moe_w3 = nrm((L, MOE_EXPERTS, D, MOE_HIDDEN), D ** -0.5)
    moe_w2 = nrm((L, MOE_EXPERTS, MOE_HIDDEN, D), MOE_HIDDEN ** -0.5)
    final_norm_g = gain((D,))
    return {'x': x, 'c': c, 'ctx': ctx, 'c_ctx': c_ctx, 'w_ada': w_ada, 'b_ada': b_ada,
            'norm1_g': norm1_g, 'norm2_g': norm2_g, 'w_in': w_in, 'w_out': w_out,
            'gdn_conv_w': gdn_conv_w, 'gdn_a_log': gdn_a_log, 'gdn_dt_bias': gdn_dt_bias,
            'gdn_norm_g': gdn_norm_g, 'lru_conv_w': lru_conv_w, 'lru_conv_b': lru_conv_b,
            'lru_w_r': lru_w_r, 'lru_b_r': lru_b_r, 'lru_w_i': lru_w_i, 'lru_b_i': lru_b_i,
            'lru_lambda': lru_lambda, 'attn_q_norm_g': attn_q_norm_g, 'attn_k_norm_g': attn_k_norm_g,
            'moe_w_group': moe_w_group, 'moe_b_group': moe_b_group, 'moe_w_expert': moe_w_expert,
            'moe_b_expert': moe_b_expert, 'moe_w1': moe_w1, 'moe_w3': moe_w3, 'moe_w2': moe_w2,
            'final_norm_g': final_norm_g}


def reference(x, c, ctx, c_ctx, w_ada, b_ada, norm1_g, norm2_g, w_in, w_out,
              gdn_conv_w, gdn_a_log, gdn_dt_bias, gdn_norm_g,
              lru_conv_w, lru_conv_b, lru_w_r, lru_b_r, lru_w_i, lru_b_i, lru_lambda,
              attn_q_norm_g, attn_k_norm_g,
              moe_w_group, moe_b_group, moe_w_expert, moe_b_expert, moe_w1, moe_w3, moe_w2,
              final_norm_g):
    rows = x.shape[1] // GRID_W
    cos, sin = axial_rope_tables(rows)
    silu_c = jax.nn.silu(c)
    silu_cc = jax.nn.silu(c_ctx)
    xc = ctx
    for l in range(DEPTH):
        mod = silu_c @ w_ada[l] + b_ada[l]
        mod_c = silu_cc @ w_ada[l] + b_ada[l]
        lp = {'norm1_g': norm1_g[l], 'norm2_g': norm2_g[l], 'w_in': w_in[l], 'w_out': w_out[l],
              'gdn_conv_w': gdn_conv_w[l], 'gdn_a_log': gdn_a_log[l], 'gdn_dt_bias': gdn_dt_bias[l],
              'gdn_norm_g': gdn_norm_g[l], 'lru_conv_w': lru_conv_w[l], 'lru_conv_b': lru_conv_b[l],
              'lru_w_r': lru_w_r[l], 'lru_b_r': lru_b_r[l], 'lru_w_i': lru_w_i[l], 'lru_b_i': lru_b_i[l],
              'lru_lambda': lru_lambda[l], 'attn_q_norm_g': attn_q_norm_g[l],
              'attn_k_norm_g': attn_k_norm_g[l], 'moe_w_group': moe_w_group[l],
              'moe_b_group': moe_b_group[l], 'moe_w_expert': moe_w_expert[l],
              'moe_b_expert': moe_b_expert[l], 'moe_w1': moe_w1[l], 'moe_w3': moe_w3[l],
              'moe_w2': moe_w2[l]}
        x, xc = trunk_layer(x, xc, mod, mod_c, lp, cos, sin, need_ctx=l < DEPTH - 1)
    return rms_norm(x, final_norm_g)
```

```python
import contextlib
import numpy as np
import concourse.bass as bass
import concourse.mybir as mybir
from concourse.bass_utils import run_bass_kernel_spmd

F32 = mybir.dt.float32
F32R = mybir.dt.float32r
BF16 = mybir.dt.bfloat16
I32 = mybir.dt.int32
AF = mybir.ActivationFunctionType
ALU = mybir.AluOpType
AX = mybir.AxisListType

D = 1024
T_CTX = 256
T_LAT = 2048
T = T_CTX + T_LAT
NT = T // 128
NCH = T // 64
DEPTH = 4
DIN = 2320
EPS = 1e-6
NEG = -30000.0


class Res:
    __slots__ = ("name", "writer", "readers", "excl")

    def __init__(self, name):
        self.name = name
        self.writer = None
        self.readers = []
        self.excl = False


class Sched:
    ENG = ("pe", "dve", "act", "pool", "sp")

    def __init__(self, nc, n_dma_sems=8):
        self.nc = nc
        self.obj = {"pe": nc.tensor, "dve": nc.vector, "act": nc.scalar,
                    "pool": nc.gpsimd, "sp": nc.sync}
        self.sem = {}
        self.count = {}
        self.waited = {e: {} for e in self.ENG}
        self._ctx = []
        for e in self.ENG:
            cm = nc.semaphore("s_" + e)
            self.sem[e] = cm.__enter__()
            self._ctx.append(cm)
            self.count[e] = 0
        self.dma_sems = {}
        self.dma_rr = {}
        self.gen = 0
        self.n_dma_sems = n_dma_sems
        for q in ("sp", "pool"):
            lst = []
            for i in range(n_dma_sems):
                cm = nc.semaphore(f"d_{q}{i}")
                lst.append([cm.__enter__(), 0])
                self._ctx.append(cm)
            self.dma_sems[q] = lst
            self.dma_rr[q] = 0
        self.n_wait = 0
        self.n_inst = 0

    def renew(self):
        self.barrier()
        self.gen += 1
        nc = self.nc
        for e in self.ENG:
            cm = nc.semaphore(f"s_{e}_g{self.gen}")
            self.sem[e] = cm.__enter__()
            self._ctx.append(cm)
            self.count[e] = 0
        for q in ("sp", "pool"):
            lst = []
            for i in range(self.n_dma_sems):
                cm = nc.semaphore(f"d_{q}{i}_g{self.gen}")
                lst.append([cm.__enter__(), 0])
                self._ctx.append(cm)
            self.dma_sems[q] = lst
            self.dma_rr[q] = 0
        self.waited = {e: {} for e in self.ENG}

    def _need(self, eng, ev, same_ok_dist=None):
        if ev is None:
            return None
        if len(ev) > 4 and ev[4] != self.gen:
            return None
        key, sem, val, src = ev[:4]
        if src == eng and src is not None:
            if same_ok_dist is None:
                return None
            if self.count[eng] - val >= same_ok_dist:
                return None
        if self.waited[eng].get(key, 0) >= val:
            return None
        return ev

    def _emit_waits(self, eng, evs):
        best = {}
        for ev in evs:
            if ev is None:
                continue
            key = ev[0]
            if key not in best or best[key][2] < ev[2]:
                best[key] = ev
        for key, evb in best.items():
            sem, val = evb[1], evb[2]
            self.obj[eng].wait_ge(sem, val)
            self.waited[eng][key] = val
            self.n_wait += 1

    def deps(self, eng, reads, writes):
        evs = []
        for r in reads:
            evs.append(self._need(eng, r.writer, same_ok_dist=4))
        for w in writes:
            evs.append(self._need(eng, w.writer, same_ok_dist=None))
            for rd in w.readers:
                evs.append(self._need(eng, rd, same_ok_dist=None))
        return evs

    def _commit(self, ev, reads, writes):
        for r in reads:
            r.readers.append(ev)
            if len(r.readers) > 48:
                best = {}
                for e in r.readers:
                    if e[4] != self.gen:
                        continue
                    if e[0] not in best or best[e[0]][2] < e[2]:
                        best[e[0]] = e
                r.readers = list(best.values())
        for w in writes:
            w.writer = ev
            w.readers = []

    def op(self, eng, fn, reads=(), writes=()):
        ex = [r for r in reads if r.excl]
        if ex:
            writes = list(writes) + ex
        self._emit_waits(eng, self.deps(eng, reads, writes))
        ins = fn(self.obj[eng])
        self.count[eng] += 1
        ins.then_inc(self.sem[eng], 1)
        ev = (eng, self.sem[eng], self.count[eng], eng, self.gen)
        self._commit(ev, reads, writes)
        self.n_inst += 1
        return ev

    def dma(self, q, fn, reads=(), writes=()):
        evs = self.deps(q, reads, writes)
        idx = self.dma_rr[q]
        slot = self.dma_sems[q][idx]
        self.dma_rr[q] = (idx + 1) % len(self.dma_sems[q])
        key = f"d_{q}{idx}"
        if slot[1] > 0:
            evs.append(self._need(q, (key, slot[0], slot[1], None, self.gen)))
        self._emit_waits(q, evs)
        ins = fn(self.obj[q])
        slot[1] += 16
        ins.then_inc(slot[0], 16)
        ev = (key, slot[0], slot[1], None, self.gen)
        self._commit(ev, reads, writes)
        self.n_inst += 1
        return ev

    def wait_event(self, eng, ev):
        self._emit_waits(eng, [self._need(eng, ev)])

    def barrier(self):
        evs = []
        for e in self.ENG:
            if self.count[e] > 0:
                evs.append((e, self.sem[e], self.count[e], e, self.gen))
        for q, lst in self.dma_sems.items():
            for i, (sem, val) in enumerate(lst):
                if val > 0:
                    evs.append((f"d_{q}{i}", sem, val, None, self.gen))
        for e in self.ENG:
            need = []
            for ev in evs:
                if ev[3] == e:
                    continue
                if self.waited[e].get(ev[0], 0) >= ev[2]:
                    continue
                need.append(ev)
            self._emit_waits(e, need)


class Tl:
    def __init__(self, ap, name, nsub=0):
        self.ap = ap
        self.name = name
        self.rs = [Res(f"{name}.{i}") for i in range(max(1, nsub))]

    def __getitem__(self, key):
        return self.ap[key]

    @property
    def r(self):
        return self.rs

    def rk(self, *ks):
        n = len(self.rs)
        return [self.rs[min(k, n - 1)] for k in ks]


def _flat(lst):
    out = []
    for x in lst:
        if isinstance(x, Tl):
            out.extend(x.rs)
        elif isinstance(x, (list, tuple)):
            out.extend(_flat(x))
        elif x is not None:
            out.append(x)
    return out


class Builder:
    def __init__(self, n_layers=DEPTH, debug=None, skip_inputs=(), flags=()):
        self.flags = set(flags)
        self.skip_inputs = set(skip_inputs)
        self.n_layers = n_layers
        self.debug = debug or {}
        self.nc = bass.Bass("TRN2", target_bir_lowering=False)
        self.S = Sched(self.nc)
        self.stack = [contextlib.ExitStack()]
        self._uid = 0
        self.outputs = []

    def push(self):
        self.stack.append(contextlib.ExitStack())

    def pop(self):
        self.S.barrier()
        self.stack.pop().close()

    def sb(self, name, shape, dt=F32, nsub=0):
        self._uid += 1
        t = self.stack[-1].enter_context(self.nc.sbuf_tensor(f"{name}_{self._uid}", list(shape), dt))
        return Tl(t, name, nsub)

    def ps(self, name, shape, dt=F32, nsub=0):
        self._uid += 1
        t = self.stack[-1].enter_context(self.nc.psum_tensor(f"{name}_{self._uid}", list(shape), dt))
        tl = Tl(t, name, nsub)
        for r in tl.rs:
            r.excl = True
        return tl

    def dram(self, name, shape, dt=F32, kind="Internal", nsub=0):
        t = self.nc.dram_tensor(name, list(shape), dt, kind=kind).ap()
        return Tl(t, name, nsub)

    op_limit = None
    op_cnt = 0

    def op(self, eng, fn, reads=(), writes=()):
        if self.op_limit is not None:
            self.op_cnt += 1
            if self.op_cnt > self.op_limit:
                return None
            if self.op_cnt == self.op_limit:
                import inspect
                fr = inspect.stack()
                print("LAST OP:", eng, [f"{f.function}:{f.lineno}" for f in fr[1:5]])
        return self.S.op(eng, fn, _flat(reads), _flat(writes))

    def dma(self, q, out, in_, reads=(), writes=(), **kw):
        return self.S.dma(q, lambda e: e.dma_start(out=out, in_=in_, **kw), _flat(reads), _flat(writes))

    def mm(self, out, lhsT, rhs, start, stop, reads, writes):
        return self.op("pe", lambda e: e.matmul(out, lhsT=lhsT, rhs=rhs, start=start, stop=stop), reads, writes)

    def tr(self, out, in_, ident, reads, writes):
        return self.op("pe", lambda e: e.transpose(out, in_, ident), reads, writes)

    def act(self, out, in_, func, reads, writes, eng="act", **kw):
        return self.op("act", lambda e: e.activation(out=out, in_=in_, func=func, **kw), reads, writes)

    def tt(self, eng, out, in0, in1, op, reads, writes):
        return self.op(eng, lambda e: e.tensor_tensor(out=out, in0=in0, in1=in1, op=op), reads, writes)

    def ts(self, eng, out, in0, s1, op0, reads, writes, s2=None, op1=None):
        if op1 is None:
            return self.op(eng, lambda e: e.tensor_scalar(out=out, in0=in0, scalar1=s1, scalar2=None, op0=op0), reads, writes)
        return self.op(eng, lambda e: e.tensor_scalar(out=out, in0=in0, scalar1=s1, scalar2=s2, op0=op0, op1=op1), reads, writes)

    def stt(self, out, in0, scalar, in1, op0, op1, reads, writes):
        return self.op("dve", lambda e: e.scalar_tensor_tensor(out=out, in0=in0, scalar=scalar, in1=in1, op0=op0, op1=op1), reads, writes)

    def copy(self, eng, out, in_, reads, writes):
        if eng == "act":
            return self.op("act", lambda e: e.copy(out=out, in_=in_), reads, writes)
        return self.op(eng, lambda e: e.tensor_copy(out=out, in_=in_), reads, writes)

    def memset(self, eng, ap, val, writes):
        return self.op(eng, lambda e: e.memset(ap, val), (), writes)


C_GQ, C_GK, C_GV, C_GG, C_GAB = 0, 256, 512, 768, 1024
C_LX, C_LY = 1040, 1296
C_AQ, C_AK, C_AV = 1552, 2064, 2192
PF_GDN, PF_LRU = 0, 768
PT_GATE, PT_AB, PT_ATT = 0, 256, 272
PT_W = 1040

EXTRA_LAYOUT = ['gdn_conv_wT', 'lru_conv_wT']
WEIGHT_NAMES = ['w_ada', 'b_ada', 'norm1_g', 'norm2_g', 'w_in', 'w_out', 'gdn_conv_w', 'gdn_a_log',
                'gdn_dt_bias', 'gdn_norm_g', 'lru_conv_w', 'lru_conv_b', 'lru_w_r', 'lru_b_r', 'lru_w_i',
                'lru_b_i', 'lru_lambda', 'attn_q_norm_g', 'attn_k_norm_g', 'moe_w_group', 'moe_b_group',
                'moe_w_expert', 'moe_b_expert', 'moe_w1', 'moe_w3', 'moe_w2', 'final_norm_g']
WEIGHT_SHAPES = {
    'w_ada': [4, 1024, 6144], 'b_ada': [4, 6144], 'norm1_g': [4, 1024], 'norm2_g': [4, 1024],
    'w_in': [4, 1024, 2320], 'w_out': [4, 1024, 1024], 'gdn_conv_w': [4, 4, 768], 'gdn_a_log': [4, 2, 4],
    'gdn_dt_bias': [4, 2, 4], 'gdn_norm_g': [4, 64], 'lru_conv_w': [4, 4, 256], 'lru_conv_b': [4, 256],
    'lru_w_r': [4, 2, 4, 64, 64], 'lru_b_r': [4, 2, 256], 'lru_w_i': [4, 2, 4, 64, 64], 'lru_b_i': [4, 2, 256],
    'lru_lambda': [4, 2, 256], 'attn_q_norm_g': [4, 64], 'attn_k_norm_g': [4, 64], 'moe_w_group': [4, 1024, 8],
    'moe_b_group': [4, 8], 'moe_w_expert': [4, 1024, 64], 'moe_b_expert': [4, 64],
    'moe_w1': [4, 64, 1024, 512], 'moe_w3': [4, 64, 1024, 512], 'moe_w2': [4, 64, 512, 1024],
    'final_norm_g': [1, 1024], 'gdn_conv_wT': [4, 768, 4], 'lru_conv_wT': [4, 256, 4]}


def host_consts():
    c = {}
    c['ident'] = np.eye(128, dtype=np.float32)
    r = np.arange(64)[:, None]
    q = np.arange(64)[None, :]
    def t4(m):
        return np.tile(m.astype(np.float32), (1, 4))
    g = np.zeros((64, 9, 256), np.float32)
    g[:, 0] = t4(np.where(r >= q, 0.0, NEG))
    g[:, 1] = t4(np.where(r <= q, 0.0, NEG))
    g[:, 2] = t4(np.where(r > q, -1.0, 0.0))
    g[:, 3] = t4(np.where(r < q, -1.0, 0.0))
    g[:, 4] = t4(np.where(r <= q, 1.0, 0.0))
    g[:, 5] = t4(np.where(r >= q, 1.0, 0.0))
    g[:, 6] = 1.0
    g[:, 7] = -1.0
    g[:, 8] = t4(np.eye(64))
    c['gconst'] = g.reshape(64, 9 * 256)
    pos = np.arange(T_LAT)
    row = (pos // 64).astype(np.float64); col = (pos % 64).astype(np.float64)
    inv = 10000.0 ** (-np.arange(0, 32, 2, dtype=np.float64) / 32)
    ang = np.concatenate([row[:, None] * inv, col[:, None] * inv], axis=-1)
    c['rope_cs'] = np.concatenate([np.cos(ang), np.sin(ang)], axis=-1).astype(np.float32)
    mc = np.zeros((128, 256), np.float32)
    mc[:, 0:64] = np.arange(64)[None, :]
    mc[:, 64:100] = 128.0 * np.arange(36)[None, :]
    mc[:, 100:136] = np.arange(36)[None, :]
    mc[:, 136] = np.arange(128)
    mc[:, 137:201] = 1.0
    c['moe_const'] = mc
    tri = np.zeros((128, 256), np.float32)
    tri[:, 0:128] = (np.arange(128)[:, None] < np.arange(128)[None, :])
    tri[:, 128:256] = 1.0
    c['moe_tri'] = tri
    return c


class Kern(Builder):
    def declare_io(self):
        nc = self.nc
        self.inp = {}
        self.inp['xs_in'] = nc.dram_tensor('xs_in', [T, D], F32, kind='ExternalInput').ap()
        self.inp['cT_in'] = nc.dram_tensor('cT_in', [128, 16], F32, kind='ExternalInput').ap()
        for n in WEIGHT_NAMES + EXTRA_LAYOUT:
            if n in self.skip_inputs:
                continue
            shp = list(WEIGHT_SHAPES[n])
            if shp[0] == 4:
                shp[0] = self.n_layers
            self.inp[n] = nc.dram_tensor(n, shp, F32, kind='ExternalInput').ap()
        for n, v in host_consts().items():
            self.inp[n] = nc.dram_tensor(n, list(v.shape), F32, kind='ExternalInput').ap()
        self.out = nc.dram_tensor('out', [T_LAT, D], F32, kind='ExternalOutput').ap()
        self.XS = self.dram('XS', [T, D], nsub=NT)
        self.MODS = self.dram('MODS', [2, 6 * D])
        self.PF = self.dram('PF', [1280, T])
        self.PT = self.dram('PT', [T, PT_W], nsub=NT)
        self.MO = self.dram('MO', [D, T])
        self.XB = self.dram('XB', [N_SLOT, D])
        self.YB = self.dram('YB', [N_SLOT, D])
        self.dbg = {}
        for name, shape in self.debug.items():
            self.dbg[name] = nc.dram_tensor('dbg_' + name, list(shape), F32, kind='ExternalOutput').ap()

    def bcast_row(self, ap_row, n):
        return ap_row.partition_broadcast(128)

    def phase_init(self):
        for tt in range(NT):
            self.dma("sp", self.XS[tt * 128:(tt + 1) * 128, :], self.inp['xs_in'][tt * 128:(tt + 1) * 128, :],
                     writes=self.XS.rk(tt))
        self.ident = self.sb('ident', [128, 128])
        self.dma("sp", self.ident[:], self.inp['ident'][:, :], writes=[self.ident])
        cT = self.sb('cT', [128, 16])
        self.dma("sp", cT[:], self.inp['cT_in'][:, :], writes=[cT])
        self.scT = self.sb('scT', [128, 8, 2], F32R)
        self.act(self.scT[:, :, 0], cT[:, 0:8], AF.Silu, [cT], [self.scT])
        self.act(self.scT[:, :, 1], cT[:, 8:16], AF.Silu, [cT], [self.scT])

    def phase_ada(self, l):
        self.push()
        wa = [self.sb(f'wa{i}', [128, 8, 512], F32R) for i in range(2)]
        ba = self.sb('ba', [2, 6 * D])
        mods = self.sb('mods', [2, 6 * D])
        pp = [self.ps(f'pada{i}', [2, 512]) for i in range(2)]
        for r in range(2):
            self.dma("sp", ba[r:r + 1, :], self.inp['b_ada'][l:l + 1, :], writes=[ba])
        wsrc = self.inp['w_ada'][l].rearrange("(kc p) n -> p kc n", p=128)
        for cg in range(12):
            w = wa[cg % 2]
            self.dma("pool", w[:], wsrc[:, :, cg * 512:(cg + 1) * 512], writes=[w])
            p = pp[cg % 2]
            for kc in range(8):
                self.mm(p[:], self.scT[:, kc, :], w[:, kc, :], kc == 0, kc == 7, [self.scT, w], [p])
            self.tt("dve", mods[:, cg * 512:(cg + 1) * 512], p[:], ba[:, cg * 512:(cg + 1) * 512], ALU.add,
                    [p, ba], [mods])
        self.dma("sp", self.MODS[:, :], mods[:], reads=[mods], writes=[self.MODS])
        self.pop()

    def load_mod(self, dst, which, r, g_name=None, l=0):
        self.dma("sp", dst[:], self.MODS[r:r + 1, which * D:(which + 1) * D].partition_broadcast(128),
                 reads=[self.MODS], writes=[dst])
        if g_name is not None:
            gb = self.sb('gb', [128, D])
            self.dma("sp", gb[:], self.inp[g_name][l:l + 1, :].partition_broadcast(128), writes=[gb])
            self.stt(dst[:], dst[:], 1.0, gb[:], ALU.add, ALU.mult, [dst, gb], [dst])

    def evac(self, i, out, in_, reads, writes):
        return self.copy("act" if i % 2 == 0 else "dve", out, in_, reads, writes)

    def norm_tile(self, xt, gm, sh, xn, junk, ss):
        self.act(junk[:], xt[:], AF.Square, [xt], [junk, ss], accum_out=ss[:, 0:1])
        self.act(ss[:, 1:2], ss[:, 0:1], AF.Sqrt, [ss], [ss], scale=1.0 / D, bias=self.eps_t[:, 0:1])
        self.op("dve", lambda e: e.reciprocal(out=ss[:, 2:3], in_=ss[:, 1:2]), [ss], [ss])
        self.stt(xn[:], xt[:], ss[:, 2:3], gm[:], ALU.mult, ALU.mult, [xt, ss, gm], [xn])
        if sh is not None:
            self.tt("pool", xn[:], xn[:], sh[:], ALU.add, [xn, sh], [xn])

    def phase_proj(self, l):
        self.push()
        win = self.sb('win', [128, 8, DIN], F32R)
        wsrc = self.inp['w_in'][l].rearrange("(kc p) n -> p kc n", p=128)
        for a, b in ((0, 1160), (1160, 2320)):
            self.dma("pool", win[:, :, a:b], wsrc[:, :, a:b], writes=[win])
        gm = [self.sb(f'gm{r}', [128, D]) for r in range(2)]
        sh = [self.sb(f'sh{r}', [128, D]) for r in range(2)]
        self.push()
        for r in range(2):
            self.load_mod(gm[r], 1, r, 'norm1_g', l)
            self.load_mod(sh[r], 0, r)
        self.pop()
        xt = [self.sb(f'xt{i}', [128, D]) for i in range(2)]
        xn = [self.sb(f'xn{i}', [128, D]) for i in range(2)]
        junk = self.sb('junk', [128, D])
        ss = [self.sb(f'ss{i}', [128, 4]) for i in range(2)]
        xnT = self.sb('xnT', [128, 8, 512], F32R, nsub=4)
        sfm = [self.sb(f'sfm{i}', [128, 512]) for i in range(2)]
        stm = [self.sb(f'stm{i}', [128, PT_W]) for i in range(2)]
        ptp = [self.ps(f'ptp{i}', [128, 512]) for i in range(2)]
        pfm = [self.ps(f'pfm{i}', [128, 512]) for i in range(2)]
        ptm = [self.ps('ptm0', [128, 272]), self.ps('ptm1', [128, 512]), self.ps('ptm2', [128, 256])]
        fm_groups = [(c0, PF_GDN + c0) for c0 in range(0, 768, 128)] + \
                    [(C_LX + c0, PF_LRU + c0) for c0 in range(0, 512, 128)]
        tm_groups = [(C_GG, 272, PT_GATE), (C_AQ, 512, PT_ATT), (C_AQ + 512, 256, PT_ATT + 512)]
        blocks = [(0, 2), (2, 4), (6, 4), (10, 4), (14, 4)]
        it = 0
        ig = 0
        for (t0, ntile) in blocks:
            r = 1 if t0 == 0 else 0
            for j in range(ntile):
                tt = t0 + j
                x_ = xt[it % 2]; xn_ = xn[it % 2]; ss_ = ss[it % 2]
                self.dma("sp", x_[:], self.XS[tt * 128:(tt + 1) * 128, :], reads=self.XS.rk(tt), writes=[x_])
                self.norm_tile(x_, gm[r], sh[r], xn_, junk, ss_)
                for half in range(2):
                    p = ptp[half]
                    for q in range(4):
                        kc = half * 4 + q
                        self.tr(p[:, q * 128:(q + 1) * 128], xn_[:, kc * 128:(kc + 1) * 128], self.ident[:],
                                [xn_, self.ident], [p])
                    self.evac(half, xnT[:, half * 4:(half + 1) * 4, j * 128:(j + 1) * 128],
                              p[:].rearrange("p (q t) -> p q t", q=4), [p], xnT.rk(j))
                st = stm[it % 2]
                for gi, (c0, n, d0) in enumerate(tm_groups):
                    p = ptm[gi]
                    for kc in range(8):
                        self.mm(p[:, 0:n], xnT[:, kc, j * 128:(j + 1) * 128], win[:, kc, c0:c0 + n], kc == 0, kc == 7,
                                [xnT.rk(j), win], [p])
                    self.evac(gi, st[:, d0:d0 + n], p[:, 0:n], [p], [st])
                self.dma("sp", self.PT[tt * 128:(tt + 1) * 128, :], st[:], reads=[st], writes=self.PT.rk(tt))
                it += 1
            ntok = ntile * 128
            for (c0, d0) in fm_groups:
                p = pfm[ig % 2]; s_ = sfm[ig % 2]
                for kc in range(8):
                    self.mm(p[:, 0:ntok], win[:, kc, c0:c0 + 128], xnT[:, kc, 0:ntok], kc == 0, kc == 7,
                            [win, xnT.rk(*range(ntile))], [p])
                self.evac(ig, s_[:, 0:ntok], p[:, 0:ntok], [p], [s_])
                self.dma("sp", self.PF[d0:d0 + 128, t0 * 128:t0 * 128 + ntok], s_[:, 0:ntok], reads=[s_], writes=[self.PF])
                ig += 1
        self.pop()

    def build(self, stop_after=None):
        self.stop = stop_after
        self.declare_io()
        self.eps_t = self.sb('eps', [128, 1])
        self.memset("pool", self.eps_t[:], EPS, [self.eps_t])
        self.phase_init()
        for l in range(self.n_layers):
            if l > 0:
                self.S.renew()
            self.phase_ada(l)
            self.phase_proj(l)
            if stop_after == 'proj':
                break
            if 'nogdn' not in self.flags:
                self.phase_gdn(l)
            if stop_after is not None and stop_after.startswith('gdn'):
                break
            self.phase_lru(l)
            if stop_after == 'lru':
                break
            self.phase_att(l)
            if stop_after == 'att':
                break
            self.phase_wout(l)
            if stop_after == 'wout':
                break
            self.phase_moe(l)
            if stop_after == 'moe':
                break
        if stop_after is None:
            self.phase_final()
        evs = []
        for name, ap in self.dbg.items():
            src = {'PF': self.PF, 'PT': self.PT, 'MODS': self.MODS, 'MO': self.MO, 'XS': self.XS}[name]
            self.S.barrier()
            evs.append(self.dma("sp", ap[:, :], src[:, :], reads=[src]))
        for ev in evs:
            self.S.wait_event("sp", ev)
        self.S.barrier()
        return self.nc


def make_in_maps(inputs, n_layers=DEPTH, skip=()):
    consts = host_consts()
    maps = []
    for b in range(8):
        m = {}
        m['xs_in'] = np.ascontiguousarray(np.concatenate([inputs['ctx'][b], inputs['x'][b]], axis=0), dtype=np.float32)
        cT = np.concatenate([np.asarray(inputs['c'][b]).reshape(8, 128).T,
                             np.asarray(inputs['c_ctx']).reshape(8, 128).T], axis=1)
        m['cT_in'] = np.ascontiguousarray(cT, dtype=np.float32)
        for n in WEIGHT_NAMES:
            if n in skip:
                continue
            a = np.asarray(inputs[n], dtype=np.float32).reshape(WEIGHT_SHAPES[n])
            if WEIGHT_SHAPES[n][0] == 4:
                a = a[:n_layers]
            m[n] = np.ascontiguousarray(a)
        m['gdn_conv_wT'] = np.ascontiguousarray(np.transpose(np.asarray(inputs['gdn_conv_w'], dtype=np.float32), (0, 2, 1))[:n_layers])
        m['lru_conv_wT'] = np.ascontiguousarray(np.transpose(np.asarray(inputs['lru_conv_w'], dtype=np.float32), (0, 2, 1))[:n_layers])
        m.update(consts)
        maps.append(m)
    return maps


def kernel(**inputs):
    kb = Kern()
    nc = kb.build()
    res = run_bass_kernel_spmd(nc, make_in_maps(inputs), core_ids=list(range(8)))
    return np.stack([np.asarray(r['out']) for r in res.results], axis=0)


G_NML, G_NMU, G_SML, G_SMU, G_L, G_U, G_ONE, G_NEG1, G_ID = range(9)


def _gdn_methods():
    def gc(self, k, n=256):
        return self.gconst[:, k * 256:k * 256 + n]

    def phase_gdn(self, l):
        self.push()
        S = self.S
        gconst = self.sb('gconst', [64, 9 * 256])
        self.gconst = gconst
        self.dma("sp", gconst[:], self.inp['gconst'][:, :], writes=[gconst])
        one_t = self.sb('one', [128, 1])
        self.memset("pool", one_t[:], 1.0, [one_t])
        qn = self.sb('qn', [64, 4, T], nsub=NCH)
        kn = self.sb('kn', [64, 4, T], nsub=NCH)
        vT = self.sb('vT', [128, 2, T], nsub=NCH)
        moA = self.sb('moA', [128, 2, T])
        OF = self.dram(f'OF{l}', [T, 256], nsub=NCH)
        cwq = self.sb('cwq', [64, 8, 4])
        cwv = self.sb('cwv', [128, 2, 4])
        cw_src = self.inp['gdn_conv_wT'][l]
        self.dma("sp", cwq[:], cw_src[0:512, :].rearrange("(h p) m -> p h m", p=64), writes=[cwq])
        self.dma("sp", cwv[:], cw_src[512:768, :].rearrange("(h p) m -> p h m", p=128), writes=[cwv])
        chunks_all = list(range(NCH))

        self.push()
        raw = [self.sb(f'raw{i}', [128, 516]) for i in range(3)]
        lnb = self.sb('lnb', [64, 4, T])
        sqb = [self.sb(f'sqb{i}', [64, 512]) for i in range(2)]
        pss = [self.ps(f'pss{i}', [64, 512]) for i in range(2)]
        segs = [(0, T_CTX, [(0, 256)]), (T_CTX, T, [(T_CTX + i * 512, 512) for i in range(4)])]
        it = 0
        for kind in range(10):
            P = 64 if kind < 8 else 128
            row0 = kind * 64 if kind < 8 else 512 + (kind - 8) * 128
            cw = cwq[:, kind, :] if kind < 8 else cwv[:, kind - 8, :]
            cwt = cwq if kind < 8 else cwv
            for (s0, s1, blks) in segs:
                for (t0, blk) in blks:
                    rw = raw[it % 3]
                    lo = max(t0 - 1, s0); hi = min(t0 + blk + 2, s1)
                    if lo > t0 - 1:
                        self.memset("pool", rw[0:P, 0:1], 0.0, [rw])
                    if hi < t0 + blk + 2:
                        self.memset("pool", rw[0:P, hi - (t0 - 1):blk + 3], 0.0, [rw])
                    self.dma("sp", rw[0:P, lo - (t0 - 1):hi - (t0 - 1)], self.PF[row0:row0 + P, lo:hi],
                             reads=[self.PF], writes=[rw])
                    if kind >= 8:
                        dt_, dst = vT, vT[:, kind - 8, t0:t0 + blk]
                    elif kind < 4:
                        dt_, dst = qn, qn[:, kind, t0:t0 + blk]
                    else:
                        dt_, dst = kn, kn[:, kind - 4, t0:t0 + blk]
                    self.ts("dve", dst, rw[0:P, 0:blk], cw[:, 0:1], ALU.mult, [rw, cwt], [dt_])
                    for m in range(1, 4):
                        self.stt(dst, rw[0:P, m:m + blk], cw[:, m:m + 1], dst, ALU.mult, ALU.add, [rw, cwt, dt_], [dt_])
                    it += 1
        f2 = lambda t: t[:].rearrange("p h t -> p (h t)")
        for t_ in (qn, kn, vT):
            self.act(f2(t_), f2(t_), AF.Silu, [t_], [t_])
        blks_all = [(0, 256)] + [(T_CTX + i * 512, 512) for i in range(4)]
        it = 0
        for t_, sc in ((qn, 0.125), (kn, 1.0)):
            for h in range(4):
                for (t0, blk) in blks_all:
                    sq = sqb[it % 2]; ps_ = pss[it % 2]
                    self.tt("pool", sq[:, 0:blk], t_[:, h, t0:t0 + blk], t_[:, h, t0:t0 + blk], ALU.mult, [t_], [sq])
                    self.mm(ps_[:, 0:blk], self.gc(G_ONE, 64), sq[:, 0:blk], True, True, [gconst, sq], [ps_])
                    self.act(lnb[:, h, t0:t0 + blk], ps_[:, 0:blk], AF.Ln, [ps_], [lnb], bias=self.eps_t[0:64, 0:1])
                    it += 1
            self.act(f2(lnb), f2(lnb), AF.Exp, [lnb], [lnb], scale=-0.5)
            self.stt(f2(t_), f2(t_), sc, f2(lnb), ALU.mult, ALU.mult, [t_, lnb], [t_])
        self.pop()

        if self.stop == 'gdn_pre':
            self.dma("sp", self.MO[0:256, :].rearrange("(m p) t -> p m t", p=128), vT[:], reads=[vT], writes=[self.MO])
            self.dma("sp", self.MO[256:512, :].rearrange("(h p) t -> p h t", p=64), kn[:], reads=[kn], writes=[self.MO])
            self.dma("sp", self.MO[512:768, :].rearrange("(h p) t -> p h t", p=64), qn[:], reads=[qn], writes=[self.MO])
            self.pop()
            return
        abT = self.sb('abT', [64, NCH, 16])
        self.dma("sp", abT[:], self.PT[:, PT_AB:PT_AB + 16].rearrange("(c p) n -> p c n", p=64),
                 reads=[self.PT], writes=[abT])
        par = self.sb('par', [64, 16])
        self.dma("sp", par[:, 0:8], self.inp['gdn_a_log'][l:l + 1].rearrange("o d h -> o (d h)").partition_broadcast(64), writes=[par])
        self.dma("sp", par[:, 8:16], self.inp['gdn_dt_bias'][l:l + 1].rearrange("o d h -> o (d h)").partition_broadcast(64), writes=[par])
        negA = self.sb('negA', [64, 8])
        self.act(negA[:], par[:, 0:8], AF.Exp, [par], [negA])
        self.ts("dve", negA[:], negA[:], -1.0, ALU.mult, [negA], [negA])
        gall = self.sb('gall', [64, NCH, 8])
        beta = self.sb('beta', [64, NCH, 8])
        ball = self.sb('ball', [64, NCH, 8])
        eb = self.sb('eb', [64, NCH, 8])
        ebeta = self.sb('ebeta', [64, NCH, 8])
        ekd = self.sb('ekd', [64, NCH, 8])
        etot = self.sb('etot', [64, NCH, 8])
        tot = self.sb('tot', [64, NCH, 8])
        self.push()
        pb = [self.ps(f'pb{i}', [64, NCH * 8]) for i in range(3)]
        self.tt("dve", gall[:], abT[:, :, 0:8], par[:, 8:16].unsqueeze(1).to_broadcast([64, NCH, 8]), ALU.add, [abT, par], [gall])
        self.act(gall[:], gall[:], AF.Exp, [gall], [gall])
        self.act(gall[:], gall[:], AF.Ln, [gall], [gall], bias=one_t[0:64, 0:1])
        self.tt("dve", gall[:], gall[:], negA[:].unsqueeze(1).to_broadcast([64, NCH, 8]), ALU.mult, [gall, negA], [gall])
        self.act(beta[:], abT[:, :, 8:16], AF.Sigmoid, [abT], [beta])
        g2 = gall[:].rearrange("p c n -> p (c n)")
        self.mm(pb[0][:], self.gc(G_L, 64), g2, True, True, [gconst, gall], [pb[0]])
        self.mm(pb[1][:], self.gc(G_U, 64), g2, True, True, [gconst, gall], [pb[1]])
        self.mm(pb[2][:], self.gc(G_ONE, 64), g2, True, True, [gconst, gall], [pb[2]])
        v3 = lambda p: p[:].rearrange("p (c n) -> p c n", n=8)
        self.copy("dve", ball[:, :, 0:4], v3(pb[0])[:, :, 0:4], [pb[0]], [ball])
        self.copy("dve", ball[:, :, 4:8], v3(pb[1])[:, :, 4:8], [pb[1]], [ball])
        self.copy("dve", tot[:], v3(pb[2]), [pb[2]], [tot])
        self.act(eb[:], ball[:], AF.Exp, [ball], [eb])
        self.tt("dve", ebeta[:], eb[:], beta[:], ALU.mult, [eb, beta], [ebeta])
        self.tt("dve", ekd[:], tot[:], ball[:], ALU.subtract, [tot, ball], [ekd])
        self.act(ekd[:], ekd[:], AF.Exp, [ekd], [ekd])
        self.act(etot[:], tot[:], AF.Exp, [tot], [etot])
        self.pop()
        gng = self.sb('gng', [64, 4, 64])
        self.dma("sp", gng[:, 0, :], self.inp['gdn_norm_g'][l:l + 1, :].partition_broadcast(64), writes=[gng])
        for h in range(1, 4):
            self.copy("dve", gng[:, h, :], gng[:, 0, :], [gng], [gng])

        if self.stop == 'gdn_scal':
            for i_, t_ in enumerate((gall, beta, ball, eb, ebeta, ekd, etot, tot)):
                self.dma("sp", self.MO[i_ * 64:(i_ + 1) * 64, 0:NCH * 8], t_[:].rearrange("p c n -> p (c n)"), reads=[t_], writes=[self.MO])
            self.pop()
            return
        pA = self.ps('pA', [128, 512]); pB = self.ps('pB', [128, 512])
        pC = self.ps('pC', [128, 512]); pD = self.ps('pD', [128, 512])
        pE = self.ps('pE', [128, 512]); pF = self.ps('pF', [128, 512])
        pG = self.ps('pG', [128, 512]); pH = self.ps('pH', [128, 512])
        H0 = slice(0, 256); H1 = slice(256, 512)
        W = [64, 256]
        GLt = self.sb('GLt', W); Em = self.sb('Em', W); EmT = self.sb('EmT', W)
        tA = self.sb('tA', W)
        X = [self.sb(f'X{i}', W) for i in range(2)]
        XT = [self.sb(f'XT{i}', W) for i in range(2)]
        Pm = [self.sb(f'Pm{i}', W) for i in range(2)]
        vb = self.sb('vb', W); kbe = self.sb('kbe', W)
        u_ = [self.sb(f'u{i}', W) for i in range(2)]
        wT_ = [self.sb(f'wT{i}', W) for i in range(2)]
        KQm_ = [self.sb(f'KQm{i}', W) for i in range(2)]
        kd_ = [self.sb(f'kd{i}', W) for i in range(2)]
        Sst = [self.sb(f'S{i}', W) for i in range(2)]
        St = self.sb('St', W)
        vnew = self.sb('vnew', W)
        o2sb = self.sb('o2sb', W); osb = [self.sb(f'osb{i}', W) for i in range(2)]
        ofl = [self.sb(f'ofl{i}', W) for i in range(2)]
        gat = [self.sb(f'gat{i}', W) for i in range(2)]
        rs4 = self.sb('rs4', [64, 8]); ysb = self.sb('ysb', W); sqo = self.sb('sqo', W)

        def bc4(t, c, d):
            return t[:, c, d * 4:(d + 1) * 4].unsqueeze(2).to_broadcast([64, 4, 64])

        def v4(ap):
            return ap.rearrange("p (h n) -> p h n", h=4)

        def local(c, d, i):
            cs = slice(c * 64, (c + 1) * 64)
            NMa, NMb = (G_NML, G_NMU) if d == 0 else (G_NMU, G_NML)
            SM = G_SML if d == 0 else G_SMU
            GLm = G_L if d == 0 else G_U
            self.tt("dve", v4(GLt[:]), v4(self.gc(G_ID)), bc4(ball, c, d), ALU.mult, [gconst, ball], [GLt])
            for h in range(4):
                hs = slice(h * 64, (h + 1) * 64)
                self.mm(pA[0:64, hs], self.gc(G_ONE, 64), GLt[:, hs], True, True, [GLt, gconst], pA.rk(0))
            for h in range(4):
                hs = slice(h * 64, (h + 1) * 64)
                hs1 = slice(256 + h * 64, 256 + (h + 1) * 64)
                self.mm(pB[0:64, hs], kn[:, h, cs], kn[:, h, cs], True, True, kn.rk(c), pB.rk(0))
                self.mm(pB[0:64, hs1], kn[:, h, cs], qn[:, h, cs], True, True, [kn.rk(c), qn.rk(c)], pB.rk(1))
                self.mm(pE[0:64, hs], kn[:, h, cs], self.ident[0:64, 0:64], True, True, [kn.rk(c), self.ident], pE.rk(0))
            for m in range(2):
                self.mm(pE[0:64, 256 + m * 128:256 + (m + 1) * 128], vT[:, m, cs], self.ident[:, :], True, True, [vT.rk(c), self.ident], pE.rk(1))
            self.tt("dve", v4(Em[:]), bc4(ball, c, d), v4(pA[0:64, H0]), ALU.subtract, [pA.rk(0), ball], [Em])
            self.tt("pool", Em[:], Em[:], self.gc(NMa), ALU.add, [Em, gconst], [Em])
            self.act(Em[:], Em[:], AF.Exp, [Em], [Em])
            self.tt("dve", v4(EmT[:]), v4(pA[0:64, H0]), bc4(ball, c, d), ALU.subtract, [pA.rk(0), ball], [EmT])
            self.tt("pool", EmT[:], EmT[:], self.gc(NMb), ALU.add, [EmT, gconst], [EmT])
            self.act(EmT[:], EmT[:], AF.Exp, [EmT], [EmT])
            self.tt("dve", tA[:], pB[0:64, H0], self.gc(SM), ALU.mult, [pB.rk(0), gconst], [tA])
            self.tt("dve", v4(tA[:]), v4(tA[:]), bc4(beta, c, d), ALU.mult, [tA, beta], [tA])
            self.tt("pool", XT[0][:], tA[:], Em[:], ALU.mult, [tA, Em], [XT[0]])
            self.tt("dve", KQm_[i][:], pB[0:64, H1], EmT[:], ALU.mult, [pB.rk(1), EmT], [KQm_[i]])
            self.tt("dve", v4(vb[:]), v4(pE[0:64, H1]), bc4(beta, c, d), ALU.mult, [pE.rk(1), beta], [vb])
            self.tt("dve", v4(kbe[:]), v4(pE[0:64, H0]), bc4(ebeta, c, d), ALU.mult, [pE.rk(0), ebeta], [kbe])
            self.tt("dve", v4(kd_[i][:]), v4(pE[0:64, H0]), bc4(ekd, c, d), ALU.mult, [pE.rk(0), ekd], [kd_[i]])
            for h in range(4):
                hs = slice(h * 64, (h + 1) * 64)
                self.mm(pD[0:64, 256 + h * 64:256 + (h + 1) * 64], XT[0][:, hs], self.ident[0:64, 0:64], True, True, [XT[0], self.ident], pD.rk(1))
            self.copy("act", X[0][:], pD[0:64, H1], pD.rk(1), [X[0]])
            self.tt("pool", Pm[0][:], X[0][:], self.gc(G_ID), ALU.add, [X[0], gconst], [Pm[0]])
            cur = 0
            pc = 0

            def p_update(xt_tile, pc):
                for h in range(4):
                    hs = slice(h * 64, (h + 1) * 64)
                    self.mm(pD[0:64, hs], xt_tile[:, hs], Pm[pc][:, hs], True, True, [xt_tile, Pm[pc]], pD.rk(0))
                self.tt("dve", Pm[1 - pc][:], pD[0:64, H0], Pm[pc][:], ALU.add, [pD.rk(0), Pm[pc]], [Pm[1 - pc]])
                return 1 - pc

            for k in range(5):
                nxt = 1 - cur
                last = (k == 4)
                for h in range(4):
                    hs = slice(h * 64, (h + 1) * 64)
                    hs1 = slice(256 + h * 64, 256 + (h + 1) * 64)
                    if not last:
                        self.mm(pC[0:64, hs], XT[cur][:, hs], X[cur][:, hs], True, True, [XT[cur], X[cur]], pC.rk(0))
                    self.mm(pC[0:64, hs1], X[cur][:, hs], XT[cur][:, hs], True, True, [XT[cur], X[cur]], pC.rk(1))
                if k >= 1:
                    pc = p_update(XT[cur], pc)
                if not last:
                    self.copy("act", X[nxt][:], pC[0:64, H0], pC.rk(0), [X[nxt]])
                self.copy("dve", XT[nxt][:], pC[0:64, H1], pC.rk(1), [XT[nxt]])
                cur = nxt
            pc = p_update(XT[cur], pc)
            assert pc == 1
            TT = Pm[1]
            for h in range(4):
                hs = slice(h * 64, (h + 1) * 64)
                hs1 = slice(256 + h * 64, 256 + (h + 1) * 64)
                self.mm(pF[0:64, hs], TT[:, hs], vb[:, hs], True, True, [TT, vb], pF.rk(0))
                self.mm(pF[0:64, hs1], kbe[:, hs], TT[:, hs], True, True, [TT, kbe], pF.rk(1))
            self.copy("act", u_[i][:], pF[0:64, H0], pF.rk(0), [u_[i]])
            self.copy("dve", wT_[i][:], pF[0:64, H1], pF.rk(1), [wT_[i]])

        def seq(c, d, i, si, step):
            cs = slice(c * 64, (c + 1) * 64)
            Sc = Sst[si]; Sn = Sst[1 - si]
            for h in range(4):
                hs = slice(h * 64, (h + 1) * 64)
                self.mm(pG[0:64, hs], wT_[i][:, hs], Sc[:, hs], True, True, [wT_[i], Sc], pG.rk(0))
            self.tt("dve", vnew[:], u_[i][:], pG[0:64, H0], ALU.subtract, [u_[i], pG.rk(0)], [vnew])
            for h in range(4):
                hs = slice(h * 64, (h + 1) * 64)
                hs1 = slice(256 + h * 64, 256 + (h + 1) * 64)
                self.mm(pG[0:64, hs1], kd_[i][:, hs], vnew[:, hs], True, True, [kd_[i], vnew], pG.rk(1))
            self.tt("dve", v4(St[:]), v4(Sc[:]), bc4(etot, c, d), ALU.mult, [Sc, etot], [St])
            self.tt("dve", Sn[:], St[:], pG[0:64, H1], ALU.add, [St, pG.rk(1)], [Sn])
            for h in range(4):
                hs = slice(h * 64, (h + 1) * 64)
                hs1 = slice(256 + h * 64, 256 + (h + 1) * 64)
                self.mm(pH[0:64, hs], qn[:, h, cs], Sc[:, hs], True, True, [qn.rk(c), Sc], pH.rk(0))
                self.mm(pH[0:64, hs1], KQm_[i][:, hs], vnew[:, hs], True, True, [KQm_[i], vnew], pH.rk(1))
            ob = osb[step % 2]
            self.copy("act", o2sb[:], pH[0:64, H1], pH.rk(1), [o2sb])
            self.tt("dve", v4(ob[:]), v4(pH[0:64, H0]), bc4(eb, c, d), ALU.mult, [pH.rk(0), eb], [ob])
            self.tt("pool", ob[:], ob[:], o2sb[:], ALU.add, [ob, o2sb], [ob])
            if d == 0:
                self.dma("sp", OF[cs, :], ob[:], reads=[ob], writes=OF.rk(c))
            else:
                of = ofl[step % 2]; ga = gat[step % 2]
                self.dma("sp", of[:], OF[cs, :], reads=OF.rk(c), writes=[of])
                self.dma("sp", ga[:], self.PT[cs, PT_GATE:PT_GATE + 256], reads=[self.PT], writes=[ga])
                self.tt("pool", ob[:], ob[:], of[:], ALU.add, [ob, of], [ob])
                self.tt("pool", sqo[:], ob[:], ob[:], ALU.mult, [ob], [sqo])
                self.op("dve", lambda e: e.tensor_reduce(out=rs4[:, 0:4], in_=v4(sqo[:]), axis=AX.X, op=ALU.add), [sqo], [rs4])
                self.act(rs4[:, 4:8], rs4[:, 0:4], AF.Sqrt, [rs4], [rs4], scale=1.0 / 64, bias=self.eps_t[0:64, 0:1])
                self.op("dve", lambda e: e.reciprocal(out=rs4[:, 0:4], in_=rs4[:, 4:8]), [rs4], [rs4])
                self.tt("dve", v4(ysb[:]), v4(ob[:]), rs4[:, 0:4].unsqueeze(2).to_broadcast([64, 4, 64]), ALU.mult, [ob, rs4], [ysb])
                self.tt("pool", ysb[:], ysb[:], gng[:].rearrange("p h n -> p (h n)"), ALU.mult, [ysb, gng], [ysb])
                self.act(ga[:], ga[:], AF.Silu, [ga], [ga])
                self.tt("dve", ysb[:], ysb[:], ga[:], ALU.mult, [ysb, ga], [ysb])
                for m in range(2):
                    self.mm(pE[:, m * 64:(m + 1) * 64], ysb[:, m * 128:(m + 1) * 128], self.ident[0:64, 0:64], True, True, [ysb, self.ident], pE.rk(0))
                self.copy("act", moA[:, :, cs], pE[:, 0:128].rearrange("p (m t) -> p m t", m=2), pE.rk(0), [moA])

        for d in range(2):
            order = list(range(NCH)) if d == 0 else [3, 2, 1, 0] + list(range(NCH - 1, 3, -1))
            self.memset("pool", Sst[0][:], 0.0, [Sst[0]])
            if self.stop == 'gdn_local1':
                import os
                self.op_limit = int(os.environ.get('OPLIM', '100000'))
                self.op_cnt = 0
            local(order[0], d, 0)
            self.op_limit = None
            if self.stop == 'gdn_local1':
                for i_, t_ in enumerate((Pm[1], u_[0], wT_[0], KQm_[0], kd_[0], Em, EmT, XT[0], vb, kbe)):
                    self.dma("sp", self.MO[i_ * 64:(i_ + 1) * 64, 0:256], t_[:], reads=[t_], writes=[self.MO])
                self.pop()
                return
            for step, c in enumerate(order):
                if self.stop == 'gdn_seq1' and step == 1:
                    for i_, t_ in enumerate((Sst[1], vnew, osb[0])):
                        self.dma("sp", self.MO[i_ * 64:(i_ + 1) * 64, 0:256], t_[:], reads=[t_], writes=[self.MO])
                    self.pop()
                    return
                if step + 1 < len(order):
                    local(order[step + 1], d, (step + 1) % 2)
                seq(c, d, step % 2, step % 2, step)
        self.dma("sp", self.MO[0:256, :].rearrange("(m p) t -> p m t", p=128), moA[:], reads=[moA], writes=[self.MO])
        self.pop()

    return dict(gc=gc, phase_gdn=phase_gdn)


for _k, _v in _gdn_methods().items():
    setattr(Kern, _k, _v)


def _lru_att_methods():
    def rev_ap(self, ap2d):
        n = ap2d.shape[1]
        last = ap2d[:, n - 1:n]
        return bass.AP(last.tensor, last.offset, [list(last.ap[0]), [-1, n]])

    def phase_lru(self, l):
        self.push()
        one_t = self.sb('one', [128, 1])
        self.memset("pool", one_t[:], 1.0, [one_t])
        xr = self.sb('xr', [128, 2, T])
        yg = self.sb('yg', [128, 2, T])
        A = self.sb('A', [128, 2, T])
        BX = self.sb('BX', [128, 2, T])
        H = [self.sb(f'H{i}', [128, 2, T]) for i in range(2)]
        cw = self.sb('cwl', [128, 2, 4])
        cb = self.sb('cbl', [128, 2])
        self.dma("sp", cw[:], self.inp['lru_conv_wT'][l].rearrange("(m p) k -> p m k", p=128), writes=[cw])
        for m in range(2):
            self.dma("sp", cb[:, m:m + 1], self.inp['lru_conv_b'][l, m * 128:(m + 1) * 128].rearrange("(p o) -> p o", o=1), writes=[cb])
        WB = self.sb('WB', [128, 8, 128])
        self.memset("pool", WB[:], 0.0, [WB])
        bri = self.sb('bri', [128, 8])
        lam = self.sb('lam', [128, 4])
        for d in range(2):
            for gi, (wn, bn) in enumerate((('lru_w_r', 'lru_b_r'), ('lru_w_i', 'lru_b_i'))):
                for m in range(2):
                    idx = (d * 2 + gi) * 2 + m
                    for hb in range(2):
                        self.dma("sp", WB[hb * 64:(hb + 1) * 64, idx, hb * 64:(hb + 1) * 64],
                                 self.inp[wn][l, d, 2 * m + hb], writes=[WB])
                    self.dma("sp", bri[:, idx:idx + 1],
                             self.inp[bn][l, d, m * 128:(m + 1) * 128].rearrange("(p o) -> p o", o=1), writes=[bri])
            for m in range(2):
                self.dma("sp", lam[:, d * 2 + m:d * 2 + m + 1],
                         self.inp['lru_lambda'][l, d, m * 128:(m + 1) * 128].rearrange("(p o) -> p o", o=1), writes=[lam])
        cch = self.sb('cch', [128, 4])
        self.act(cch[:], lam[:], AF.Exp, [lam], [cch], scale=-1.0)
        self.act(cch[:], cch[:], AF.Ln, [cch], [cch], bias=one_t[:, 0:1])
        self.ts("dve", cch[:], cch[:], -8.0, ALU.mult, [cch], [cch])
        self.push()
        raw = [self.sb(f'raw{i}', [128, 516]) for i in range(2)]
        t1 = self.sb('t1', [128, 512]); t2 = self.sb('t2', [128, 512])
        segs = [(0, T_CTX, [(0, 256)]), (T_CTX, T, [(T_CTX + i * 512, 512) for i in range(4)])]
        it = 0
        for m in range(2):
            row0 = PF_LRU + m * 128
            for (s0, s1, blks) in segs:
                for (t0, blk) in blks:
                    rw = raw[it % 2]
                    lo = max(t0 - 1, s0); hi = min(t0 + blk + 2, s1)
                    if lo > t0 - 1:
                        self.memset("pool", rw[:, 0:1], 0.0, [rw])
                    if hi < t0 + blk + 2:
                        self.memset("pool", rw[:, hi - (t0 - 1):blk + 3], 0.0, [rw])
                    self.dma("sp", rw[:, lo - (t0 - 1):hi - (t0 - 1)], self.PF[row0:row0 + 128, lo:hi], reads=[self.PF], writes=[rw])
                    dst = xr[:, m, t0:t0 + blk]
                    self.ts("dve", dst, rw[:, 0:blk], cw[:, m, 0:1], ALU.mult, [rw, cw, cb], [xr], s2=cb[:, m:m + 1], op1=ALU.add)
                    for k in range(1, 4):
                        self.stt(dst, rw[:, k:k + blk], cw[:, m, k:k + 1], dst, ALU.mult, ALU.add, [rw, cw, xr], [xr])
                    it += 1
                    rg = raw[it % 2]
                    self.dma("sp", rg[:, 0:blk], self.PF[row0 + 256:row0 + 384, t0:t0 + blk], reads=[self.PF], writes=[rg])
                    self.tt("pool", t1[:, 0:blk], rg[:, 0:blk], rg[:, 0:blk], ALU.mult, [rg], [t1])
                    self.ts("dve", t1[:, 0:blk], t1[:, 0:blk], 0.044715, ALU.mult, [t1], [t1], s2=1.0, op1=ALU.add)
                    self.tt("pool", t1[:, 0:blk], t1[:, 0:blk], rg[:, 0:blk], ALU.mult, [t1, rg], [t1])
                    self.act(t1[:, 0:blk], t1[:, 0:blk], AF.Tanh, [t1], [t1], scale=0.7978845608028654)
                    self.ts("dve", t2[:, 0:blk], rg[:, 0:blk], 0.5, ALU.mult, [rg], [t2])
                    self.stt(yg[:, m, t0:t0 + blk], t1[:, 0:blk], 1.0, t2[:, 0:blk], ALU.add, ALU.mult, [t1, t2], [yg])
                    it += 1
        self.pop()
        pr = [self.ps(f'pr{i}', [128, 512]) for i in range(2)]
        pi = [self.ps(f'pi{i}', [128, 512]) for i in range(2)]
        rt = self.sb('rt', [128, 512]); itl = self.sb('itl', [128, 512]); mt = self.sb('mt', [128, 512])
        blocks = [(0, 256)] + [(T_CTX + i * 512, 512) for i in range(4)]
        it = 0
        for d in range(2):
            for m in range(2):
                for (t0, blk) in blocks:
                    p1 = pr[it % 2]; p2 = pi[it % 2]
                    src = xr[:, m, t0:t0 + blk]
                    ir = (d * 2 + 0) * 2 + m; ii = (d * 2 + 1) * 2 + m
                    self.mm(p1[:, 0:blk], WB[:, ir, :], src, True, True, [WB, xr], [p1])
                    self.mm(p2[:, 0:blk], WB[:, ii, :], src, True, True, [WB, xr], [p2])
                    self.act(rt[:, 0:blk], p1[:, 0:blk], AF.Sigmoid, [p1, bri], [rt], bias=bri[:, ir:ir + 1])
                    self.act(itl[:, 0:blk], p2[:, 0:blk], AF.Sigmoid, [p2, bri], [itl], bias=bri[:, ii:ii + 1])
                    a_ = A[:, m, t0:t0 + blk]
                    self.act(a_, rt[:, 0:blk], AF.Exp, [rt, cch], [A], scale=cch[:, d * 2 + m:d * 2 + m + 1])
                    self.tt("pool", mt[:, 0:blk], a_, a_, ALU.mult, [A], [mt])
                    self.ts("dve", mt[:, 0:blk], mt[:, 0:blk], -1.0, ALU.mult, [mt], [mt], s2=1.0, op1=ALU.add)
                    self.act(mt[:, 0:blk], mt[:, 0:blk], AF.Sqrt, [mt], [mt])
                    self.tt("dve", mt[:, 0:blk], mt[:, 0:blk], itl[:, 0:blk], ALU.mult, [mt, itl], [mt])
                    self.tt("pool", BX[:, m, t0:t0 + blk], mt[:, 0:blk], src, ALU.mult, [mt, xr], [BX])
                    it += 1
                if d == 0:
                    self.op("dve", lambda e: e.tensor_tensor_scan(out=H[0][:, m, :], data0=A[:, m, :], data1=BX[:, m, :],
                                                                   initial=0.0, op0=ALU.mult, op1=ALU.add), [A, BX], [H[0]])
                else:
                    rv = self.rev_ap
                    self.op("dve", lambda e: e.tensor_tensor_scan(out=rv(H[1][:, m, 0:T_CTX]), data0=rv(A[:, m, 0:T_CTX]),
                                                                   data1=rv(BX[:, m, 0:T_CTX]), initial=0.0,
                                                                   op0=ALU.mult, op1=ALU.add), [A, BX], [H[1]])
                    self.op("dve", lambda e: e.tensor_tensor_scan(out=rv(H[1][:, m, T_CTX:T]), data0=rv(A[:, m, T_CTX:T]),
                                                                   data1=rv(BX[:, m, T_CTX:T]), initial=H[1][:, m, 0:1],
                                                                   op0=ALU.mult, op1=ALU.add), [A, BX, H[1]], [H[1]])
        for m in range(2):
            self.tt("pool", H[0][:, m, :], H[0][:, m, :], H[1][:, m, :], ALU.add, [H[0], H[1]], [H[0]])
            self.tt("dve", H[0][:, m, :], H[0][:, m, :], yg[:, m, :], ALU.mult, [H[0], yg], [H[0]])
        self.dma("sp", self.MO[256:512, :].rearrange("(m p) t -> p m t", p=128), H[0][:], reads=[H[0]], writes=[self.MO])
        self.pop()

    def phase_att(self, l):
        self.push()
        qT = self.sb('qT', [64, 8, T], F32R)
        kT = self.sb('kT', [64, 2, T], F32R)
        V = self.sb('V', [128, NT, 2, 65], F32R)
        onesf = self.sb('onesf', [128, NT * 2])
        self.memset("pool", onesf[:], 1.0, [onesf])
        self.copy("dve", V[:, :, :, 64], onesf[:].rearrange("p (t g) -> p t g", g=2), [onesf], [V])
        gbc = self.sb('gbc', [128, 10, 64])
        self.dma("sp", gbc[:, 0, :], self.inp['attn_q_norm_g'][l:l + 1, :].partition_broadcast(128), writes=[gbc])
        self.dma("sp", gbc[:, 8, :], self.inp['attn_k_norm_g'][l:l + 1, :].partition_broadcast(128), writes=[gbc])
        negc = self.sb('negc', [128, 4])
        self.op("dve", lambda e: e.tensor_reduce(out=negc[:, 0:1], in_=gbc[:, 0, :], axis=AX.X, op=ALU.max, apply_absolute_value=True), [gbc], [negc])
        self.op("dve", lambda e: e.tensor_reduce(out=negc[:, 1:2], in_=gbc[:, 8, :], axis=AX.X, op=ALU.max, apply_absolute_value=True), [gbc], [negc])
        self.tt("dve", negc[:, 2:3], negc[:, 0:1], negc[:, 1:2], ALU.mult, [negc], [negc])
        self.ts("dve", negc[:, 3:4], negc[:, 2:3], -8.0, ALU.mult, [negc], [negc])
        self.ts("dve", gbc[:, 0, :], gbc[:, 0, :], 0.125, ALU.mult, [gbc], [gbc])
        for h in range(1, 8):
            self.copy("dve", gbc[:, h, :], gbc[:, 0, :], [gbc], [gbc])
        self.copy("dve", gbc[:, 9, :], gbc[:, 8, :], [gbc], [gbc])
        self.push()
        at = [self.sb(f'at{i}', [128, 768]) for i in range(2)]
        an = [self.sb(f'an{i}', [128, 640]) for i in range(2)]
        sqa = self.sb('sqa', [128, 640])
        ssa = self.sb('ssa', [128, 20])
        cs_t = [self.sb(f'cs{i}', [128, 64]) for i in range(2)]
        r1 = self.sb('r1', [128, 10, 2, 16]); r2 = self.sb('r2', [128, 10, 2, 16])
        r3 = self.sb('r3', [128, 10, 2, 16]); r4 = self.sb('r4', [128, 10, 2, 16])
        ptq = [self.ps(f'ptq{i}', [128, 512]) for i in range(3)]
        for tt in range(NT):
            a_ = at[tt % 2]; n_ = an[tt % 2]
            self.dma("sp", a_[:], self.PT[tt * 128:(tt + 1) * 128, PT_ATT:PT_ATT + 768], reads=self.PT.rk(tt), writes=[a_])
            self.tt("pool", sqa[:], a_[:, 0:640], a_[:, 0:640], ALU.mult, [a_], [sqa])
            self.op("dve", lambda e: e.tensor_reduce(out=ssa[:, 0:10], in_=sqa[:].rearrange("p (h n) -> p h n", h=10), axis=AX.X, op=ALU.add), [sqa], [ssa])
            self.act(ssa[:, 10:20], ssa[:, 0:10], AF.Sqrt, [ssa], [ssa], scale=1.0 / 64, bias=self.eps_t[:, 0:1])
            self.op("dve", lambda e: e.reciprocal(out=ssa[:, 0:10], in_=ssa[:, 10:20]), [ssa], [ssa])
            n3 = n_[:].rearrange("p (h n) -> p h n", h=10)
            self.tt("dve", n3, a_[:, 0:640].rearrange("p (h n) -> p h n", h=10), ssa[:, 0:10].unsqueeze(2).to_broadcast([128, 10, 64]), ALU.mult, [a_, ssa], [n_])
            self.tt("pool", n_[:], n_[:], gbc[:].rearrange("p h n -> p (h n)"), ALU.mult, [n_, gbc], [n_])
            if tt >= 2:
                c_ = cs_t[tt % 2]
                lt = tt - 2
                self.dma("sp", c_[:], self.inp['rope_cs'][lt * 128:(lt + 1) * 128, :], writes=[c_])
                n5 = n_[:].rearrange("p (h a f n) -> p h a f n", h=10, a=2, f=2)
                x1 = n5[:, :, :, 0, :]; x2 = n5[:, :, :, 1, :]
                cosb = c_[:, 0:32].rearrange("p (a n) -> p a n", a=2).unsqueeze(1).to_broadcast([128, 10, 2, 16])
                sinb = c_[:, 32:64].rearrange("p (a n) -> p a n", a=2).unsqueeze(1).to_broadcast([128, 10, 2, 16])
                self.tt("dve", r1[:], x1, cosb, ALU.mult, [n_, c_], [r1])
                self.tt("pool", r2[:], x2, sinb, ALU.mult, [n_, c_], [r2])
                self.tt("dve", r3[:], x1, sinb, ALU.mult, [n_, c_], [r3])
                self.tt("pool", r4[:], x2, cosb, ALU.mult, [n_, c_], [r4])
                self.tt("dve", x1, r1[:], r2[:], ALU.subtract, [r1, r2], [n_])
                self.tt("pool", x2, r3[:], r4[:], ALU.add, [r3, r4], [n_])
            ts_ = slice(tt * 128, (tt + 1) * 128)
            for grp in range(3):
                p = ptq[grp]
                hs = range(4) if grp < 2 else range(2)
                for j in hs:
                    h = grp * 4 + j
                    self.mm(p[0:64, j * 128:(j + 1) * 128], n_[:, h * 64:(h + 1) * 64], self.ident[:, :], True, True, [n_, self.ident], [p])
                if grp < 2:
                    self.evac(grp, qT[:, grp * 4:(grp + 1) * 4, ts_], p[0:64, :].rearrange("p (h t) -> p h t", h=4), [p], [qT])
                else:
                    self.evac(grp, kT[:, :, ts_], p[0:64, 0:256].rearrange("p (h t) -> p h t", h=2), [p], [kT])
            self.copy("dve", V[:, tt, :, 0:64], a_[:, 640:768].rearrange("p (g n) -> p g n", g=2), [a_], [V])
        self.pop()
        pS = [self.ps(f'pS{i}', [128, 512]) for i in range(2)]
        pO = [self.ps(f'pO{i}', [128, 512]) for i in range(2)]
        pBc = self.ps('pBc', [128, 512])
        ones64 = self.sb('ones64', [128, 64])
        self.memset("pool", ones64[:], 1.0, [ones64])
        Pt = [self.sb(f'Pt{i}', [128, 512], F32R) for i in range(3)]
        rsb = self.sb('rsb', [128, 512]); bcs = self.sb('bcs', [64, 512])
        osg = [self.sb(f'osg{i}', [64, 512]) for i in range(2)]
        qblocks = [(0, 256, [0, 1])] + [(T_CTX + i * 512, 512, list(range(NT))) for i in range(4)]
        ip = 0; io = 0
        for (q0, qn_, ktiles) in qblocks:
            for h in range(8):
                g = h // 4
                po = pO[io % 2]
                for ki, kt in enumerate(ktiles):
                    ps_ = pS[ip % 2]; pt = Pt[ip % 3]
                    self.mm(ps_[:, 0:qn_], kT[:, g, kt * 128:(kt + 1) * 128], qT[:, h, q0:q0 + qn_], True, True, [kT, qT], [ps_])
                    self.act(pt[:, 0:qn_], ps_[:, 0:qn_], AF.Exp, [ps_, negc], [pt], bias=negc[:, 3:4])
                    self.mm(po[0:65, 0:qn_], V[:, kt, g, :], pt[:, 0:qn_], ki == 0, ki == len(ktiles) - 1, [V, pt], [po])
                    ip += 1
                self.op("dve", lambda e: e.reciprocal(out=rsb[64:65, 0:qn_], in_=po[64:65, 0:qn_]), [po], [rsb])
                self.mm(pBc[0:64, 0:qn_], onesf[64:65, 0:64 - 28] if False else ones64[64:65, 0:64], rsb[64:65, 0:qn_], True, True, [ones64, rsb], [pBc])
                self.copy("act", bcs[:, 0:qn_], pBc[0:64, 0:qn_], [pBc], [bcs])
                og = osg[io % 2]
                self.tt("dve", og[:, 0:qn_], po[0:64, 0:qn_], bcs[:, 0:qn_], ALU.mult, [po, bcs], [og])
                self.dma("sp", self.MO[512 + h * 64:512 + (h + 1) * 64, q0:q0 + qn_], og[:, 0:qn_], reads=[og], writes=[self.MO])
                io += 1
        self.pop()

    return dict(rev_ap=rev_ap, phase_lru=phase_lru, phase_att=phase_att)


for _k, _v in _lru_att_methods().items():
    setattr(Kern, _k, _v)


N_OV = 36
N_SLOT = (64 + N_OV) * 128


def _tail_methods():
    def phase_wout(self, l):
        self.push()
        wo = self.sb('wo', [128, 8, D], F32R)
        self.dma("pool", wo[:], self.inp['w_out'][l].rearrange("(kc p) n -> p kc n", p=128), writes=[wo])
        g1 = [self.sb(f'g1{r}', [128, D]) for r in range(2)]
        for r in range(2):
            self.load_mod(g1[r], 2, r)
        mo = [self.sb(f'mo{i}', [128, 8, 512], F32R) for i in range(2)]
        xt = [self.sb(f'xw{i}', [128, D]) for i in range(2)]
        tw = [self.sb(f'tw{i}', [128, D]) for i in range(2)]
        pw = [self.ps(f'pw{i}', [128, 512]) for i in range(4)]
        blocks = [(0, 2), (2, 4), (6, 4), (10, 4), (14, 4)]
        msrc = self.MO[:, :].rearrange("(kc p) t -> p kc t", p=128)
        it = 0
        for bi, (t0, ntile) in enumerate(blocks):
            r = 1 if t0 == 0 else 0
            m_ = mo[bi % 2]
            ntok = ntile * 128
            self.dma("pool", m_[:, :, 0:ntok], msrc[:, :, t0 * 128:t0 * 128 + ntok], reads=[self.MO], writes=[m_])
            for j in range(ntile):
                tt = t0 + j
                x_ = xt[it % 2]; t_ = tw[it % 2]
                self.dma("sp", x_[:], self.XS[tt * 128:(tt + 1) * 128, :], reads=self.XS.rk(tt), writes=[x_])
                for half in range(2):
                    p = pw[(it % 2) * 2 + half]
                    for kc in range(8):
                        self.mm(p[:], m_[:, kc, j * 128:(j + 1) * 128], wo[:, kc, half * 512:(half + 1) * 512], kc == 0, kc == 7, [m_, wo], [p])
                    hs = slice(half * 512, (half + 1) * 512)
                    self.tt("dve", t_[:, hs], p[:], g1[r][:, hs], ALU.mult, [p, g1[r]], [t_])
                self.tt("pool", x_[:], x_[:], t_[:], ALU.add, [x_, t_], [x_])
                self.dma("sp", self.XS[tt * 128:(tt + 1) * 128, :], x_[:], reads=[x_], writes=self.XS.rk(tt))
                it += 1
        self.pop()

    def phase_moe(self, l):
        self.push()
        mc = self.sb('mc', [128, 256])
        self.dma("sp", mc[:], self.inp['moe_const'][:, :], writes=[mc])
        iota64 = mc[:, 0:64]; thr = mc[:, 64:100]; bvals = mc[:, 100:136]; pidx = mc[:, 136:137]; ones64 = mc[:, 137:201]
        ustr = self.sb('ustr', [128, 256])
        self.dma("sp", ustr[:], self.inp['moe_tri'][:, :], writes=[ustr])
        OH = self.sb('OH', [128, NT, 2, 64])
        gates = self.sb('gates', [128, NT, 2])
        dest = self.sb('dest', [128, NT, 2], I32)
        wix = self.sb('wix', [128, 2, N_OV], I32)
        XB, YB = self.XB, self.YB
        self.push()
        gm = [self.sb(f'gm2{r}', [128, D]) for r in range(2)]
        sh = [self.sb(f'sh2{r}', [128, D]) for r in range(2)]
        self.push()
        for r in range(2):
            self.load_mod(gm[r], 4, r, 'norm2_g', l)
            self.load_mod(sh[r], 3, r)
        self.pop()
        hbuf = self.sb('hbuf', [128, NT, D], nsub=NT)
        wr = self.sb('wr', [128, 8, 72])
        self.dma("sp", wr[:, :, 0:8], self.inp['moe_w_group'][l].rearrange("(kc p) n -> p kc n", p=128), writes=[wr])
        self.dma("sp", wr[:, :, 8:72], self.inp['moe_w_expert'][l].rearrange("(kc p) n -> p kc n", p=128), writes=[wr])
        rb = self.sb('rb', [128, 72])
        self.dma("sp", rb[:, 0:8], self.inp['moe_b_group'][l:l + 1, :].partition_broadcast(128), writes=[rb])
        self.dma("sp", rb[:, 8:72], self.inp['moe_b_expert'][l:l + 1, :].partition_broadcast(128), writes=[rb])
        xt = [self.sb(f'xm{i}', [128, D]) for i in range(2)]
        junk = self.sb('junk', [128, D])
        ss = [self.sb(f'ssm{i}', [128, 4]) for i in range(2)]
        hT = self.sb('hT', [128, 8, 128])
        ptp = [self.ps(f'ptp{i}', [128, 512]) for i in range(2)]
        plg = self.ps('plg', [128, 72])
        prk = self.ps('prk', [128, 64]); pcs = self.ps('pcs', [128, 64])
        lg = self.sb('lg', [128, 72]); sm = self.sb('sm', [128, 16])
        ohg = self.sb('ohg', [128, 8]); tmp64 = self.sb('tmp64', [128, 64]); le = self.sb('le', [128, 8]); le2 = self.sb('le2', [128, 8])
        oh1 = self.sb('oh1', [128, 8]); oh2 = self.sb('oh2', [128, 8]); eg = self.sb('eg', [128, 8])
        for tt in range(NT):
            r = 1 if tt < 2 else 0
            x_ = xt[tt % 2]; ss_ = ss[tt % 2]
            self.dma("sp", x_[:], self.XS[tt * 128:(tt + 1) * 128, :], reads=self.XS.rk(tt), writes=[x_])
            hcur = Tl(hbuf[:, tt, :], 'hcur'); hcur.rs = hbuf.rk(tt)
            self.norm_tile(x_, gm[r], sh[r], hcur, junk, ss_)
            for half in range(2):
                p = ptp[half]
                for q in range(4):
                    kc = half * 4 + q
                    self.tr(p[:, q * 128:(q + 1) * 128], hbuf[:, tt, kc * 128:(kc + 1) * 128], self.ident[:], [hbuf.rk(tt), self.ident], [p])
                self.evac(half, hT[:, half * 4:(half + 1) * 4, :], p[:].rearrange("p (q t) -> p q t", q=4), [p], [hT])
            for kc in range(8):
                self.mm(plg[:], hT[:, kc, :], wr[:, kc, :], kc == 0, kc == 7, [hT, wr], [plg])
            self.tt("dve", lg[:], plg[:], rb[:], ALU.add, [plg, rb], [lg])
            self.op("dve", lambda e: e.tensor_reduce(out=sm[:, 0:1], in_=lg[:, 0:8], axis=AX.X, op=ALU.max), [lg], [sm])
            self.ts("dve", ohg[:], lg[:, 0:8], sm[:, 0:1], ALU.is_equal, [lg, sm], [ohg])
            self.ts("dve", sm[:, 1:2], sm[:, 0:1], -1.0, ALU.mult, [sm], [sm])
            self.act(eg[:], lg[:, 0:8], AF.Exp, [lg, sm], [eg, sm], bias=sm[:, 1:2], accum_out=sm[:, 2:3])
            self.op("dve", lambda e: e.reciprocal(out=sm[:, 3:4], in_=sm[:, 2:3]), [sm], [sm])
            self.tt("dve", tmp64[:].rearrange("p (g e) -> p g e", g=8), lg[:, 8:72].rearrange("p (g e) -> p g e", g=8),
                    ohg[:].unsqueeze(2).to_broadcast([128, 8, 8]), ALU.mult, [lg, ohg], [tmp64])
            self.op("dve", lambda e: e.tensor_reduce(out=le[:], in_=tmp64[:].rearrange("p (g e) -> p e g", g=8), axis=AX.X, op=ALU.add), [tmp64], [le])
            self.op("dve", lambda e: e.tensor_reduce(out=sm[:, 4:5], in_=le[:], axis=AX.X, op=ALU.max), [le], [sm])
            self.ts("dve", oh1[:], le[:], sm[:, 4:5], ALU.is_equal, [le, sm], [oh1])
            self.stt(le2[:], oh1[:], -1.0e30, le[:], ALU.mult, ALU.add, [oh1, le], [le2])
            self.op("dve", lambda e: e.tensor_reduce(out=sm[:, 5:6], in_=le2[:], axis=AX.X, op=ALU.max), [le2], [sm])
            self.ts("dve", oh2[:], le2[:], sm[:, 5:6], ALU.is_equal, [le2, sm], [oh2])
            self.tt("dve", sm[:, 6:7], sm[:, 5:6], sm[:, 4:5], ALU.subtract, [sm], [sm])
            self.act(sm[:, 7:8], sm[:, 6:7], AF.Exp, [sm], [sm])
            self.ts("dve", sm[:, 8:9], sm[:, 7:8], 1.0, ALU.add, [sm], [sm])
            self.op("dve", lambda e: e.reciprocal(out=sm[:, 9:10], in_=sm[:, 8:9]), [sm], [sm])
            self.tt("dve", gates[:, tt, 0:1], sm[:, 3:4], sm[:, 9:10], ALU.mult, [sm], [gates])
            self.tt("dve", gates[:, tt, 1:2], gates[:, tt, 0:1], sm[:, 7:8], ALU.mult, [sm, gates], [gates])
            for k, ohk in enumerate((oh1, oh2)):
                self.tt("dve", OH[:, tt, k, :].rearrange("p (g e) -> p g e", g=8), ohg[:].unsqueeze(2).to_broadcast([128, 8, 8]),
                        ohk[:].unsqueeze(1).to_broadcast([128, 8, 8]), ALU.mult, [ohg, ohk], [OH])
        Osum = self.sb('Osum', [128, NT, 64])
        self.tt("dve", Osum[:], OH[:, :, 0, :], OH[:, :, 1, :], ALU.add, [OH], [Osum])
        rank = self.sb('rank', [128, NT, 64])
        pref = self.sb('pref', [128, 64])
        self.memset("pool", pref[:], 0.0, [pref])
        for tt in range(NT):
            self.mm(prk[:], ustr[:, 0:128], Osum[:, tt, :], True, True, [ustr, Osum], [prk])
            self.mm(pcs[:], ustr[:, 128:256], Osum[:, tt, :], True, True, [ustr, Osum], [pcs])
            self.tt("dve", rank[:, tt, :], prk[:], pref[:], ALU.add, [prk, pref], [rank])
            self.tt("dve", pref[:], pcs[:], pref[:], ALU.add, [pcs, pref], [pref])
        cmpb = self.sb('cmpb', [128, 64, 36])
        nblk = self.sb('nblk', [128, 64]); ovf = self.sb('ovf', [128, 64]); incl = self.sb('incl', [128, 64]); delta = self.sb('delta', [128, 64])
        self.tt("dve", cmpb[:], pref[:].unsqueeze(2).to_broadcast([128, 64, 36]), thr.unsqueeze(1).to_broadcast([128, 64, 36]), ALU.is_gt, [pref, mc], [cmpb])
        self.op("dve", lambda e: e.tensor_reduce(out=nblk[:], in_=cmpb[:], axis=AX.X, op=ALU.add), [cmpb], [nblk])
        self.ts("dve", ovf[:], nblk[:], -1.0, ALU.add, [nblk], [ovf], s2=0.0, op1=ALU.max)
        self.op("dve", lambda e: e.tensor_tensor_scan(out=incl[:], data0=ones64, data1=ovf[:], initial=0.0, op0=ALU.mult, op1=ALU.add), [mc, ovf], [incl])
        self.tt("dve", delta[:], incl[:], ovf[:], ALU.subtract, [incl, ovf], [delta])
        self.tt("dve", delta[:], delta[:], iota64, ALU.subtract, [delta, mc], [delta])
        self.ts("dve", delta[:], delta[:], 128.0, ALU.mult, [delta], [delta], s2=8064.0, op1=ALU.add)
        base1 = self.sb('base1', [128, 64])
        self.ts("dve", base1[:], iota64, 128.0, ALU.mult, [mc], [base1])
        dall = self.sb('dall', [128, NT, 64]); ge = self.sb('ge', [128, NT, 64]); destf = self.sb('destf', [128, NT, 2])
        self.ts("dve", ge[:], rank[:], 128.0, ALU.is_ge, [rank], [ge])
        self.tt("dve", ge[:], ge[:], delta[:].unsqueeze(1).to_broadcast([128, NT, 64]), ALU.mult, [ge, delta], [ge])
        self.tt("dve", dall[:], rank[:], base1[:].unsqueeze(1).to_broadcast([128, NT, 64]), ALU.add, [rank, base1], [dall])
        self.tt("dve", dall[:], dall[:], ge[:], ALU.add, [dall, ge], [dall])
        for k in range(2):
            self.tt("dve", ge[:], OH[:, :, k, :], dall[:], ALU.mult, [OH, dall], [ge])
            self.op("dve", lambda e: e.tensor_reduce(out=destf[:, :, k], in_=ge[:], axis=AX.X, op=ALU.add), [ge], [destf])
        self.copy("dve", dest[:], destf[:], [destf], [dest])
        cmp2 = self.sb('cmp2', [128, 36, 64]); bke = self.sb('bke', [128, 36]); wixf = self.sb('wixf', [128, 2, 36])
        self.tt("dve", cmp2[:], incl[:].unsqueeze(1).to_broadcast([128, 36, 64]), bvals.unsqueeze(2).to_broadcast([128, 36, 64]), ALU.is_le, [incl, mc], [cmp2])
        self.op("dve", lambda e: e.tensor_reduce(out=bke[:], in_=cmp2[:], axis=AX.X, op=ALU.add), [cmp2], [bke])
        p2 = self.sb('p2', [128, 1])
        self.ts("dve", p2[:], pidx, 2.0, ALU.mult, [mc], [p2])
        self.ts("dve", wixf[:, 0, :], bke[:], 256.0, ALU.mult, [bke, p2], [wixf], s2=p2[:, 0:1], op1=ALU.add)
        self.ts("dve", wixf[:, 1, :], bke[:], 256.0, ALU.mult, [bke, p2], [wixf], s2=p2[:, 0:1], op1=ALU.add)
        self.copy("dve", wix[:], wixf[:], [wixf], [wix])
        for tt in range(NT):
            for k in range(2):
                self.S.dma("pool", lambda e, tt=tt, k=k: e.indirect_dma_start(
                    out=XB[:, :], out_offset=bass.IndirectOffsetOnAxis(ap=dest[:, tt, k:k + 1], axis=0),
                    in_=hbuf[:, tt, :], in_offset=None), _flat([hbuf.rk(tt), dest]), _flat([XB]))
        self.pop()
        self.push()
        NWB = 3
        w1b = [self.sb(f'w1b{i}', [128, 8, 512], F32R) for i in range(NWB)]
        w3b = [self.sb(f'w3b{i}', [128, 8, 512], F32R) for i in range(NWB)]
        w2b = [self.sb(f'w2b{i}', [128, 4, D], F32R) for i in range(NWB)]
        xb = [self.sb(f'xb{i}', [128, D]) for i in range(2)]
        xbT = self.sb('xbT', [128, 8, 128], F32R)
        a1 = self.sb('a1', [128, 512]); hh = self.sb('hh', [128, 512])
        hhT = self.sb('hhT', [128, 4, 128], F32R)
        yb = [self.sb(f'yb{i}', [128, D]) for i in range(2)]
        ptp = [self.ps(f'ptq{i}', [128, 512]) for i in range(2)]
        ph1 = self.ps('ph1', [128, 512]); ph3 = self.ps('ph3', [128, 512]); pht = self.ps('pht', [128, 512])
        py = [self.ps(f'py{i}', [128, 512]) for i in range(2)]
        w1v = self.inp['moe_w1'].rearrange("l e (r kc) n -> (l e r) (kc n)", kc=4)
        w3v = self.inp['moe_w3'].rearrange("l e (r kc) n -> (l e r) (kc n)", kc=4)
        w2v = self.inp['moe_w2'].rearrange("l e (r c) n -> (l e r) (c n)", c=2)
        loff = l * 64 * 1024 * 512
        if not hasattr(self, 'reg_b13'):
            self.reg_b13 = self.nc.gpsimd.to_reg(16383)
            self.reg_b2 = self.nc.gpsimd.to_reg(32767)
        reg_b13, reg_b2 = self.reg_b13, self.reg_b2
        order = []
        nxt2 = 0
        for e_ in range(64):
            order.append(e_)
            while nxt2 < N_OV and (nxt2 + 1) * 64 <= (e_ + 1) * N_OV:
                order.append(64 + nxt2)
                nxt2 += 1
        assert len(order) == 64 + N_OV and nxt2 == N_OV
        for pos, blk in enumerate(order):
            i = pos % 2
            iw = pos % NWB
            W1, W3, W2 = w1b[iw], w3b[iw], w2b[iw]
            if blk < 64:
                self.dma("pool", W1[:], self.inp['moe_w1'][l, blk].rearrange("(p kc) n -> p kc n", kc=8), writes=[W1], max_dma_last_dim=8192)
                self.dma("pool", W3[:], self.inp['moe_w3'][l, blk].rearrange("(p kc) n -> p kc n", kc=8), writes=[W3], max_dma_last_dim=8192)
                self.dma("pool", W2[:], self.inp['moe_w2'][l, blk].rearrange("(p kc) n -> p kc n", kc=4), writes=[W2], max_dma_last_dim=8192)
            else:
                b = blk - 64
                for (Wt, wv) in ((W1, w1v), (W3, w3v), (W2, w2v)):
                    w2d = Wt[:].rearrange("p k n -> p (k n)")
                    for half in range(2):
                        self.S.dma("pool", lambda e, w2d=w2d, wv=wv, half=half, b=b: e.indirect_dma_start(
                            out=w2d[:, half * 2048:(half + 1) * 2048], out_offset=None, in_=wv[:, :],
                            in_offset=bass.IndirectOffsetOnAxis(ap=wix[:, 0, b:b + 1], axis=0),
                            element_offset=loff + half * 2048, bounds_check=reg_b13, oob_is_err=False), _flat([wix]), _flat([Wt]))
            x_ = xb[i]
            self.dma("sp", x_[:], XB[blk * 128:(blk + 1) * 128, :], reads=[XB], writes=[x_])
            for half in range(2):
                p = ptp[half]
                for q in range(4):
                    kc = half * 4 + q
                    self.tr(p[:, q * 128:(q + 1) * 128], x_[:, kc:D:8], self.ident[:], [x_, self.ident], [p])
                self.evac(half, xbT[:, half * 4:(half + 1) * 4, :], p[:].rearrange("p (q t) -> p q t", q=4), [p], [xbT])
            for kc in range(8):
                self.mm(ph1[:], xbT[:, kc, :], W1[:, kc, :], kc == 0, kc == 7, [xbT, W1], [ph1])
            for kc in range(8):
                self.mm(ph3[:], xbT[:, kc, :], W3[:, kc, :], kc == 0, kc == 7, [xbT, W3], [ph3])
            self.act(a1[:], ph1[:], AF.Silu, [ph1], [a1])
            self.tt("dve", hh[:], a1[:], ph3[:], ALU.mult, [a1, ph3], [hh])
            for q in range(4):
                self.tr(pht[:, q * 128:(q + 1) * 128], hh[:, q:512:4], self.ident[:], [hh, self.ident], [pht])
            self.copy("act", hhT[:], pht[:].rearrange("p (q t) -> p q t", q=4), [pht], [hhT])
            y_ = yb[i]
            for half in range(2):
                p = py[half]
                for c in range(4):
                    self.mm(p[:], hhT[:, c, :], W2[:, c, half * 512:(half + 1) * 512], c == 0, c == 3, [hhT, W2], [p])
                self.evac(half, y_[:, half * 512:(half + 1) * 512], p[:], [p], [y_])
            self.dma("sp", YB[blk * 128:(blk + 1) * 128, :], y_[:], reads=[y_], writes=[YB])
        self.pop()
        self.push()
        g2 = [self.sb(f'g2{r}', [128, D]) for r in range(2)]
        for r in range(2):
            self.load_mod(g2[r], 5, r)
        Y0 = [self.sb(f'Y0{i}', [128, D]) for i in range(2)]
        Y1 = [self.sb(f'Y1{i}', [128, D]) for i in range(2)]
        xt = [self.sb(f'xf{i}', [128, D]) for i in range(2)]
        for tt in range(NT):
            r = 1 if tt < 2 else 0
            i = tt % 2
            for k, Yk in enumerate((Y0[i], Y1[i])):
                self.S.dma("pool", lambda e, Yk=Yk, tt=tt, k=k: e.indirect_dma_start(
                    out=Yk[:, :], out_offset=None, in_=YB[:, :],
                    in_offset=bass.IndirectOffsetOnAxis(ap=dest[:, tt, k:k + 1], axis=0)), _flat([YB, dest]), _flat([Yk]))
            x_ = xt[i]
            self.dma("sp", x_[:], self.XS[tt * 128:(tt + 1) * 128, :], reads=self.XS.rk(tt), writes=[x_])
            self.ts("dve", Y0[i][:], Y0[i][:], gates[:, tt, 0:1], ALU.mult, [Y0[i], gates], [Y0[i]])
            self.stt(Y0[i][:], Y1[i][:], gates[:, tt, 1:2], Y0[i][:], ALU.mult, ALU.add, [Y1[i], Y0[i], gates], [Y0[i]])
            self.tt("pool", Y0[i][:], Y0[i][:], g2[r][:], ALU.mult, [Y0[i], g2[r]], [Y0[i]])
            self.tt("dve", x_[:], x_[:], Y0[i][:], ALU.add, [x_, Y0[i]], [x_])
            self.dma("sp", self.XS[tt * 128:(tt + 1) * 128, :], x_[:], reads=[x_], writes=self.XS.rk(tt))
        self.pop()
        self.pop()

    def phase_final(self):
        self.push()
        fg = self.sb('fg', [128, D])
        self.dma("sp", fg[:], self.inp['final_norm_g'][0:1, :].partition_broadcast(128), writes=[fg])
        xt = [self.sb(f'xo{i}', [128, D]) for i in range(2)]
        xn = [self.sb(f'xq{i}', [128, D]) for i in range(2)]
        junk = self.sb('junk', [128, D])
        ss = [self.sb(f'sso{i}', [128, 4]) for i in range(2)]
        evs = []
        for tt in range(2, NT):
            i = tt % 2
            self.dma("sp", xt[i][:], self.XS[tt * 128:(tt + 1) * 128, :], reads=self.XS.rk(tt), writes=[xt[i]])
            self.norm_tile(xt[i], fg, None, xn[i], junk, ss[i])
            evs.append(self.dma("sp", self.out[(tt - 2) * 128:(tt - 1) * 128, :], xn[i][:], reads=[xn[i]]))
        for ev in evs:
            self.S.wait_event("sp", ev)
        self.pop()

    return dict(phase_wout=phase_wout, phase_moe=phase_moe, phase_final=phase_final)


for _k, _v in _tail_methods().items():
    setattr(Kern, _k, _v)
```

```python
import contextlib
import numpy as np
import concourse.bass as bass
import concourse.mybir as mybir
from concourse.bass_utils import run_bass_kernel_spmd

F32 = mybir.dt.float32
F32R = mybir.dt.float32r
BF16 = mybir.dt.bfloat16
I32 = mybir.dt.int32
AF = mybir.ActivationFunctionType
ALU = mybir.AluOpType
AX = mybir.AxisListType

D = 1024
T_CTX = 256
T_LAT = 2048
T = T_CTX + T_LAT
NT = T // 128
NCH = T // 64
DEPTH = 4
DIN = 2320
EPS = 1e-6
NEG = -30000.0


class Res:
    __slots__ = ("name", "writer", "readers", "excl")

    def __init__(self, name):
        self.name = name
        self.writer = None
        self.readers = []
        self.excl = False


class Sched:
    ENG = ("pe", "dve", "act", "pool", "sp")

    def __init__(self, nc, n_dma_sems=8):
        self.nc = nc
        self.obj = {"pe": nc.tensor, "dve": nc.vector, "act": nc.scalar,
                    "pool": nc.gpsimd, "sp": nc.sync}
        self.sem = {}
        self.count = {}
        self.waited = {e: {} for e in self.ENG}
        self._ctx = []
        for e in self.ENG:
            cm = nc.semaphore("s_" + e)
            self.sem[e] = cm.__enter__()
            self._ctx.append(cm)
            self.count[e] = 0
        self.dma_sems = {}
        self.dma_rr = {}
        self.gen = 0
        self.n_dma_sems = n_dma_sems
        for q in ("sp", "pool"):
            lst = []
            for i in range(n_dma_sems):
                cm = nc.semaphore(f"d_{q}{i}")
                lst.append([cm.__enter__(), 0])
                self._ctx.append(cm)
            self.dma_sems[q] = lst
            self.dma_rr[q] = 0
        self.n_wait = 0
        self.n_inst = 0

    def renew(self):
        self.barrier()
        self.gen += 1
        nc = self.nc
        for e in self.ENG:
            cm = nc.semaphore(f"s_{e}_g{self.gen}")
            self.sem[e] = cm.__enter__()
            self._ctx.append(cm)
            self.count[e] = 0
        for q in ("sp", "pool"):
            lst = []
            for i in range(self.n_dma_sems):
                cm = nc.semaphore(f"d_{q}{i}_g{self.gen}")
                lst.append([cm.__enter__(), 0])
                self._ctx.append(cm)
            self.dma_sems[q] = lst
            self.dma_rr[q] = 0
        self.waited = {e: {} for e in self.ENG}

    def _need(self, eng, ev, same_ok_dist=None):
        if ev is None:
            return None
        if len(ev) > 4 and ev[4] != self.gen:
            return None
        key, sem, val, src = ev[:4]
        if src == eng and src is not None:
            if same_ok_dist is None:
                return None
            if self.count[eng] - val >= same_ok_dist:
                return None
        if self.waited[eng].get(key, 0) >= val:
            return None
        return ev

    def _emit_waits(self, eng, evs):
        best = {}
        for ev in evs:
            if ev is None:
                continue
            key = ev[0]
            if key not in best or best[key][2] < ev[2]:
                best[key] = ev
        for key, evb in best.items():
            sem, val = evb[1], evb[2]
            self.obj[eng].wait_ge(sem, val)
            self.waited[eng][key] = val
            self.n_wait += 1

    def deps(self, eng, reads, writes):
        evs = []
        for r in reads:
            evs.append(self._need(eng, r.writer, same_ok_dist=4))
        for w in writes:
            evs.append(self._need(eng, w.writer, same_ok_dist=None))
            for rd in w.readers:
                evs.append(self._need(eng, rd, same_ok_dist=None))
        return evs

    def _commit(self, ev, reads, writes):
        for r in reads:
            r.readers.append(ev)
            if len(r.readers) > 48:
                best = {}
                for e in r.readers:
                    if e[4] != self.gen:
                        continue
                    if e[0] not in best or best[e[0]][2] < e[2]:
                        best[e[0]] = e
                r.readers = list(best.values())
        for w in writes:
            w.writer = ev
            w.readers = []

    def op(self, eng, fn, reads=(), writes=()):
        ex = [r for r in reads if r.excl]
        if ex:
            writes = list(writes) + ex
        self._emit_waits(eng, self.deps(eng, reads, writes))
        ins = fn(self.obj[eng])
        self.count[eng] += 1
        ins.then_inc(self.sem[eng], 1)
        ev = (eng, self.sem[eng], self.count[eng], eng, self.gen)
        self._commit(ev, reads, writes)
        self.n_inst += 1
        return ev

    def dma(self, q, fn, reads=(), writes=()):
        evs = self.deps(q, reads, writes)
        idx = self.dma_rr[q]
        slot = self.dma_sems[q][idx]
        self.dma_rr[q] = (idx + 1) % len(self.dma_sems[q])
        key = f"d_{q}{idx}"
        if slot[1] > 0:
            evs.append(self._need(q, (key, slot[0], slot[1], None, self.gen)))
        self._emit_waits(q, evs)
        ins = fn(self.obj[q])
        slot[1] += 16
        ins.then_inc(slot[0], 16)
        ev = (key, slot[0], slot[1], None, self.gen)
        self._commit(ev, reads, writes)
        self.n_inst += 1
        return ev

    def wait_event(self, eng, ev):
        self._emit_waits(eng, [self._need(eng, ev)])

    def barrier(self):
        evs = []
        for e in self.ENG:
            if self.count[e] > 0:
                evs.append((e, self.sem[e], self.count[e], e, self.gen))
        for q, lst in self.dma_sems.items():
            for i, (sem, val) in enumerate(lst):
                if val > 0:
                    evs.append((f"d_{q}{i}", sem, val, None, self.gen))
        for e in self.ENG:
            need = []
            for ev in evs:
                if ev[3] == e:
                    continue
                if self.waited[e].get(ev[0], 0) >= ev[2]:
                    continue
                need.append(ev)
            self._emit_waits(e, need)


class Tl:
    def __init__(self, ap, name, nsub=0):
        self.ap = ap
        self.name = name
        self.rs = [Res(f"{name}.{i}") for i in range(max(1, nsub))]

    def __getitem__(self, key):
        return self.ap[key]

    @property
    def r(self):
        return self.rs

    def rk(self, *ks):
        n = len(self.rs)
        return [self.rs[min(k, n - 1)] for k in ks]


def _flat(lst):
    out = []
    for x in lst:
        if isinstance(x, Tl):
            out.extend(x.rs)
        elif isinstance(x, (list, tuple)):
            out.extend(_flat(x))
        elif x is not None:
            out.append(x)
    return out


class Builder:
    def __init__(self, n_layers=DEPTH, debug=None, skip_inputs=(), flags=()):
        self.flags = set(flags)
        self.skip_inputs = set(skip_inputs)
        self.n_layers = n_layers
        self.debug = debug or {}
        self.nc = bass.Bass("TRN2", target_bir_lowering=False)
        self.S = Sched(self.nc)
        self.stack = [contextlib.ExitStack()]
        self._uid = 0
        self.outputs = []

    def push(self):
        self.stack.append(contextlib.ExitStack())

    def pop(self):
        self.S.barrier()
        self.stack.pop().close()

    def sb(self, name, shape, dt=F32, nsub=0):
        self._uid += 1
        t = self.stack[-1].enter_context(self.nc.sbuf_tensor(f"{name}_{self._uid}", list(shape), dt))
        return Tl(t, name, nsub)

    def ps(self, name, shape, dt=F32, nsub=0):
        self._uid += 1
        t = self.stack[-1].enter_context(self.nc.psum_tensor(f"{name}_{self._uid}", list(shape), dt))
        tl = Tl(t, name, nsub)
        for r in tl.rs:
            r.excl = True
        return tl

    def dram(self, name, shape, dt=F32, kind="Internal", nsub=0):
        t = self.nc.dram_tensor(name, list(shape), dt, kind=kind).ap()
        return Tl(t, name, nsub)

    op_limit = None
    op_cnt = 0

    def op(self, eng, fn, reads=(), writes=()):
        if self.op_limit is not None:
            self.op_cnt += 1
            if self.op_cnt > self.op_limit:
                return None
            if self.op_cnt == self.op_limit:
                import inspect
                fr = inspect.stack()
                print("LAST OP:", eng, [f"{f.function}:{f.lineno}" for f in fr[1:5]])
        return self.S.op(eng, fn, _flat(reads), _flat(writes))

    def dma(self, q, out, in_, reads=(), writes=(), **kw):
        return self.S.dma(q, lambda e: e.dma_start(out=out, in_=in_, **kw), _flat(reads), _flat(writes))

    def mm(self, out, lhsT, rhs, start, stop, reads, writes):
        return self.op("pe", lambda e: e.matmul(out, lhsT=lhsT, rhs=rhs, start=start, stop=stop), reads, writes)

    def tr(self, out, in_, ident, reads, writes):
        return self.op("pe", lambda e: e.transpose(out, in_, ident), reads, writes)

    def act(self, out, in_, func, reads, writes, eng="act", **kw):
        return self.op("act", lambda e: e.activation(out=out, in_=in_, func=func, **kw), reads, writes)

    def tt(self, eng, out, in0, in1, op, reads, writes):
        return self.op(eng, lambda e: e.tensor_tensor(out=out, in0=in0, in1=in1, op=op), reads, writes)

    def ts(self, eng, out, in0, s1, op0, reads, writes, s2=None, op1=None):
        if op1 is None:
            return self.op(eng, lambda e: e.tensor_scalar(out=out, in0=in0, scalar1=s1, scalar2=None, op0=op0), reads, writes)
        return self.op(eng, lambda e: e.tensor_scalar(out=out, in0=in0, scalar1=s1, scalar2=s2, op0=op0, op1=op1), reads, writes)

    def stt(self, out, in0, scalar, in1, op0, op1, reads, writes):
        return self.op("dve", lambda e: e.scalar_tensor_tensor(out=out, in0=in0, scalar=scalar, in1=in1, op0=op0, op1=op1), reads, writes)

    def copy(self, eng, out, in_, reads, writes):
        if eng == "act":
            return self.op("act", lambda e: e.copy(out=out, in_=in_), reads, writes)
        return self.op(eng, lambda e: e.tensor_copy(out=out, in_=in_), reads, writes)

    def memset(self, eng, ap, val, writes):
        return self.op(eng, lambda e: e.memset(ap, val), (), writes)


C_GQ, C_GK, C_GV, C_GG, C_GAB = 0, 256, 512, 768, 1024
C_LX, C_LY = 1040, 1296
C_AQ, C_AK, C_AV = 1552, 2064, 2192
PF_GDN, PF_LRU = 0, 768
PT_GATE, PT_AB, PT_ATT = 0, 256, 272
PT_W = 1040

EXTRA_LAYOUT = ['gdn_conv_wT', 'lru_conv_wT']
WEIGHT_NAMES = ['w_ada', 'b_ada', 'norm1_g', 'norm2_g', 'w_in', 'w_out', 'gdn_conv_w', 'gdn_a_log',
                'gdn_dt_bias', 'gdn_norm_g', 'lru_conv_w', 'lru_conv_b', 'lru_w_r', 'lru_b_r', 'lru_w_i',
                'lru_b_i', 'lru_lambda', 'attn_q_norm_g', 'attn_k_norm_g', 'moe_w_group', 'moe_b_group',
                'moe_w_expert', 'moe_b_expert', 'moe_w1', 'moe_w3', 'moe_w2', 'final_norm_g']
WEIGHT_SHAPES = {
    'w_ada': [4, 1024, 6144], 'b_ada': [4, 6144], 'norm1_g': [4, 1024], 'norm2_g': [4, 1024],
    'w_in': [4, 1024, 2320], 'w_out': [4, 1024, 1024], 'gdn_conv_w': [4, 4, 768], 'gdn_a_log': [4, 2, 4],
    'gdn_dt_bias': [4, 2, 4], 'gdn_norm_g': [4, 64], 'lru_conv_w': [4, 4, 256], 'lru_conv_b': [4, 256],
    'lru_w_r': [4, 2, 4, 64, 64], 'lru_b_r': [4, 2, 256], 'lru_w_i': [4, 2, 4, 64, 64], 'lru_b_i': [4, 2, 256],
    'lru_lambda': [4, 2, 256], 'attn_q_norm_g': [4, 64], 'attn_k_norm_g': [4, 64], 'moe_w_group': [4, 1024, 8],
    'moe_b_group': [4, 8], 'moe_w_expert': [4, 1024, 64], 'moe_b_expert': [4, 64],
    'moe_w1': [4, 64, 1024, 512], 'moe_w3': [4, 64, 1024, 512], 'moe_w2': [4, 64, 512, 1024],
    'final_norm_g': [1, 1024], 'gdn_conv_wT': [4, 768, 4], 'lru_conv_wT': [4, 256, 4]}


def host_consts():
    c = {}
    c['ident'] = np.eye(128, dtype=np.float32)
    r = np.arange(64)[:, None]
    q = np.arange(64)[None, :]
    def t4(m):
        return np.tile(m.astype(np.float32), (1, 4))
    g = np.zeros((64, 9, 256), np.float32)
    g[:, 0] = t4(np.where(r >= q, 0.0, NEG))
    g[:, 1] = t4(np.where(r <= q, 0.0, NEG))
    g[:, 2] = t4(np.where(r > q, -1.0, 0.0))
    g[:, 3] = t4(np.where(r < q, -1.0, 0.0))
    g[:, 4] = t4(np.where(r <= q, 1.0, 0.0))
    g[:, 5] = t4(np.where(r >= q, 1.0, 0.0))
    g[:, 6] = 1.0
    g[:, 7] = -1.0
    g[:, 8] = t4(np.eye(64))
    c['gconst'] = g.reshape(64, 9 * 256)
    pos = np.arange(T_LAT)
    row = (pos // 64).astype(np.float64); col = (pos % 64).astype(np.float64)
    inv = 10000.0 ** (-np.arange(0, 32, 2, dtype=np.float64) / 32)
    ang = np.concatenate([row[:, None] * inv, col[:, None] * inv], axis=-1)
    c['rope_cs'] = np.concatenate([np.cos(ang), np.sin(ang)], axis=-1).astype(np.float32)
    mc = np.zeros((128, 256), np.float32)
    mc[:, 0:64] = np.arange(64)[None, :]
    mc[:, 64:100] = 128.0 * np.arange(36)[None, :]
    mc[:, 100:136] = np.arange(36)[None, :]
    mc[:, 136] = np.arange(128)
    mc[:, 137:201] = 1.0
    c['moe_const'] = mc
    tri = np.zeros((128, 256), np.float32)
    tri[:, 0:128] = (np.arange(128)[:, None] < np.arange(128)[None, :])
    tri[:, 128:256] = 1.0
    c['moe_tri'] = tri
    return c


class Kern(Builder):
    def declare_io(self):
        nc = self.nc
        self.inp = {}
        self.inp['xs_in'] = nc.dram_tensor('xs_in', [T, D], F32, kind='ExternalInput').ap()
        self.inp['cT_in'] = nc.dram_tensor('cT_in', [128, 16], F32, kind='ExternalInput').ap()
        for n in WEIGHT_NAMES + EXTRA_LAYOUT:
            if n in self.skip_inputs:
                continue
            shp = list(WEIGHT_SHAPES[n])
            if shp[0] == 4:
                shp[0] = self.n_layers
            self.inp[n] = nc.dram_tensor(n, shp, F32, kind='ExternalInput').ap()
        for n, v in host_consts().items():
            self.inp[n] = nc.dram_tensor(n, list(v.shape), F32, kind='ExternalInput').ap()
        self.out = nc.dram_tensor('out', [T_LAT, D], F32, kind='ExternalOutput').ap()
        self.XS = self.dram('XS', [T, D], nsub=NT)
        self.MODS = self.dram('MODS', [2, 6 * D])
        self.PF = self.dram('PF', [1280, T])
        self.PT = self.dram('PT', [T, PT_W], nsub=NT)
        self.MO = self.dram('MO', [D, T])
        self.XB = self.dram('XB', [N_SLOT, D])
        self.YB = self.dram('YB', [N_SLOT, D])
        self.dbg = {}
        for name, shape in self.debug.items():
            self.dbg[name] = nc.dram_tensor('dbg_' + name, list(shape), F32, kind='ExternalOutput').ap()

    def bcast_row(self, ap_row, n):
        return ap_row.partition_broadcast(128)

    def phase_init(self):
        for tt in range(NT):
            self.dma("sp", self.XS[tt * 128:(tt + 1) * 128, :], self.inp['xs_in'][tt * 128:(tt + 1) * 128, :],
                     writes=self.XS.rk(tt))
        self.ident = self.sb('ident', [128, 128])
        self.dma("sp", self.ident[:], self.inp['ident'][:, :], writes=[self.ident])
        cT = self.sb('cT', [128, 16])
        self.dma("sp", cT[:], self.inp['cT_in'][:, :], writes=[cT])
        self.scT = self.sb('scT', [128, 8, 2], F32R)
        self.act(self.scT[:, :, 0], cT[:, 0:8], AF.Silu, [cT], [self.scT])
        self.act(self.scT[:, :, 1], cT[:, 8:16], AF.Silu, [cT], [self.scT])

    def phase_ada(self, l):
        self.push()
        wa = [self.sb(f'wa{i}', [128, 8, 512], F32R) for i in range(2)]
        ba = self.sb('ba', [2, 6 * D])
        mods = self.sb('mods', [2, 6 * D])
        pp = [self.ps(f'pada{i}', [2, 512]) for i in range(2)]
        for r in range(2):
            self.dma("sp", ba[r:r + 1, :], self.inp['b_ada'][l:l + 1, :], writes=[ba])
        wsrc = self.inp['w_ada'][l].rearrange("(kc p) n -> p kc n", p=128)
        for cg in range(12):
            w = wa[cg % 2]
            self.dma("pool", w[:], wsrc[:, :, cg * 512:(cg + 1) * 512], writes=[w])
            p = pp[cg % 2]
            for kc in range(8):
                self.mm(p[:], self.scT[:, kc, :], w[:, kc, :], kc == 0, kc == 7, [self.scT, w], [p])
            self.tt("dve", mods[:, cg * 512:(cg + 1) * 512], p[:], ba[:, cg * 512:(cg + 1) * 512], ALU.add,
                    [p, ba], [mods])
        self.dma("sp", self.MODS[:, :], mods[:], reads=[mods], writes=[self.MODS])
        self.pop()

    def load_mod(self, dst, which, r, g_name=None, l=0):
        self.dma("sp", dst[:], self.MODS[r:r + 1, which * D:(which + 1) * D].partition_broadcast(128),
                 reads=[self.MODS], writes=[dst])
        if g_name is not None:
            gb = self.sb('gb', [128, D])
            self.dma("sp", gb[:], self.inp[g_name][l:l + 1, :].partition_broadcast(128), writes=[gb])
            self.stt(dst[:], dst[:], 1.0, gb[:], ALU.add, ALU.mult, [dst, gb], [dst])

    def evac(self, i, out, in_, reads, writes):
        return self.copy("act" if i % 2 == 0 else "dve", out, in_, reads, writes)

    def norm_tile(self, xt, gm, sh, xn, junk, ss):
        self.act(junk[:], xt[:], AF.Square, [xt], [junk, ss], accum_out=ss[:, 0:1])
        self.act(ss[:, 1:2], ss[:, 0:1], AF.Sqrt, [ss], [ss], scale=1.0 / D, bias=self.eps_t[:, 0:1])
        self.op("dve", lambda e: e.reciprocal(out=ss[:, 2:3], in_=ss[:, 1:2]), [ss], [ss])
        self.stt(xn[:], xt[:], ss[:, 2:3], gm[:], ALU.mult, ALU.mult, [xt, ss, gm], [xn])
        if sh is not None:
            self.tt("pool", xn[:], xn[:], sh[:], ALU.add, [xn, sh], [xn])

    def phase_proj(self, l):
        self.push()
        win = self.sb('win', [128, 8, DIN], F32R)
        wsrc = self.inp['w_in'][l].rearrange("(kc p) n -> p kc n", p=128)
        for a, b in ((0, 1160), (1160, 2320)):
            self.dma("pool", win[:, :, a:b], wsrc[:, :, a:b], writes=[win])
        gm = [self.sb(f'gm{r}', [128, D]) for r in range(2)]
        sh = [self.sb(f'sh{r}', [128, D]) for r in range(2)]
        self.push()
        for r in range(2):
            self.load_mod(gm[r], 1, r, 'norm1_g', l)
            self.load_mod(sh[r], 0, r)
        self.pop()
        xt = [self.sb(f'xt{i}', [128, D]) for i in range(2)]
        xn = [self.sb(f'xn{i}', [128, D]) for i in range(2)]
        junk = self.sb('junk', [128, D])
        ss = [self.sb(f'ss{i}', [128, 4]) for i in range(2)]
        xnT = self.sb('xnT', [128, 8, 512], F32R, nsub=4)
        sfm = [self.sb(f'sfm{i}', [128, 512]) for i in range(2)]
        stm = [self.sb(f'stm{i}', [128, PT_W]) for i in range(2)]
        ptp = [self.ps(f'ptp{i}', [128, 512]) for i in range(2)]
        pfm = [self.ps(f'pfm{i}', [128, 512]) for i in range(2)]
        ptm = [self.ps('ptm0', [128, 272]), self.ps('ptm1', [128, 512]), self.ps('ptm2', [128, 256])]
        fm_groups = [(c0, PF_GDN + c0) for c0 in range(0, 768, 128)] + \
                    [(C_LX + c0, PF_LRU + c0) for c0 in range(0, 512, 128)]
        tm_groups = [(C_GG, 272, PT_GATE), (C_AQ, 512, PT_ATT), (C_AQ + 512, 256, PT_ATT + 512)]
        blocks = [(0, 2), (2, 4), (6, 4), (10, 4), (14, 4)]
        it = 0
        ig = 0
        for (t0, ntile) in blocks:
            r = 1 if t0 == 0 else 0
            for j in range(ntile):
                tt = t0 + j
                x_ = xt[it % 2]; xn_ = xn[it % 2]; ss_ = ss[it % 2]
                self.dma("sp", x_[:], self.XS[tt * 128:(tt + 1) * 128, :], reads=self.XS.rk(tt), writes=[x_])
                self.norm_tile(x_, gm[r], sh[r], xn_, junk, ss_)
                for half in range(2):
                    p = ptp[half]
                    for q in range(4):
                        kc = half * 4 + q
                        self.tr(p[:, q * 128:(q + 1) * 128], xn_[:, kc * 128:(kc + 1) * 128], self.ident[:],
                                [xn_, self.ident], [p])
                    self.evac(half, xnT[:, half * 4:(half + 1) * 4, j * 128:(j + 1) * 128],
                              p[:].rearrange("p (q t) -> p q t", q=4), [p], xnT.rk(j))
                st = stm[it % 2]
                for gi, (c0, n, d0) in enumerate(tm_groups):
                    p = ptm[gi]
                    for kc in range(8):
                        self.mm(p[:, 0:n], xnT[:, kc, j * 128:(j + 1) * 128], win[:, kc, c0:c0 + n], kc == 0, kc == 7,
                                [xnT.rk(j), win], [p])
                    self.evac(gi, st[:, d0:d0 + n], p[:, 0:n], [p], [st])
                self.dma("sp", self.PT[tt * 128:(tt + 1) * 128, :], st[:], reads=[st], writes=self.PT.rk(tt))
                it += 1
            ntok = ntile * 128
            for (c0, d0) in fm_groups:
                p = pfm[ig % 2]; s_ = sfm[ig % 2]
                for kc in range(8):
                    self.mm(p[:, 0:ntok], win[:, kc, c0:c0 + 128], xnT[:, kc, 0:ntok], kc == 0, kc == 7,
                            [win, xnT.rk(*range(ntile))], [p])
                self.evac(ig, s_[:, 0:ntok], p[:, 0:ntok], [p], [s_])
                self.dma("sp", self.PF[d0:d0 + 128, t0 * 128:t0 * 128 + ntok], s_[:, 0:ntok], reads=[s_], writes=[self.PF])
                ig += 1
        self.pop()

    def build(self, stop_after=None):
        self.stop = stop_after
        self.declare_io()
        self.eps_t = self.sb('eps', [128, 1])
        self.memset("pool", self.eps_t[:], EPS, [self.eps_t])
        self.phase_init()
        for l in range(self.n_layers):
            if l > 0:
                self.S.renew()
            self.phase_ada(l)
            self.phase_proj(l)
            if stop_after == 'proj':
                break
            if 'nogdn' not in self.flags:
                self.phase_gdn(l)
            if stop_after is not None and stop_after.startswith('gdn'):
                break
            self.phase_lru(l)
            if stop_after == 'lru':
                break
            self.phase_att(l)
            if stop_after == 'att':
                break
            self.phase_wout(l)
            if stop_after == 'wout':
                break
            self.phase_moe(l)
            if stop_after == 'moe':
                break
        if stop_after is None:
            self.phase_final()
        evs = []
        for name, ap in self.dbg.items():
            src = {'PF': self.PF, 'PT': self.PT, 'MODS': self.MODS, 'MO': self.MO, 'XS': self.XS}[name]
            self.S.barrier()
            evs.append(self.dma("sp", ap[:, :], src[:, :], reads=[src]))
        for ev in evs:
            self.S.wait_event("sp", ev)
        self.S.barrier()
        return self.nc


def make_in_maps(inputs, n_layers=DEPTH, skip=()):
    consts = host_consts()
    maps = []
    for b in range(8):
        m = {}
        m['xs_in'] = np.ascontiguousarray(np.concatenate([inputs['ctx'][b], inputs['x'][b]], axis=0), dtype=np.float32)
        cT = np.concatenate([np.asarray(inputs['c'][b]).reshape(8, 128).T,
                             np.asarray(inputs['c_ctx']).reshape(8, 128).T], axis=1)
        m['cT_in'] = np.ascontiguousarray(cT, dtype=np.float32)
        for n in WEIGHT_NAMES:
            if n in skip:
                continue
            a = np.asarray(inputs[n], dtype=np.float32).reshape(WEIGHT_SHAPES[n])
            if WEIGHT_SHAPES[n][0] == 4:
                a = a[:n_layers]
            m[n] = np.ascontiguousarray(a)
        m['gdn_conv_wT'] = np.ascontiguousarray(np.transpose(np.asarray(inputs['gdn_conv_w'], dtype=np.float32), (0, 2, 1))[:n_layers])
        m['lru_conv_wT'] = np.ascontiguousarray(np.transpose(np.asarray(inputs['lru_conv_w'], dtype=np.float32), (0, 2, 1))[:n_layers])
        m.update(consts)
        maps.append(m)
    return maps


def kernel(**inputs):
    kb = Kern()
    nc = kb.build()
    res = run_bass_kernel_spmd(nc, make_in_maps(inputs), core_ids=list(range(8)))
    return np.stack([np.asarray(r['out']) for r in res.results], axis=0)


G_NML, G_NMU, G_SML, G_SMU, G_L, G_U, G_ONE, G_NEG1, G_ID = range(9)


def _gdn_methods():
    def gc(self, k, n=256):
        return self.gconst[:, k * 256:k * 256 + n]

    def phase_gdn(self, l):
        self.push()
        S = self.S
        gconst = self.sb('gconst', [64, 9 * 256])
        self.gconst = gconst
        self.dma("sp", gconst[:], self.inp['gconst'][:, :], writes=[gconst])
        one_t = self.sb('one', [128, 1])
        self.memset("pool", one_t[:], 1.0, [one_t])
        qn = self.sb('qn', [64, 4, T], nsub=NCH)
        kn = self.sb('kn', [64, 4, T], nsub=NCH)
        vT = self.sb('vT', [128, 2, T], nsub=NCH)
        moA = self.sb('moA', [128, 2, T])
        OF = self.dram(f'OF{l}', [T, 256], nsub=NCH)
        cwq = self.sb('cwq', [64, 8, 4])
        cwv = self.sb('cwv', [128, 2, 4])
        cw_src = self.inp['gdn_conv_wT'][l]
        self.dma("sp", cwq[:], cw_src[0:512, :].rearrange("(h p) m -> p h m", p=64), writes=[cwq])
        self.dma("sp", cwv[:], cw_src[512:768, :].rearrange("(h p) m -> p h m", p=128), writes=[cwv])
        chunks_all = list(range(NCH))

        self.push()
        raw = [self.sb(f'raw{i}', [128, 516]) for i in range(3)]
        lnb = self.sb('lnb', [64, 4, T])
        sqb = [self.sb(f'sqb{i}', [64, 512]) for i in range(2)]
        pss = [self.ps(f'pss{i}', [64, 512]) for i in range(2)]
        segs = [(0, T_CTX, [(0, 256)]), (T_CTX, T, [(T_CTX + i * 512, 512) for i in range(4)])]
        it = 0
        for kind in range(10):
            P = 64 if kind < 8 else 128
            row0 = kind * 64 if kind < 8 else 512 + (kind - 8) * 128
            cw = cwq[:, kind, :] if kind < 8 else cwv[:, kind - 8, :]
            cwt = cwq if kind < 8 else cwv
            for (s0, s1, blks) in segs:
                for (t0, blk) in blks:
                    rw = raw[it % 3]
                    lo = max(t0 - 1, s0); hi = min(t0 + blk + 2, s1)
                    if lo > t0 - 1:
                        self.memset("pool", rw[0:P, 0:1], 0.0, [rw])
                    if hi < t0 + blk + 2:
                        self.memset("pool", rw[0:P, hi - (t0 - 1):blk + 3], 0.0, [rw])
                    self.dma("sp", rw[0:P, lo - (t0 - 1):hi - (t0 - 1)], self.PF[row0:row0 + P, lo:hi],
                             reads=[self.PF], writes=[rw])
                    if kind >= 8:
                        dt_, dst = vT, vT[:, kind - 8, t0:t0 + blk]
                    elif kind < 4:
                        dt_, dst = qn, qn[:, kind, t0:t0 + blk]
                    else:
                        dt_, dst = kn, kn[:, kind - 4, t0:t0 + blk]
                    self.ts("dve", dst, rw[0:P, 0:blk], cw[:, 0:1], ALU.mult, [rw, cwt], [dt_])
                    for m in range(1, 4):
                        self.stt(dst, rw[0:P, m:m + blk], cw[:, m:m + 1], dst, ALU.mult, ALU.add, [rw, cwt, dt_], [dt_])
                    it += 1
        f2 = lambda t: t[:].rearrange("p h t -> p (h t)")
        for t_ in (qn, kn, vT):
            self.act(f2(t_), f2(t_), AF.Silu, [t_], [t_])
        blks_all = [(0, 256)] + [(T_CTX + i * 512, 512) for i in range(4)]
        it = 0
        for t_, sc in ((qn, 0.125), (kn, 1.0)):
            for h in range(4):
                for (t0, blk) in blks_all:
                    sq = sqb[it % 2]; ps_ = pss[it % 2]
                    self.tt("pool", sq[:, 0:blk], t_[:, h, t0:t0 + blk], t_[:, h, t0:t0 + blk], ALU.mult, [t_], [sq])
                    self.mm(ps_[:, 0:blk], self.gc(G_ONE, 64), sq[:, 0:blk], True, True, [gconst, sq], [ps_])
                    self.act(lnb[:, h, t0:t0 + blk], ps_[:, 0:blk], AF.Ln, [ps_], [lnb], bias=self.eps_t[0:64, 0:1])
                    it += 1
            self.act(f2(lnb), f2(lnb), AF.Exp, [lnb], [lnb], scale=-0.5)
            self.stt(f2(t_), f2(t_), sc, f2(lnb), ALU.mult, ALU.mult, [t_, lnb], [t_])
        self.pop()

        if self.stop == 'gdn_pre':
            self.dma("sp", self.MO[0:256, :].rearrange("(m p) t -> p m t", p=128), vT[:], reads=[vT], writes=[self.MO])
            self.dma("sp", self.MO[256:512, :].rearrange("(h p) t -> p h t", p=64), kn[:], reads=[kn], writes=[self.MO])
            self.dma("sp", self.MO[512:768, :].rearrange("(h p) t -> p h t", p=64), qn[:], reads=[qn], writes=[self.MO])
            self.pop()
            return
        abT = self.sb('abT', [64, NCH, 16])
        self.dma("sp", abT[:], self.PT[:, PT_AB:PT_AB + 16].rearrange("(c p) n -> p c n", p=64),
                 reads=[self.PT], writes=[abT])
        par = self.sb('par', [64, 16])
        self.dma("sp", par[:, 0:8], self.inp['gdn_a_log'][l:l + 1].rearrange("o d h -> o (d h)").partition_broadcast(64), writes=[par])
        self.dma("sp", par[:, 8:16], self.inp['gdn_dt_bias'][l:l + 1].rearrange("o d h -> o (d h)").partition_broadcast(64), writes=[par])
        negA = self.sb('negA', [64, 8])
        self.act(negA[:], par[:, 0:8], AF.Exp, [par], [negA])
        self.ts("dve", negA[:], negA[:], -1.0, ALU.mult, [negA], [negA])
        gall = self.sb('gall', [64, NCH, 8])
        beta = self.sb('beta', [64, NCH, 8])
        ball = self.sb('ball', [64, NCH, 8])
        eb = self.sb('eb', [64, NCH, 8])
        ebeta = self.sb('ebeta', [64, NCH, 8])
        ekd = self.sb('ekd', [64, NCH, 8])
        etot = self.sb('etot', [64, NCH, 8])
        tot = self.sb('tot', [64, NCH, 8])
        self.push()
        pb = [self.ps(f'pb{i}', [64, NCH * 8]) for i in range(3)]
        self.tt("dve", gall[:], abT[:, :, 0:8], par[:, 8:16].unsqueeze(1).to_broadcast([64, NCH, 8]), ALU.add, [abT, par], [gall])
        self.act(gall[:], gall[:], AF.Exp, [gall], [gall])
        self.act(gall[:], gall[:], AF.Ln, [gall], [gall], bias=one_t[0:64, 0:1])
        self.tt("dve", gall[:], gall[:], negA[:].unsqueeze(1).to_broadcast([64, NCH, 8]), ALU.mult, [gall, negA], [gall])
        self.act(beta[:], abT[:, :, 8:16], AF.Sigmoid, [abT], [beta])
        g2 = gall[:].rearrange("p c n -> p (c n)")
        self.mm(pb[0][:], self.gc(G_L, 64), g2, True, True, [gconst, gall], [pb[0]])
        self.mm(pb[1][:], self.gc(G_U, 64), g2, True, True, [gconst, gall], [pb[1]])
        self.mm(pb[2][:], self.gc(G_ONE, 64), g2, True, True, [gconst, gall], [pb[2]])
        v3 = lambda p: p[:].rearrange("p (c n) -> p c n", n=8)
        self.copy("dve", ball[:, :, 0:4], v3(pb[0])[:, :, 0:4], [pb[0]], [ball])
        self.copy("dve", ball[:, :, 4:8], v3(pb[1])[:, :, 4:8], [pb[1]], [ball])
        self.copy("dve", tot[:], v3(pb[2]), [pb[2]], [tot])
        self.act(eb[:], ball[:], AF.Exp, [ball], [eb])
        self.tt("dve", ebeta[:], eb[:], beta[:], ALU.mult, [eb, beta], [ebeta])
        self.tt("dve", ekd[:], tot[:], ball[:], ALU.subtract, [tot, ball], [ekd])
        self.act(ekd[:], ekd[:], AF.Exp, [ekd], [ekd])
        self.act(etot[:], tot[:], AF.Exp, [tot], [etot])
        self.pop()
        gng = self.sb('gng', [64, 4, 64])
        self.dma("sp", gng[:, 0, :], self.inp['gdn_norm_g'][l:l + 1, :].partition_broadcast(64), writes=[gng])
        for h in range(1, 4):
            self.copy("dve", gng[:, h, :], gng[:, 0, :], [gng], [gng])

        if self.stop == 'gdn_scal':
            for i_, t_ in enumerate((gall, beta, ball, eb, ebeta, ekd, etot, tot)):
                self.dma("sp", self.MO[i_ * 64:(i_ + 1) * 64, 0:NCH * 8], t_[:].rearrange("p c n -> p (c n)"), reads=[t_], writes=[self.MO])
            self.pop()
            return
        pA = self.ps('pA', [128, 512]); pB = self.ps('pB', [128, 512])
        pC = self.ps('pC', [128, 512]); pD = self.ps('pD', [128, 512])
        pE = self.ps('pE', [128, 512]); pF = self.ps('pF', [128, 512])
        pG = self.ps('pG', [128, 512]); pH = self.ps('pH', [128, 512])
        H0 = slice(0, 256); H1 = slice(256, 512)
        W = [64, 256]
        GLt = self.sb('GLt', W); Em = self.sb('Em', W); EmT = self.sb('EmT', W)
        tA = self.sb('tA', W)
        X = [self.sb(f'X{i}', W) for i in range(2)]
        XT = [self.sb(f'XT{i}', W) for i in range(2)]
        Pm = [self.sb(f'Pm{i}', W) for i in range(2)]
        vb = self.sb('vb', W); kbe = self.sb('kbe', W)
        u_ = [self.sb(f'u{i}', W) for i in range(2)]
        wT_ = [self.sb(f'wT{i}', W) for i in range(2)]
        KQm_ = [self.sb(f'KQm{i}', W) for i in range(2)]
        kd_ = [self.sb(f'kd{i}', W) for i in range(2)]
        Sst = [self.sb(f'S{i}', W) for i in range(2)]
        St = self.sb('St', W)
        vnew = self.sb('vnew', W)
        o2sb = self.sb('o2sb', W); osb = [self.sb(f'osb{i}', W) for i in range(2)]
        ofl = [self.sb(f'ofl{i}', W) for i in range(2)]
        gat = [self.sb(f'gat{i}', W) for i in range(2)]
        rs4 = self.sb('rs4', [64, 8]); ysb = self.sb('ysb', W); sqo = self.sb('sqo', W)

        def bc4(t, c, d):
            return t[:, c, d * 4:(d + 1) * 4].unsqueeze(2).to_broadcast([64, 4, 64])

        def v4(ap):
            return ap.rearrange("p (h n) -> p h n", h=4)

        def local(c, d, i):
            cs = slice(c * 64, (c + 1) * 64)
            NMa, NMb = (G_NML, G_NMU) if d == 0 else (G_NMU, G_NML)
            SM = G_SML if d == 0 else G_SMU
            GLm = G_L if d == 0 else G_U
            self.tt("dve", v4(GLt[:]), v4(self.gc(G_ID)), bc4(ball, c, d), ALU.mult, [gconst, ball], [GLt])
            for h in range(4):
                hs = slice(h * 64, (h + 1) * 64)
                self.mm(pA[0:64, hs], self.gc(G_ONE, 64), GLt[:, hs], True, True, [GLt, gconst], pA.rk(0))
            for h in range(4):
                hs = slice(h * 64, (h + 1) * 64)
                hs1 = slice(256 + h * 64, 256 + (h + 1) * 64)
                self.mm(pB[0:64, hs], kn[:, h, cs], kn[:, h, cs], True, True, kn.rk(c), pB.rk(0))
                self.mm(pB[0:64, hs1], kn[:, h, cs], qn[:, h, cs], True, True, [kn.rk(c), qn.rk(c)], pB.rk(1))
                self.mm(pE[0:64, hs], kn[:, h, cs], self.ident[0:64, 0:64], True, True, [kn.rk(c), self.ident], pE.rk(0))
            for m in range(2):
                self.mm(pE[0:64, 256 + m * 128:256 + (m + 1) * 128], vT[:, m, cs], self.ident[:, :], True, True, [vT.rk(c), self.ident], pE.rk(1))
            self.tt("dve", v4(Em[:]), bc4(ball, c, d), v4(pA[0:64, H0]), ALU.subtract, [pA.rk(0), ball], [Em])
            self.tt("pool", Em[:], Em[:], self.gc(NMa), ALU.add, [Em, gconst], [Em])
            self.act(Em[:], Em[:], AF.Exp, [Em], [Em])
            self.tt("dve", v4(EmT[:]), v4(pA[0:64, H0]), bc4(ball, c, d), ALU.subtract, [pA.rk(0), ball], [EmT])
            self.tt("pool", EmT[:], EmT[:], self.gc(NMb), ALU.add, [EmT, gconst], [EmT])
            self.act(EmT[:], EmT[:], AF.Exp, [EmT], [EmT])
            self.tt("dve", tA[:], pB[0:64, H0], self.gc(SM), ALU.mult, [pB.rk(0), gconst], [tA])
            self.tt("dve", v4(tA[:]), v4(tA[:]), bc4(beta, c, d), ALU.mult, [tA, beta], [tA])
            self.tt("pool", XT[0][:], tA[:], Em[:], ALU.mult, [tA, Em], [XT[0]])
            self.tt("dve", KQm_[i][:], pB[0:64, H1], EmT[:], ALU.mult, [pB.rk(1), EmT], [KQm_[i]])
            self.tt("dve", v4(vb[:]), v4(pE[0:64, H1]), bc4(beta, c, d), ALU.mult, [pE.rk(1), beta], [vb])
            self.tt("dve", v4(kbe[:]), v4(pE[0:64, H0]), bc4(ebeta, c, d), ALU.mult, [pE.rk(0), ebeta], [kbe])
            self.tt("dve", v4(kd_[i][:]), v4(pE[0:64, H0]), bc4(ekd, c, d), ALU.mult, [pE.rk(0), ekd], [kd_[i]])
            for h in range(4):
                hs = slice(h * 64, (h + 1) * 64)
                self.mm(pD[0:64, 256 + h * 64:256 + (h + 1) * 64], XT[0][:, hs], self.ident[0:64, 0:64], True, True, [XT[0], self.ident], pD.rk(1))
            self.copy("act", X[0][:], pD[0:64, H1], pD.rk(1), [X[0]])
            self.tt("pool", Pm[0][:], X[0][:], self.gc(G_ID), ALU.add, [X[0], gconst], [Pm[0]])
            cur = 0
            pc = 0

            def p_update(xt_tile, pc):
                for h in range(4):
                    hs = slice(h * 64, (h + 1) * 64)
                    self.mm(pD[0:64, hs], xt_tile[:, hs], Pm[pc][:, hs], True, True, [xt_tile, Pm[pc]], pD.rk(0))
                self.tt("dve", Pm[1 - pc][:], pD[0:64, H0], Pm[pc][:], ALU.add, [pD.rk(0), Pm[pc]], [Pm[1 - pc]])
                return 1 - pc

            for k in range(5):
                nxt = 1 - cur
                last = (k == 4)
                for h in range(4):
                    hs = slice(h * 64, (h + 1) * 64)
                    hs1 = slice(256 + h * 64, 256 + (h + 1) * 64)
                    if not last:
                        self.mm(pC[0:64, hs], XT[cur][:, hs], X[cur][:, hs], True, True, [XT[cur], X[cur]], pC.rk(0))
                    self.mm(pC[0:64, hs1], X[cur][:, hs], XT[cur][:, hs], True, True, [XT[cur], X[cur]], pC.rk(1))
                if k >= 1:
                    pc = p_update(XT[cur], pc)
                if not last:
                    self.copy("act", X[nxt][:], pC[0:64, H0], pC.rk(0), [X[nxt]])
                self.copy("dve", XT[nxt][:], pC[0:64, H1], pC.rk(1), [XT[nxt]])
                cur = nxt
            pc = p_update(XT[cur], pc)
            assert pc == 1
            TT = Pm[1]
            for h in range(4):
                hs = slice(h * 64, (h + 1) * 64)
                hs1 = slice(256 + h * 64, 256 + (h + 1) * 64)
                self.mm(pF[0:64, hs], TT[:, hs], vb[:, hs], True, True, [TT, vb], pF.rk(0))
                self.mm(pF[0:64, hs1], kbe[:, hs], TT[:, hs], True, True, [TT, kbe], pF.rk(1))
            self.copy("act", u_[i][:], pF[0:64, H0], pF.rk(0), [u_[i]])
            self.copy("dve", wT_[i][:], pF[0:64, H1], pF.rk(1), [wT_[i]])

        def seq(c, d, i, si, step):
            cs = slice(c * 64, (c + 1) * 64)
            Sc = Sst[si]; Sn = Sst[1 - si]
            for h in range(4):
                hs = slice(h * 64, (h + 1) * 64)
                self.mm(pG[0:64, hs], wT_[i][:, hs], Sc[:, hs], True, True, [wT_[i], Sc], pG.rk(0))
            self.tt("dve", vnew[:], u_[i][:], pG[0:64, H0], ALU.subtract, [u_[i], pG.rk(0)], [vnew])
            for h in range(4):
                hs = slice(h * 64, (h + 1) * 64)
                hs1 = slice(256 + h * 64, 256 + (h + 1) * 64)
                self.mm(pG[0:64, hs1], kd_[i][:, hs], vnew[:, hs], True, True, [kd_[i], vnew], pG.rk(1))
            self.tt("dve", v4(St[:]), v4(Sc[:]), bc4(etot, c, d), ALU.mult, [Sc, etot], [St])
            self.tt("dve", Sn[:], St[:], pG[0:64, H1], ALU.add, [St, pG.rk(1)], [Sn])
            for h in range(4):
                hs = slice(h * 64, (h + 1) * 64)
                hs1 = slice(256 + h * 64, 256 + (h + 1) * 64)
                self.mm(pH[0:64, hs], qn[:, h, cs], Sc[:, hs], True, True, [qn.rk(c), Sc], pH.rk(0))
                self.mm(pH[0:64, hs1], KQm_[i][:, hs], vnew[:, hs], True, True, [KQm_[i], vnew], pH.rk(1))
            ob = osb[step % 2]
            self.copy("act", o2sb[:], pH[0:64, H1], pH.rk(1), [o2sb])
            self.tt("dve", v4(ob[:]), v4(pH[0:64, H0]), bc4(eb, c, d), ALU.mult, [pH.rk(0), eb], [ob])
            self.tt("pool", ob[:], ob[:], o2sb[:], ALU.add, [ob, o2sb], [ob])
            if d == 0:
                self.dma("sp", OF[cs, :], ob[:], reads=[ob], writes=OF.rk(c))
            else:
                of = ofl[step % 2]; ga = gat[step % 2]
                self.dma("sp", of[:], OF[cs, :], reads=OF.rk(c), writes=[of])
                self.dma("sp", ga[:], self.PT[cs, PT_GATE:PT_GATE + 256], reads=[self.PT], writes=[ga])
                self.tt("pool", ob[:], ob[:], of[:], ALU.add, [ob, of], [ob])
                self.tt("pool", sqo[:], ob[:], ob[:], ALU.mult, [ob], [sqo])
                self.op("dve", lambda e: e.tensor_reduce(out=rs4[:, 0:4], in_=v4(sqo[:]), axis=AX.X, op=ALU.add), [sqo], [rs4])
                self.act(rs4[:, 4:8], rs4[:, 0:4], AF.Sqrt, [rs4], [rs4], scale=1.0 / 64, bias=self.eps_t[0:64, 0:1])
                self.op("dve", lambda e: e.reciprocal(out=rs4[:, 0:4], in_=rs4[:, 4:8]), [rs4], [rs4])
                self.tt("dve", v4(ysb[:]), v4(ob[:]), rs4[:, 0:4].unsqueeze(2).to_broadcast([64, 4, 64]), ALU.mult, [ob, rs4], [ysb])
                self.tt("pool", ysb[:], ysb[:], gng[:].rearrange("p h n -> p (h n)"), ALU.mult, [ysb, gng], [ysb])
                self.act(ga[:], ga[:], AF.Silu, [ga], [ga])
                self.tt("dve", ysb[:], ysb[:], ga[:], ALU.mult, [ysb, ga], [ysb])
                for m in range(2):
                    self.mm(pE[:, m * 64:(m + 1) * 64], ysb[:, m * 128:(m + 1) * 128], self.ident[0:64, 0:64], True, True, [ysb, self.ident], pE.rk(0))
                self.copy("act", moA[:, :, cs], pE[:, 0:128].rearrange("p (m t) -> p m t", m=2), pE.rk(0), [moA])

        for d in range(2):
            order = list(range(NCH)) if d == 0 else [3, 2, 1, 0] + list(range(NCH - 1, 3, -1))
            self.memset("pool", Sst[0][:], 0.0, [Sst[0]])
            if self.stop == 'gdn_local1':
                import os
                self.op_limit = int(os.environ.get('OPLIM', '100000'))
                self.op_cnt = 0
            local(order[0], d, 0)
            self.op_limit = None
            if self.stop == 'gdn_local1':
                for i_, t_ in enumerate((Pm[1], u_[0], wT_[0], KQm_[0], kd_[0], Em, EmT, XT[0], vb, kbe)):
                    self.dma("sp", self.MO[i_ * 64:(i_ + 1) * 64, 0:256], t_[:], reads=[t_], writes=[self.MO])
                self.pop()
                return
            for step, c in enumerate(order):
                if self.stop == 'gdn_seq1' and step == 1:
                    for i_, t_ in enumerate((Sst[1], vnew, osb[0])):
                        self.dma("sp", self.MO[i_ * 64:(i_ + 1) * 64, 0:256], t_[:], reads=[t_], writes=[self.MO])
                    self.pop()
                    return
                if step + 1 < len(order):
                    local(order[step + 1], d, (step + 1) % 2)
                seq(c, d, step % 2, step % 2, step)
        self.dma("sp", self.MO[0:256, :].rearrange("(m p) t -> p m t", p=128), moA[:], reads=[moA], writes=[self.MO])
        self.pop()

    return dict(gc=gc, phase_gdn=phase_gdn)


for _k, _v in _gdn_methods().items():
    setattr(Kern, _k, _v)


def _lru_att_methods():
    def rev_ap(self, ap2d):
        n = ap2d.shape[1]
        last = ap2d[:, n - 1:n]
        return bass.AP(last.tensor, last.offset, [list(last.ap[0]), [-1, n]])

    def phase_lru(self, l):
        self.push()
        one_t = self.sb('one', [128, 1])
        self.memset("pool", one_t[:], 1.0, [one_t])
        xr = self.sb('xr', [128, 2, T])
        yg = self.sb('yg', [128, 2, T])
        A = self.sb('A', [128, 2, T])
        BX = self.sb('BX', [128, 2, T])
        H = [self.sb(f'H{i}', [128, 2, T]) for i in range(2)]
        cw = self.sb('cwl', [128, 2, 4])
        cb = self.sb('cbl', [128, 2])
        self.dma("sp", cw[:], self.inp['lru_conv_wT'][l].rearrange("(m p) k -> p m k", p=128), writes=[cw])
        for m in range(2):
            self.dma("sp", cb[:, m:m + 1], self.inp['lru_conv_b'][l, m * 128:(m + 1) * 128].rearrange("(p o) -> p o", o=1), writes=[cb])
        WB = self.sb('WB', [128, 8, 128])
        self.memset("pool", WB[:], 0.0, [WB])
        bri = self.sb('bri', [128, 8])
        lam = self.sb('lam', [128, 4])
        for d in range(2):
            for gi, (wn, bn) in enumerate((('lru_w_r', 'lru_b_r'), ('lru_w_i', 'lru_b_i'))):
                for m in range(2):
                    idx = (d * 2 + gi) * 2 + m
                    for hb in range(2):
                        self.dma("sp", WB[hb * 64:(hb + 1) * 64, idx, hb * 64:(hb + 1) * 64],
                                 self.inp[wn][l, d, 2 * m + hb], writes=[WB])
                    self.dma("sp", bri[:, idx:idx + 1],
                             self.inp[bn][l, d, m * 128:(m + 1) * 128].rearrange("(p o) -> p o", o=1), writes=[bri])
            for m in range(2):
                self.dma("sp", lam[:, d * 2 + m:d * 2 + m + 1],
                         self.inp['lru_lambda'][l, d, m * 128:(m + 1) * 128].rearrange("(p o) -> p o", o=1), writes=[lam])
        cch = self.sb('cch', [128, 4])
        self.act(cch[:], lam[:], AF.Exp, [lam], [cch], scale=-1.0)
        self.act(cch[:], cch[:], AF.Ln, [cch], [cch], bias=one_t[:, 0:1])
        self.ts("dve", cch[:], cch[:], -8.0, ALU.mult, [cch], [cch])
        self.push()
        raw = [self.sb(f'raw{i}', [128, 516]) for i in range(2)]
        t1 = self.sb('t1', [128, 512]); t2 = self.sb('t2', [128, 512])
        segs = [(0, T_CTX, [(0, 256)]), (T_CTX, T, [(T_CTX + i * 512, 512) for i in range(4)])]
        it = 0
        for m in range(2):
            row0 = PF_LRU + m * 128
            for (s0, s1, blks) in segs:
                for (t0, blk) in blks:
                    rw = raw[it % 2]
                    lo = max(t0 - 1, s0); hi = min(t0 + blk + 2, s1)
                    if lo > t0 - 1:
                        self.memset("pool", rw[:, 0:1], 0.0, [rw])
                    if hi < t0 + blk + 2:
                        self.memset("pool", rw[:, hi - (t0 - 1):blk + 3], 0.0, [rw])
                    self.dma("sp", rw[:, lo - (t0 - 1):hi - (t0 - 1)], self.PF[row0:row0 + 128, lo:hi], reads=[self.PF], writes=[rw])
                    dst = xr[:, m, t0:t0 + blk]
                    self.ts("dve", dst, rw[:, 0:blk], cw[:, m, 0:1], ALU.mult, [rw, cw, cb], [xr], s2=cb[:, m:m + 1], op1=ALU.add)
                    for k in range(1, 4):
                        self.stt(dst, rw[:, k:k + blk], cw[:, m, k:k + 1], dst, ALU.mult, ALU.add, [rw, cw, xr], [xr])
                    it += 1
                    rg = raw[it % 2]
                    self.dma("sp", rg[:, 0:blk], self.PF[row0 + 256:row0 + 384, t0:t0 + blk], reads=[self.PF], writes=[rg])
                    self.tt("pool", t1[:, 0:blk], rg[:, 0:blk], rg[:, 0:blk], ALU.mult, [rg], [t1])
                    self.ts("dve", t1[:, 0:blk], t1[:, 0:blk], 0.044715, ALU.mult, [t1], [t1], s2=1.0, op1=ALU.add)
                    self.tt("pool", t1[:, 0:blk], t1[:, 0:blk], rg[:, 0:blk], ALU.mult, [t1, rg], [t1])
                    self.act(t1[:, 0:blk], t1[:, 0:blk], AF.Tanh, [t1], [t1], scale=0.7978845608028654)
                    self.ts("dve", t2[:, 0:blk], rg[:, 0:blk], 0.5, ALU.mult, [rg], [t2])
                    self.stt(yg[:, m, t0:t0 + blk], t1[:, 0:blk], 1.0, t2[:, 0:blk], ALU.add, ALU.mult, [t1, t2], [yg])
                    it += 1
        self.pop()
        pr = [self.ps(f'pr{i}', [128, 512]) for i in range(2)]
        pi = [self.ps(f'pi{i}', [128, 512]) for i in range(2)]
        rt = self.sb('rt', [128, 512]); itl = self.sb('itl', [128, 512]); mt = self.sb('mt', [128, 512])
        blocks = [(0, 256)] + [(T_CTX + i * 512, 512) for i in range(4)]
        it = 0
        for d in range(2):
            for m in range(2):
                for (t0, blk) in blocks:
                    p1 = pr[it % 2]; p2 = pi[it % 2]
                    src = xr[:, m, t0:t0 + blk]
                    ir = (d * 2 + 0) * 2 + m; ii = (d * 2 + 1) * 2 + m
                    self.mm(p1[:, 0:blk], WB[:, ir, :], src, True, True, [WB, xr], [p1])
                    self.mm(p2[:, 0:blk], WB[:, ii, :], src, True, True, [WB, xr], [p2])
                    self.act(rt[:, 0:blk], p1[:, 0:blk], AF.Sigmoid, [p1, bri], [rt], bias=bri[:, ir:ir + 1])
                    self.act(itl[:, 0:blk], p2[:, 0:blk], AF.Sigmoid, [p2, bri], [itl], bias=bri[:, ii:ii + 1])
                    a_ = A[:, m, t0:t0 + blk]
                    self.act(a_, rt[:, 0:blk], AF.Exp, [rt, cch], [A], scale=cch[:, d * 2 + m:d * 2 + m + 1])
                    self.tt("pool", mt[:, 0:blk], a_, a_, ALU.mult, [A], [mt])
                    self.ts("dve", mt[:, 0:blk], mt[:, 0:blk], -1.0, ALU.mult, [mt], [mt], s2=1.0, op1=ALU.add)
                    self.act(mt[:, 0:blk], mt[:, 0:blk], AF.Sqrt, [mt], [mt])
                    self.tt("dve", mt[:, 0:blk], mt[:, 0:blk], itl[:, 0:blk], ALU.mult, [mt, itl], [mt])
                    self.tt("pool", BX[:, m, t0:t0 + blk], mt[:, 0:blk], src, ALU.mult, [mt, xr], [BX])
                    it += 1
                if d == 0:
                    self.op("dve", lambda e: e.tensor_tensor_scan(out=H[0][:, m, :], data0=A[:, m, :], data1=BX[:, m, :],
                                                                   initial=0.0, op0=ALU.mult, op1=ALU.add), [A, BX], [H[0]])
                else:
                    rv = self.rev_ap
                    self.op("dve", lambda e: e.tensor_tensor_scan(out=rv(H[1][:, m, 0:T_CTX]), data0=rv(A[:, m, 0:T_CTX]),
                                                                   data1=rv(BX[:, m, 0:T_CTX]), initial=0.0,
                                                                   op0=ALU.mult, op1=ALU.add), [A, BX], [H[1]])
                    self.op("dve", lambda e: e.tensor_tensor_scan(out=rv(H[1][:, m, T_CTX:T]), data0=rv(A[:, m, T_CTX:T]),
                                                                   data1=rv(BX[:, m, T_CTX:T]), initial=H[1][:, m, 0:1],
                                                                   op0=ALU.mult, op1=ALU.add), [A, BX, H[1]], [H[1]])
        for m in range(2):
            self.tt("pool", H[0][:, m, :], H[0][:, m, :], H[1][:, m, :], ALU.add, [H[0], H[1]], [H[0]])
            self.tt("dve", H[0][:, m, :], H[0][:, m, :], yg[:, m, :], ALU.mult, [H[0], yg], [H[0]])
        self.dma("sp", self.MO[256:512, :].rearrange("(m p) t -> p m t", p=128), H[0][:], reads=[H[0]], writes=[self.MO])
        self.pop()

    def phase_att(self, l):
        self.push()
        qT = self.sb('qT', [64, 8, T], F32R)
        kT = self.sb('kT', [64, 2, T], F32R)
        V = self.sb('V', [128, NT, 2, 65], F32R)
        onesf = self.sb('onesf', [128, NT * 2])
        self.memset("pool", onesf[:], 1.0, [onesf])
        self.copy("dve", V[:, :, :, 64], onesf[:].rearrange("p (t g) -> p t g", g=2), [onesf], [V])
        gbc = self.sb('gbc', [128, 10, 64])
        self.dma("sp", gbc[:, 0, :], self.inp['attn_q_norm_g'][l:l + 1, :].partition_broadcast(128), writes=[gbc])
        self.dma("sp", gbc[:, 8, :], self.inp['attn_k_norm_g'][l:l + 1, :].partition_broadcast(128), writes=[gbc])
        negc = self.sb('negc', [128, 4])
        self.op("dve", lambda e: e.tensor_reduce(out=negc[:, 0:1], in_=gbc[:, 0, :], axis=AX.X, op=ALU.max, apply_absolute_value=True), [gbc], [negc])
        self.op("dve", lambda e: e.tensor_reduce(out=negc[:, 1:2], in_=gbc[:, 8, :], axis=AX.X, op=ALU.max, apply_absolute_value=True), [gbc], [negc])
        self.tt("dve", negc[:, 2:3], negc[:, 0:1], negc[:, 1:2], ALU.mult, [negc], [negc])
        self.ts("dve", negc[:, 3:4], negc[:, 2:3], -8.0, ALU.mult, [negc], [negc])
        self.ts("dve", gbc[:, 0, :], gbc[:, 0, :], 0.125, ALU.mult, [gbc], [gbc])
        for h in range(1, 8):
            self.copy("dve", gbc[:, h, :], gbc[:, 0, :], [gbc], [gbc])
        self.copy("dve", gbc[:, 9, :], gbc[:, 8, :], [gbc], [gbc])
        self.push()
        at = [self.sb(f'at{i}', [128, 768]) for i in range(2)]
        an = [self.sb(f'an{i}', [128, 640]) for i in range(2)]
        sqa = self.sb('sqa', [128, 640])
        ssa = self.sb('ssa', [128, 20])
        cs_t = [self.sb(f'cs{i}', [128, 64]) for i in range(2)]
        r1 = self.sb('r1', [128, 10, 2, 16]); r2 = self.sb('r2', [128, 10, 2, 16])
        r3 = self.sb('r3', [128, 10, 2, 16]); r4 = self.sb('r4', [128, 10, 2, 16])
        ptq = [self.ps(f'ptq{i}', [128, 512]) for i in range(3)]
        for tt in range(NT):
            a_ = at[tt % 2]; n_ = an[tt % 2]
            self.dma("sp", a_[:], self.PT[tt * 128:(tt + 1) * 128, PT_ATT:PT_ATT + 768], reads=self.PT.rk(tt), writes=[a_])
            self.tt("pool", sqa[:], a_[:, 0:640], a_[:, 0:640], ALU.mult, [a_], [sqa])
            self.op("dve", lambda e: e.tensor_reduce(out=ssa[:, 0:10], in_=sqa[:].rearrange("p (h n) -> p h n", h=10), axis=AX.X, op=ALU.add), [sqa], [ssa])
            self.act(ssa[:, 10:20], ssa[:, 0:10], AF.Sqrt, [ssa], [ssa], scale=1.0 / 64, bias=self.eps_t[:, 0:1])
            self.op("dve", lambda e: e.reciprocal(out=ssa[:, 0:10], in_=ssa[:, 10:20]), [ssa], [ssa])
            n3 = n_[:].rearrange("p (h n) -> p h n", h=10)
            self.tt("dve", n3, a_[:, 0:640].rearrange("p (h n) -> p h n", h=10), ssa[:, 0:10].unsqueeze(2).to_broadcast([128, 10, 64]), ALU.mult, [a_, ssa], [n_])
            self.tt("pool", n_[:], n_[:], gbc[:].rearrange("p h n -> p (h n)"), ALU.mult, [n_, gbc], [n_])
            if tt >= 2:
                c_ = cs_t[tt % 2]
                lt = tt - 2
                self.dma("sp", c_[:], self.inp['rope_cs'][lt * 128:(lt + 1) * 128, :], writes=[c_])
                n5 = n_[:].rearrange("p (h a f n) -> p h a f n", h=10, a=2, f=2)
                x1 = n5[:, :, :, 0, :]; x2 = n5[:, :, :, 1, :]
                cosb = c_[:, 0:32].rearrange("p (a n) -> p a n", a=2).unsqueeze(1).to_broadcast([128, 10, 2, 16])
                sinb = c_[:, 32:64].rearrange("p (a n) -> p a n", a=2).unsqueeze(1).to_broadcast([128, 10, 2, 16])
                self.tt("dve", r1[:], x1, cosb, ALU.mult, [n_, c_], [r1])
                self.tt("pool", r2[:], x2, sinb, ALU.mult, [n_, c_], [r2])
                self.tt("dve", r3[:], x1, sinb, ALU.mult, [n_, c_], [r3])
                self.tt("pool", r4[:], x2, cosb, ALU.mult, [n_, c_], [r4])
                self.tt("dve", x1, r1[:], r2[:], ALU.subtract, [r1, r2], [n_])
                self.tt("pool", x2, r3[:], r4[:], ALU.add, [r3, r4], [n_])
            ts_ = slice(tt * 128, (tt + 1) * 128)
            for grp in range(3):
                p = ptq[grp]
                hs = range(4) if grp < 2 else range(2)
                for j in hs:
                    h = grp * 4 + j
                    self.mm(p[0:64, j * 128:(j + 1) * 128], n_[:, h * 64:(h + 1) * 64], self.ident[:, :], True, True, [n_, self.ident], [p])
                if grp < 2:
                    self.evac(grp, qT[:, grp * 4:(grp + 1) * 4, ts_], p[0:64, :].rearrange("p (h t) -> p h t", h=4), [p], [qT])
                else:
                    self.evac(grp, kT[:, :, ts_], p[0:64, 0:256].rearrange("p (h t) -> p h t", h=2), [p], [kT])
            self.copy("dve", V[:, tt, :, 0:64], a_[:, 640:768].rearrange("p (g n) -> p g n", g=2), [a_], [V])
        self.pop()
        pS = [self.ps(f'pS{i}', [128, 512]) for i in range(2)]
        pO = [self.ps(f'pO{i}', [128, 512]) for i in range(2)]
        pBc = self.ps('pBc', [128, 512])
        ones64 = self.sb('ones64', [128, 64])
        self.memset("pool", ones64[:], 1.0, [ones64])
        Pt = [self.sb(f'Pt{i}', [128, 512], F32R) for i in range(3)]
        rsb = self.sb('rsb', [128, 512]); bcs = self.sb('bcs', [64, 512])
        osg = [self.sb(f'osg{i}', [64, 512]) for i in range(2)]
        qblocks = [(0, 256, [0, 1])] + [(T_CTX + i * 512, 512, list(range(NT))) for i in range(4)]
        ip = 0; io = 0
        for (q0, qn_, ktiles) in qblocks:
            for h in range(8):
                g = h // 4
                po = pO[io % 2]
                pend = None
                nk = len(ktiles)
                for ki, kt in enumerate(ktiles):
                    ps_ = pS[ip % 2]; pt = Pt[ip % 3]
                    self.mm(ps_[:, 0:qn_], kT[:, g, kt * 128:(kt + 1) * 128], qT[:, h, q0:q0 + qn_], True, True, [kT, qT], [ps_])
                    self.act(pt[:, 0:qn_], ps_[:, 0:qn_], AF.Exp, [ps_, negc], [pt], bias=negc[:, 3:4])
                    if pend is not None:
                        pkt, ppt, pki = pend
                        self.mm(po[0:65, 0:qn_], V[:, pkt, g, :], ppt[:, 0:qn_], pki == 0, pki == nk - 1, [V, ppt], [po])
                    pend = (kt, pt, ki)
                    ip += 1
                pkt, ppt, pki = pend
                self.mm(po[0:65, 0:qn_], V[:, pkt, g, :], ppt[:, 0:qn_], pki == 0, pki == nk - 1, [V, ppt], [po])
                self.op("dve", lambda e: e.reciprocal(out=rsb[64:65, 0:qn_], in_=po[64:65, 0:qn_]), [po], [rsb])
                self.mm(pBc[0:64, 0:qn_], onesf[64:65, 0:64 - 28] if False else ones64[64:65, 0:64], rsb[64:65, 0:qn_], True, True, [ones64, rsb], [pBc])
                self.copy("act", bcs[:, 0:qn_], pBc[0:64, 0:qn_], [pBc], [bcs])
                og = osg[io % 2]
                self.tt("dve", og[:, 0:qn_], po[0:64, 0:qn_], bcs[:, 0:qn_], ALU.mult, [po, bcs], [og])
                self.dma("sp", self.MO[512 + h * 64:512 + (h + 1) * 64, q0:q0 + qn_], og[:, 0:qn_], reads=[og], writes=[self.MO])
                io += 1
        self.pop()

    return dict(rev_ap=rev_ap, phase_lru=phase_lru, phase_att=phase_att)


for _k, _v in _lru_att_methods().items():
    setattr(Kern, _k, _v)


N_OV = 36
N_SLOT = (64 + N_OV) * 128


def _tail_methods():
    def phase_wout(self, l):
        self.push()
        wo = self.sb('wo', [128, 8, D], F32R)
        self.dma("pool", wo[:], self.inp['w_out'][l].rearrange("(kc p) n -> p kc n", p=128), writes=[wo])
        g1 = [self.sb(f'g1{r}', [128, D]) for r in range(2)]
        for r in range(2):
            self.load_mod(g1[r], 2, r)
        mo = [self.sb(f'mo{i}', [128, 8, 512], F32R) for i in range(2)]
        xt = [self.sb(f'xw{i}', [128, D]) for i in range(2)]
        tw = [self.sb(f'tw{i}', [128, D]) for i in range(2)]
        pw = [self.ps(f'pw{i}', [128, 512]) for i in range(4)]
        blocks = [(0, 2), (2, 4), (6, 4), (10, 4), (14, 4)]
        msrc = self.MO[:, :].rearrange("(kc p) t -> p kc t", p=128)
        it = 0
        for bi, (t0, ntile) in enumerate(blocks):
            r = 1 if t0 == 0 else 0
            m_ = mo[bi % 2]
            ntok = ntile * 128
            self.dma("pool", m_[:, :, 0:ntok], msrc[:, :, t0 * 128:t0 * 128 + ntok], reads=[self.MO], writes=[m_])
            for j in range(ntile):
                tt = t0 + j
                x_ = xt[it % 2]; t_ = tw[it % 2]
                self.dma("sp", x_[:], self.XS[tt * 128:(tt + 1) * 128, :], reads=self.XS.rk(tt), writes=[x_])
                for half in range(2):
                    p = pw[(it % 2) * 2 + half]
                    for kc in range(8):
                        self.mm(p[:], m_[:, kc, j * 128:(j + 1) * 128], wo[:, kc, half * 512:(half + 1) * 512], kc == 0, kc == 7, [m_, wo], [p])
                    hs = slice(half * 512, (half + 1) * 512)
                    self.tt("dve", t_[:, hs], p[:], g1[r][:, hs], ALU.mult, [p, g1[r]], [t_])
                self.tt("pool", x_[:], x_[:], t_[:], ALU.add, [x_, t_], [x_])
                self.dma("sp", self.XS[tt * 128:(tt + 1) * 128, :], x_[:], reads=[x_], writes=self.XS.rk(tt))
                it += 1
        self.pop()

    def phase_moe(self, l):
        self.push()
        mc = self.sb('mc', [128, 256])
        self.dma("sp", mc[:], self.inp['moe_const'][:, :], writes=[mc])
        iota64 = mc[:, 0:64]; thr = mc[:, 64:100]; bvals = mc[:, 100:136]; pidx = mc[:, 136:137]; ones64 = mc[:, 137:201]
        ustr = self.sb('ustr', [128, 256])
        self.dma("sp", ustr[:], self.inp['moe_tri'][:, :], writes=[ustr])
        OH = self.sb('OH', [128, NT, 2, 64])
        gates = self.sb('gates', [128, NT, 2])
        dest = self.sb('dest', [128, NT, 2], I32)
        wix = self.sb('wix', [128, 2, N_OV], I32)
        XB, YB = self.XB, self.YB
        self.push()
        gm = [self.sb(f'gm2{r}', [128, D]) for r in range(2)]
        sh = [self.sb(f'sh2{r}', [128, D]) for r in range(2)]
        self.push()
        for r in range(2):
            self.load_mod(gm[r], 4, r, 'norm2_g', l)
            self.load_mod(sh[r], 3, r)
        self.pop()
        hbuf = self.sb('hbuf', [128, NT, D], nsub=NT)
        wr = self.sb('wr', [128, 8, 72])
        self.dma("sp", wr[:, :, 0:8], self.inp['moe_w_group'][l].rearrange("(kc p) n -> p kc n", p=128), writes=[wr])
        self.dma("sp", wr[:, :, 8:72], self.inp['moe_w_expert'][l].rearrange("(kc p) n -> p kc n", p=128), writes=[wr])
        rb = self.sb('rb', [128, 72])
        self.dma("sp", rb[:, 0:8], self.inp['moe_b_group'][l:l + 1, :].partition_broadcast(128), writes=[rb])
        self.dma("sp", rb[:, 8:72], self.inp['moe_b_expert'][l:l + 1, :].partition_broadcast(128), writes=[rb])
        xt = [self.sb(f'xm{i}', [128, D]) for i in range(2)]
        junk = self.sb('junk', [128, D])
        ss = [self.sb(f'ssm{i}', [128, 4]) for i in range(2)]
        hT = self.sb('hT', [128, 8, 128])
        ptp = [self.ps(f'ptp{i}', [128, 512]) for i in range(2)]
        plg = self.ps('plg', [128, 72])
        prk = self.ps('prk', [128, 64]); pcs = self.ps('pcs', [128, 64])
        lg = self.sb('lg', [128, 72]); sm = self.sb('sm', [128, 16])
        ohg = self.sb('ohg', [128, 8]); tmp64 = self.sb('tmp64', [128, 64]); le = self.sb('le', [128, 8]); le2 = self.sb('le2', [128, 8])
        oh1 = self.sb('oh1', [128, 8]); oh2 = self.sb('oh2', [128, 8]); eg = self.sb('eg', [128, 8])
        for tt in range(NT):
            r = 1 if tt < 2 else 0
            x_ = xt[tt % 2]; ss_ = ss[tt % 2]
            self.dma("sp", x_[:], self.XS[tt * 128:(tt + 1) * 128, :], reads=self.XS.rk(tt), writes=[x_])
            hcur = Tl(hbuf[:, tt, :], 'hcur'); hcur.rs = hbuf.rk(tt)
            self.norm_tile(x_, gm[r], sh[r], hcur, junk, ss_)
            for half in range(2):
                p = ptp[half]
                for q in range(4):
                    kc = half * 4 + q
                    self.tr(p[:, q * 128:(q + 1) * 128], hbuf[:, tt, kc * 128:(kc + 1) * 128], self.ident[:], [hbuf.rk(tt), self.ident], [p])
                self.evac(half, hT[:, half * 4:(half + 1) * 4, :], p[:].rearrange("p (q t) -> p q t", q=4), [p], [hT])
            for kc in range(8):
                self.mm(plg[:], hT[:, kc, :], wr[:, kc, :], kc == 0, kc == 7, [hT, wr], [plg])
            self.tt("dve", lg[:], plg[:], rb[:], ALU.add, [plg, rb], [lg])
            self.op("dve", lambda e: e.tensor_reduce(out=sm[:, 0:1], in_=lg[:, 0:8], axis=AX.X, op=ALU.max), [lg], [sm])
            self.ts("dve", ohg[:], lg[:, 0:8], sm[:, 0:1], ALU.is_equal, [lg, sm], [ohg])
            self.ts("dve", sm[:, 1:2], sm[:, 0:1], -1.0, ALU.mult, [sm], [sm])
            self.act(eg[:], lg[:, 0:8], AF.Exp, [lg, sm], [eg, sm], bias=sm[:, 1:2], accum_out=sm[:, 2:3])
            self.op("dve", lambda e: e.reciprocal(out=sm[:, 3:4], in_=sm[:, 2:3]), [sm], [sm])
            self.tt("dve", tmp64[:].rearrange("p (g e) -> p g e", g=8), lg[:, 8:72].rearrange("p (g e) -> p g e", g=8),
                    ohg[:].unsqueeze(2).to_broadcast([128, 8, 8]), ALU.mult, [lg, ohg], [tmp64])
            self.op("dve", lambda e: e.tensor_reduce(out=le[:], in_=tmp64[:].rearrange("p (g e) -> p e g", g=8), axis=AX.X, op=ALU.add), [tmp64], [le])
            self.op("dve", lambda e: e.tensor_reduce(out=sm[:, 4:5], in_=le[:], axis=AX.X, op=ALU.max), [le], [sm])
            self.ts("dve", oh1[:], le[:], sm[:, 4:5], ALU.is_equal, [le, sm], [oh1])
            self.stt(le2[:], oh1[:], -1.0e30, le[:], ALU.mult, ALU.add, [oh1, le], [le2])
            self.op("dve", lambda e: e.tensor_reduce(out=sm[:, 5:6], in_=le2[:], axis=AX.X, op=ALU.max), [le2], [sm])
            self.ts("dve", oh2[:], le2[:], sm[:, 5:6], ALU.is_equal, [le2, sm], [oh2])
            self.tt("dve", sm[:, 6:7], sm[:, 5:6], sm[:, 4:5], ALU.subtract, [sm], [sm])
            self.act(sm[:, 7:8], sm[:, 6:7], AF.Exp, [sm], [sm])
            self.ts("dve", sm[:, 8:9], sm[:, 7:8], 1.0, ALU.add, [sm], [sm])
            self.op("dve", lambda e: e.reciprocal(out=sm[:, 9:10], in_=sm[:, 8:9]), [sm], [sm])
            self.tt("dve", gates[:, tt, 0:1], sm[:, 3:4], sm[:, 9:10], ALU.mult, [sm], [gates])
            self.tt("dve", gates[:, tt, 1:2], gates[:, tt, 0:1], sm[:, 7:8], ALU.mult, [sm, gates], [gates])
            for k, ohk in enumerate((oh1, oh2)):
                self.tt("dve", OH[:, tt, k, :].rearrange("p (g e) -> p g e", g=8), ohg[:].unsqueeze(2).to_broadcast([128, 8, 8]),
                        ohk[:].unsqueeze(1).to_broadcast([128, 8, 8]), ALU.mult, [ohg, ohk], [OH])
        Osum = self.sb('Osum', [128, NT, 64])
        self.tt("dve", Osum[:], OH[:, :, 0, :], OH[:, :, 1, :], ALU.add, [OH], [Osum])
        rank = self.sb('rank', [128, NT, 64])
        pref = self.sb('pref', [128, 64])
        self.memset("pool", pref[:], 0.0, [pref])
        for tt in range(NT):
            self.mm(prk[:], ustr[:, 0:128], Osum[:, tt, :], True, True, [ustr, Osum], [prk])
            self.mm(pcs[:], ustr[:, 128:256], Osum[:, tt, :], True, True, [ustr, Osum], [pcs])
            self.tt("dve", rank[:, tt, :], prk[:], pref[:], ALU.add, [prk, pref], [rank])
            self.tt("dve", pref[:], pcs[:], pref[:], ALU.add, [pcs, pref], [pref])
        cmpb = self.sb('cmpb', [128, 64, 36])
        nblk = self.sb('nblk', [128, 64]); ovf = self.sb('ovf', [128, 64]); incl = self.sb('incl', [128, 64]); delta = self.sb('delta', [128, 64])
        self.tt("dve", cmpb[:], pref[:].unsqueeze(2).to_broadcast([128, 64, 36]), thr.unsqueeze(1).to_broadcast([128, 64, 36]), ALU.is_gt, [pref, mc], [cmpb])
        self.op("dve", lambda e: e.tensor_reduce(out=nblk[:], in_=cmpb[:], axis=AX.X, op=ALU.add), [cmpb], [nblk])
        self.ts("dve", ovf[:], nblk[:], -1.0, ALU.add, [nblk], [ovf], s2=0.0, op1=ALU.max)
        self.op("dve", lambda e: e.tensor_tensor_scan(out=incl[:], data0=ones64, data1=ovf[:], initial=0.0, op0=ALU.mult, op1=ALU.add), [mc, ovf], [incl])
        self.tt("dve", delta[:], incl[:], ovf[:], ALU.subtract, [incl, ovf], [delta])
        self.tt("dve", delta[:], delta[:], iota64, ALU.subtract, [delta, mc], [delta])
        self.ts("dve", delta[:], delta[:], 128.0, ALU.mult, [delta], [delta], s2=8064.0, op1=ALU.add)
        base1 = self.sb('base1', [128, 64])
        self.ts("dve", base1[:], iota64, 128.0, ALU.mult, [mc], [base1])
        dall = self.sb('dall', [128, NT, 64]); ge = self.sb('ge', [128, NT, 64]); destf = self.sb('destf', [128, NT, 2])
        self.ts("dve", ge[:], rank[:], 128.0, ALU.is_ge, [rank], [ge])
        self.tt("dve", ge[:], ge[:], delta[:].unsqueeze(1).to_broadcast([128, NT, 64]), ALU.mult, [ge, delta], [ge])
        self.tt("dve", dall[:], rank[:], base1[:].unsqueeze(1).to_broadcast([128, NT, 64]), ALU.add, [rank, base1], [dall])
        self.tt("dve", dall[:], dall[:], ge[:], ALU.add, [dall, ge], [dall])
        for k in range(2):
            self.tt("dve", ge[:], OH[:, :, k, :], dall[:], ALU.mult, [OH, dall], [ge])
            self.op("dve", lambda e: e.tensor_reduce(out=destf[:, :, k], in_=ge[:], axis=AX.X, op=ALU.add), [ge], [destf])
        self.copy("dve", dest[:], destf[:], [destf], [dest])
        cmp2 = self.sb('cmp2', [128, 36, 64]); bke = self.sb('bke', [128, 36]); wixf = self.sb('wixf', [128, 2, 36])
        self.tt("dve", cmp2[:], incl[:].unsqueeze(1).to_broadcast([128, 36, 64]), bvals.unsqueeze(2).to_broadcast([128, 36, 64]), ALU.is_le, [incl, mc], [cmp2])
        self.op("dve", lambda e: e.tensor_reduce(out=bke[:], in_=cmp2[:], axis=AX.X, op=ALU.add), [cmp2], [bke])
        p2 = self.sb('p2', [128, 1])
        self.ts("dve", p2[:], pidx, 2.0, ALU.mult, [mc], [p2])
        self.ts("dve", wixf[:, 0, :], bke[:], 256.0, ALU.mult, [bke, p2], [wixf], s2=p2[:, 0:1], op1=ALU.add)
        self.ts("dve", wixf[:, 1, :], bke[:], 256.0, ALU.mult, [bke, p2], [wixf], s2=p2[:, 0:1], op1=ALU.add)
        self.copy("dve", wix[:], wixf[:], [wixf], [wix])
        for tt in range(NT):
            for k in range(2):
                self.S.dma("pool", lambda e, tt=tt, k=k: e.indirect_dma_start(
                    out=XB[:, :], out_offset=bass.IndirectOffsetOnAxis(ap=dest[:, tt, k:k + 1], axis=0),
                    in_=hbuf[:, tt, :], in_offset=None), _flat([hbuf.rk(tt), dest]), _flat([XB]))
        self.pop()
        self.push()
        NWB = 3
        w1b = [self.sb(f'w1b{i}', [128, 8, 512], F32R) for i in range(NWB)]
        w3b = [self.sb(f'w3b{i}', [128, 8, 512], F32R) for i in range(NWB)]
        w2b = [self.sb(f'w2b{i}', [128, 4, D], F32R) for i in range(NWB)]
        xb = [self.sb(f'xb{i}', [128, D]) for i in range(2)]
        xbT = self.sb('xbT', [128, 8, 128], F32R)
        a1 = self.sb('a1', [128, 512]); hh = self.sb('hh', [128, 512])
        hhT = self.sb('hhT', [128, 4, 128], F32R)
        yb = [self.sb(f'yb{i}', [128, D]) for i in range(2)]
        ptp = [self.ps(f'ptq{i}', [128, 512]) for i in range(2)]
        ph1 = self.ps('ph1', [128, 512]); ph3 = self.ps('ph3', [128, 512]); pht = self.ps('pht', [128, 512])
        py = [self.ps(f'py{i}', [128, 512]) for i in range(2)]
        w1v = self.inp['moe_w1'].rearrange("l e (r kc) n -> (l e r) (kc n)", kc=4)
        w3v = self.inp['moe_w3'].rearrange("l e (r kc) n -> (l e r) (kc n)", kc=4)
        w2v = self.inp['moe_w2'].rearrange("l e (r c) n -> (l e r) (c n)", c=2)
        loff = l * 64 * 1024 * 512
        if not hasattr(self, 'reg_b13'):
            self.reg_b13 = self.nc.gpsimd.to_reg(16383)
            self.reg_b2 = self.nc.gpsimd.to_reg(32767)
        reg_b13, reg_b2 = self.reg_b13, self.reg_b2
        order = []
        nxt2 = 0
        for e_ in range(64):
            order.append(e_)
            while nxt2 < N_OV and (nxt2 + 1) * 64 <= (e_ + 1) * N_OV:
                order.append(64 + nxt2)
                nxt2 += 1
        assert len(order) == 64 + N_OV and nxt2 == N_OV
        def load_w(pos, blk):
            iw = pos % NWB
            W1, W3, W2 = w1b[iw], w3b[iw], w2b[iw]
            if blk < 64:
                self.dma("pool", W1[:], self.inp['moe_w1'][l, blk].rearrange("(p kc) n -> p kc n", kc=8), writes=[W1], max_dma_last_dim=8192)
                self.dma("pool", W3[:], self.inp['moe_w3'][l, blk].rearrange("(p kc) n -> p kc n", kc=8), writes=[W3], max_dma_last_dim=8192)
                self.dma("pool", W2[:], self.inp['moe_w2'][l, blk].rearrange("(p kc) n -> p kc n", kc=4), writes=[W2], max_dma_last_dim=8192)
            else:
                b = blk - 64
                for (Wt, wv) in ((W1, w1v), (W3, w3v), (W2, w2v)):
                    w2d = Wt[:].rearrange("p k n -> p (k n)")
                    for half in range(2):
                        self.S.dma("pool", lambda e, w2d=w2d, wv=wv, half=half, b=b: e.indirect_dma_start(
                            out=w2d[:, half * 2048:(half + 1) * 2048], out_offset=None, in_=wv[:, :],
                            in_offset=bass.IndirectOffsetOnAxis(ap=wix[:, 0, b:b + 1], axis=0),
                            element_offset=loff + half * 2048, bounds_check=reg_b13, oob_is_err=False), _flat([wix]), _flat([Wt]))

        def stage1(pos, blk):
            x_ = xb[pos % 2]
            self.dma("sp", x_[:], XB[blk * 128:(blk + 1) * 128, :], reads=[XB], writes=[x_])
            for half in range(2):
                p = ptp[half]
                for q in range(4):
                    kc = half * 4 + q
                    self.tr(p[:, q * 128:(q + 1) * 128], x_[:, kc:D:8], self.ident[:], [x_, self.ident], [p])
                self.evac(half, xbT[:, half * 4:(half + 1) * 4, :], p[:].rearrange("p (q t) -> p q t", q=4), [p], [xbT])

        def stage2(pos, blk):
            iw = pos % NWB
            W1, W3 = w1b[iw], w3b[iw]
            for kc in range(8):
                self.mm(ph1[:], xbT[:, kc, :], W1[:, kc, :], kc == 0, kc == 7, [xbT, W1], [ph1])
            for kc in range(8):
                self.mm(ph3[:], xbT[:, kc, :], W3[:, kc, :], kc == 0, kc == 7, [xbT, W3], [ph3])
            self.act(a1[:], ph1[:], AF.Silu, [ph1], [a1])
            self.tt("dve", hh[:], a1[:], ph3[:], ALU.mult, [a1, ph3], [hh])

        def stage34(pos, blk):
            iw = pos % NWB
            W2 = w2b[iw]
            for q in range(4):
                self.tr(pht[:, q * 128:(q + 1) * 128], hh[:, q:512:4], self.ident[:], [hh, self.ident], [pht])
            self.copy("act", hhT[:], pht[:].rearrange("p (q t) -> p q t", q=4), [pht], [hhT])
            y_ = yb[pos % 2]
            for half in range(2):
                p = py[half]
                for c in range(4):
                    self.mm(p[:], hhT[:, c, :], W2[:, c, half * 512:(half + 1) * 512], c == 0, c == 3, [hhT, W2], [p])
                self.evac(half, y_[:, half * 512:(half + 1) * 512], p[:], [p], [y_])
            self.dma("sp", YB[blk * 128:(blk + 1) * 128, :], y_[:], reads=[y_], writes=[YB])

        nb = len(order)
        load_w(0, order[0])
        if nb > 1:
            load_w(1, order[1])
        stage1(0, order[0])
        for pos, blk in enumerate(order):
            if pos + 2 < nb:
                load_w(pos + 2, order[pos + 2])
            stage2(pos, blk)
            if pos + 1 < nb:
                stage1(pos + 1, order[pos + 1])
            stage34(pos, blk)
        self.pop()
        self.push()
        g2 = [self.sb(f'g2{r}', [128, D]) for r in range(2)]
        for r in range(2):
            self.load_mod(g2[r], 5, r)
        Y0 = [self.sb(f'Y0{i}', [128, D]) for i in range(2)]
        Y1 = [self.sb(f'Y1{i}', [128, D]) for i in range(2)]
        xt = [self.sb(f'xf{i}', [128, D]) for i in range(2)]
        for tt in range(NT):
            r = 1 if tt < 2 else 0
            i = tt % 2
            for k, Yk in enumerate((Y0[i], Y1[i])):
                self.S.dma("pool", lambda e, Yk=Yk, tt=tt, k=k: e.indirect_dma_start(
                    out=Yk[:, :], out_offset=None, in_=YB[:, :],
                    in_offset=bass.IndirectOffsetOnAxis(ap=dest[:, tt, k:k + 1], axis=0)), _flat([YB, dest]), _flat([Yk]))
            x_ = xt[i]
            self.dma("sp", x_[:], self.XS[tt * 128:(tt + 1) * 128, :], reads=self.XS.rk(tt), writes=[x_])
            self.ts("dve", Y0[i][:], Y0[i][:], gates[:, tt, 0:1], ALU.mult, [Y0[i], gates], [Y0[i]])
            self.stt(Y0[i][:], Y1[i][:], gates[:, tt, 1:2], Y0[i][:], ALU.mult, ALU.add, [Y1[i], Y0[i], gates], [Y0[i]])
            self.tt("pool", Y0[i][:], Y0[i][:], g2[r][:], ALU.mult, [Y0[i], g2[r]], [Y0[i]])
            self.tt("dve", x_[:], x_[:], Y0[i][:], ALU.add, [x_, Y0[i]], [x_])
            self.dma("sp", self.XS[tt * 128:(tt + 1) * 128, :], x_[:], reads=[x_], writes=self.XS.rk(tt))
        self.pop()
        self.pop()

    def phase_final(self):
        self.push()
        fg = self.sb('fg', [128, D])
        self.dma("sp", fg[:], self.inp['final_norm_g'][0:1, :].partition_broadcast(128), writes=[fg])
        xt = [self.sb(f'xo{i}', [128, D]) for i in range(2)]
        xn = [self.sb(f'xq{i}', [128, D]) for i in range(2)]
        junk = self.sb('junk', [128, D])
        ss = [self.sb(f'sso{i}', [128, 4]) for i in range(2)]
        evs = []
        for tt in range(2, NT):
            i = tt % 2
            self.dma("sp", xt[i][:], self.XS[tt * 128:(tt + 1) * 128, :], reads=self.XS.rk(tt), writes=[xt[i]])
            self.norm_tile(xt[i], fg, None, xn[i], junk, ss[i])
            evs.append(self.dma("sp", self.out[(tt - 2) * 128:(tt - 1) * 128, :], xn[i][:], reads=[xn[i]]))
        for ev in evs:
            self.S.wait_event("sp", ev)
        self.pop()

    return dict(phase_wout=phase_wout, phase_moe=phase_moe, phase_final=phase_final)


for _k, _v in _tail_methods().items():
    setattr(Kern, _k, _v)
```

```python
import contextlib
import numpy as np
import concourse.bass as bass
import concourse.mybir as mybir
from concourse.bass_utils import run_bass_kernel_spmd

F32 = mybir.dt.float32
F32R = mybir.dt.float32r
BF16 = mybir.dt.bfloat16
I32 = mybir.dt.int32
AF = mybir.ActivationFunctionType
ALU = mybir.AluOpType
AX = mybir.AxisListType

D = 1024
T_CTX = 256
T_LAT = 2048
T = T_CTX + T_LAT
NT = T // 128
NCH = T // 64
DEPTH = 4
DIN = 2320
EPS = 1e-6
NEG = -30000.0


class Res:
    __slots__ = ("name", "writer", "readers", "excl")

    def __init__(self, name):
        self.name = name
        self.writer = None
        self.readers = []
        self.excl = False


class Sched:
    ENG = ("pe", "dve", "act", "pool", "sp")

    def __init__(self, nc, n_dma_sems=8):
        self.nc = nc
        self.obj = {"pe": nc.tensor, "dve": nc.vector, "act": nc.scalar,
                    "pool": nc.gpsimd, "sp": nc.sync}
        self.sem = {}
        self.count = {}
        self.waited = {e: {} for e in self.ENG}
        self._ctx = []
        for e in self.ENG:
            cm = nc.semaphore("s_" + e)
            self.sem[e] = cm.__enter__()
            self._ctx.append(cm)
            self.count[e] = 0
        self.dma_sems = {}
        self.dma_rr = {}
        self.gen = 0
        self.n_dma_sems = n_dma_sems
        for q in ("sp", "pool"):
            lst = []
            for i in range(n_dma_sems):
                cm = nc.semaphore(f"d_{q}{i}")
                lst.append([cm.__enter__(), 0])
                self._ctx.append(cm)
            self.dma_sems[q] = lst
            self.dma_rr[q] = 0
        self.n_wait = 0
        self.n_inst = 0

    def renew(self):
        self.barrier()
        self.gen += 1
        nc = self.nc
        for e in self.ENG:
            cm = nc.semaphore(f"s_{e}_g{self.gen}")
            self.sem[e] = cm.__enter__()
            self._ctx.append(cm)
            self.count[e] = 0
        for q in ("sp", "pool"):
            lst = []
            for i in range(self.n_dma_sems):
                cm = nc.semaphore(f"d_{q}{i}_g{self.gen}")
                lst.append([cm.__enter__(), 0])
                self._ctx.append(cm)
            self.dma_sems[q] = lst
            self.dma_rr[q] = 0
        self.waited = {e: {} for e in self.ENG}

    def _need(self, eng, ev, same_ok_dist=None):
        if ev is None:
            return None
        if len(ev) > 4 and ev[4] != self.gen:
            return None
        key, sem, val, src = ev[:4]
        if src == eng and src is not None:
            if same_ok_dist is None:
                return None
            if self.count[eng] - val >= same_ok_dist:
                return None
        if self.waited[eng].get(key, 0) >= val:
            return None
        return ev

    def _emit_waits(self, eng, evs):
        best = {}
        for ev in evs:
            if ev is None:
                continue
            key = ev[0]
            if key not in best or best[key][2] < ev[2]:
                best[key] = ev
        for key, evb in best.items():
            sem, val = evb[1], evb[2]
            self.obj[eng].wait_ge(sem, val)
            self.waited[eng][key] = val
            self.n_wait += 1

    def deps(self, eng, reads, writes):
        evs = []
        for r in reads:
            evs.append(self._need(eng, r.writer, same_ok_dist=4))
        for w in writes:
            evs.append(self._need(eng, w.writer, same_ok_dist=None))
            for rd in w.readers:
                evs.append(self._need(eng, rd, same_ok_dist=None))
        return evs

    def _commit(self, ev, reads, writes):
        for r in reads:
            r.readers.append(ev)
            if len(r.readers) > 48:
                best = {}
                for e in r.readers:
                    if e[4] != self.gen:
                        continue
                    if e[0] not in best or best[e[0]][2] < e[2]:
                        best[e[0]] = e
                r.readers = list(best.values())
        for w in writes:
            w.writer = ev
            w.readers = []

    def op(self, eng, fn, reads=(), writes=()):
        ex = [r for r in reads if r.excl]
        if ex:
            writes = list(writes) + ex
        self._emit_waits(eng, self.deps(eng, reads, writes))
        ins = fn(self.obj[eng])
        self.count[eng] += 1
        ins.then_inc(self.sem[eng], 1)
        ev = (eng, self.sem[eng], self.count[eng], eng, self.gen)
        self._commit(ev, reads, writes)
        self.n_inst += 1
        return ev

    def dma(self, q, fn, reads=(), writes=()):
        evs = self.deps(q, reads, writes)
        idx = self.dma_rr[q]
        slot = self.dma_sems[q][idx]
        self.dma_rr[q] = (idx + 1) % len(self.dma_sems[q])
        key = f"d_{q}{idx}"
        if slot[1] > 0:
            evs.append(self._need(q, (key, slot[0], slot[1], None, self.gen)))
        self._emit_waits(q, evs)
        ins = fn(self.obj[q])
        slot[1] += 16
        ins.then_inc(slot[0], 16)
        ev = (key, slot[0], slot[1], None, self.gen)
        self._commit(ev, reads, writes)
        self.n_inst += 1
        return ev

    def wait_event(self, eng, ev):
        self._emit_waits(eng, [self._need(eng, ev)])

    def barrier(self):
        evs = []
        for e in self.ENG:
            if self.count[e] > 0:
                evs.append((e, self.sem[e], self.count[e], e, self.gen))
        for q, lst in self.dma_sems.items():
            for i, (sem, val) in enumerate(lst):
                if val > 0:
                    evs.append((f"d_{q}{i}", sem, val, None, self.gen))
        for e in self.ENG:
            need = []
            for ev in evs:
                if ev[3] == e:
                    continue
                if self.waited[e].get(ev[0], 0) >= ev[2]:
                    continue
                need.append(ev)
            self._emit_waits(e, need)


class Tl:
    def __init__(self, ap, name, nsub=0):
        self.ap = ap
        self.name = name
        self.rs = [Res(f"{name}.{i}") for i in range(max(1, nsub))]

    def __getitem__(self, key):
        return self.ap[key]

    @property
    def r(self):
        return self.rs

    def rk(self, *ks):
        n = len(self.rs)
        return [self.rs[min(k, n - 1)] for k in ks]


def _flat(lst):
    out = []
    for x in lst:
        if isinstance(x, Tl):
            out.extend(x.rs)
        elif isinstance(x, (list, tuple)):
            out.extend(_flat(x))
        elif x is not None:
            out.append(x)
    return out


class Builder:
    def __init__(self, n_layers=DEPTH, debug=None, skip_inputs=(), flags=()):
        self.flags = set(flags)
        self.skip_inputs = set(skip_inputs)
        self.n_layers = n_layers
        self.debug = debug or {}
        self.nc = bass.Bass("TRN2", target_bir_lowering=False)
        self.S = Sched(self.nc)
        self.stack = [contextlib.ExitStack()]
        self._uid = 0
        self.outputs = []

    def push(self):
        self.stack.append(contextlib.ExitStack())

    def pop(self):
        self.S.barrier()
        self.stack.pop().close()

    def sb(self, name, shape, dt=F32, nsub=0):
        self._uid += 1
        t = self.stack[-1].enter_context(self.nc.sbuf_tensor(f"{name}_{self._uid}", list(shape), dt))
        return Tl(t, name, nsub)

    def ps(self, name, shape, dt=F32, nsub=0):
        self._uid += 1
        t = self.stack[-1].enter_context(self.nc.psum_tensor(f"{name}_{self._uid}", list(shape), dt))
        tl = Tl(t, name, nsub)
        for r in tl.rs:
            r.excl = True
        return tl

    def dram(self, name, shape, dt=F32, kind="Internal", nsub=0):
        t = self.nc.dram_tensor(name, list(shape), dt, kind=kind).ap()
        return Tl(t, name, nsub)

    op_limit = None
    op_cnt = 0

    def op(self, eng, fn, reads=(), writes=()):
        if self.op_limit is not None:
            self.op_cnt += 1
            if self.op_cnt > self.op_limit:
                return None
            if self.op_cnt == self.op_limit:
                import inspect
                fr = inspect.stack()
                print("LAST OP:", eng, [f"{f.function}:{f.lineno}" for f in fr[1:5]])
        return self.S.op(eng, fn, _flat(reads), _flat(writes))

    def dma(self, q, out, in_, reads=(), writes=(), **kw):
        return self.S.dma(q, lambda e: e.dma_start(out=out, in_=in_, **kw), _flat(reads), _flat(writes))

    def mm(self, out, lhsT, rhs, start, stop, reads, writes):
        return self.op("pe", lambda e: e.matmul(out, lhsT=lhsT, rhs=rhs, start=start, stop=stop), reads, writes)

    def tr(self, out, in_, ident, reads, writes):
        return self.op("pe", lambda e: e.transpose(out, in_, ident), reads, writes)

    def act(self, out, in_, func, reads, writes, eng="act", **kw):
        return self.op("act", lambda e: e.activation(out=out, in_=in_, func=func, **kw), reads, writes)

    def tt(self, eng, out, in0, in1, op, reads, writes):
        return self.op(eng, lambda e: e.tensor_tensor(out=out, in0=in0, in1=in1, op=op), reads, writes)

    def ts(self, eng, out, in0, s1, op0, reads, writes, s2=None, op1=None):
        if op1 is None:
            return self.op(eng, lambda e: e.tensor_scalar(out=out, in0=in0, scalar1=s1, scalar2=None, op0=op0), reads, writes)
        return self.op(eng, lambda e: e.tensor_scalar(out=out, in0=in0, scalar1=s1, scalar2=s2, op0=op0, op1=op1), reads, writes)

    def stt(self, out, in0, scalar, in1, op0, op1, reads, writes):
        return self.op("dve", lambda e: e.scalar_tensor_tensor(out=out, in0=in0, scalar=scalar, in1=in1, op0=op0, op1=op1), reads, writes)

    def copy(self, eng, out, in_, reads, writes):
        if eng == "act":
            return self.op("act", lambda e: e.copy(out=out, in_=in_), reads, writes)
        return self.op(eng, lambda e: e.tensor_copy(out=out, in_=in_), reads, writes)

    def memset(self, eng, ap, val, writes):
        return self.op(eng, lambda e: e.memset(ap, val), (), writes)


C_GQ, C_GK, C_GV, C_GG, C_GAB = 0, 256, 512, 768, 1024
C_LX, C_LY = 1040, 1296
C_AQ, C_AK, C_AV = 1552, 2064, 2192
PF_GDN, PF_LRU = 0, 768
PT_GATE, PT_AB, PT_ATT = 0, 256, 272
PT_W = 1040

EXTRA_LAYOUT = ['gdn_conv_wT', 'lru_conv_wT']
WEIGHT_NAMES = ['w_ada', 'b_ada', 'norm1_g', 'norm2_g', 'w_in', 'w_out', 'gdn_conv_w', 'gdn_a_log',
                'gdn_dt_bias', 'gdn_norm_g', 'lru_conv_w', 'lru_conv_b', 'lru_w_r', 'lru_b_r', 'lru_w_i',
                'lru_b_i', 'lru_lambda', 'attn_q_norm_g', 'attn_k_norm_g', 'moe_w_group', 'moe_b_group',
                'moe_w_expert', 'moe_b_expert', 'moe_w1', 'moe_w3', 'moe_w2', 'final_norm_g']
WEIGHT_SHAPES = {
    'w_ada': [4, 1024, 6144], 'b_ada': [4, 6144], 'norm1_g': [4, 1024], 'norm2_g': [4, 1024],
    'w_in': [4, 1024, 2320], 'w_out': [4, 1024, 1024], 'gdn_conv_w': [4, 4, 768], 'gdn_a_log': [4, 2, 4],
    'gdn_dt_bias': [4, 2, 4], 'gdn_norm_g': [4, 64], 'lru_conv_w': [4, 4, 256], 'lru_conv_b': [4, 256],
    'lru_w_r': [4, 2, 4, 64, 64], 'lru_b_r': [4, 2, 256], 'lru_w_i': [4, 2, 4, 64, 64], 'lru_b_i': [4, 2, 256],
    'lru_lambda': [4, 2, 256], 'attn_q_norm_g': [4, 64], 'attn_k_norm_g': [4, 64], 'moe_w_group': [4, 1024, 8],
    'moe_b_group': [4, 8], 'moe_w_expert': [4, 1024, 64], 'moe_b_expert': [4, 64],
    'moe_w1': [4, 64, 1024, 512], 'moe_w3': [4, 64, 1024, 512], 'moe_w2': [4, 64, 512, 1024],
    'final_norm_g': [1, 1024], 'gdn_conv_wT': [4, 768, 4], 'lru_conv_wT': [4, 256, 4]}


def host_consts():
    c = {}
    c['ident'] = np.eye(128, dtype=np.float32)
    r = np.arange(64)[:, None]
    q = np.arange(64)[None, :]
    def t4(m):
        return np.tile(m.astype(np.float32), (1, 4))
    g = np.zeros((64, 9, 256), np.float32)
    g[:, 0] = t4(np.where(r >= q, 0.0, NEG))
    g[:, 1] = t4(np.where(r <= q, 0.0, NEG))
    g[:, 2] = t4(np.where(r > q, -1.0, 0.0))
    g[:, 3] = t4(np.where(r < q, -1.0, 0.0))
    g[:, 4] = t4(np.where(r <= q, 1.0, 0.0))
    g[:, 5] = t4(np.where(r >= q, 1.0, 0.0))
    g[:, 6] = 1.0
    g[:, 7] = -1.0
    g[:, 8] = t4(np.eye(64))
    c['gconst'] = g.reshape(64, 9 * 256)
    pos = np.arange(T_LAT)
    row = (pos // 64).astype(np.float64); col = (pos % 64).astype(np.float64)
    inv = 10000.0 ** (-np.arange(0, 32, 2, dtype=np.float64) / 32)
    ang = np.concatenate([row[:, None] * inv, col[:, None] * inv], axis=-1)
    c['rope_cs'] = np.concatenate([np.cos(ang), np.sin(ang)], axis=-1).astype(np.float32)
    mc = np.zeros((128, 256), np.float32)
    mc[:, 0:64] = np.arange(64)[None, :]
    mc[:, 64:100] = 128.0 * np.arange(36)[None, :]
    mc[:, 100:136] = np.arange(36)[None, :]
    mc[:, 136] = np.arange(128)
    mc[:, 137:201] = 1.0
    c['moe_const'] = mc
    tri = np.zeros((128, 256), np.float32)
    tri[:, 0:128] = (np.arange(128)[:, None] < np.arange(128)[None, :])
    tri[:, 128:256] = 1.0
    c['moe_tri'] = tri
    return c


class Kern(Builder):
    def declare_io(self):
        nc = self.nc
        self.inp = {}
        self.inp['xs_in'] = nc.dram_tensor('xs_in', [T, D], F32, kind='ExternalInput').ap()
        self.inp['cT_in'] = nc.dram_tensor('cT_in', [128, 16], F32, kind='ExternalInput').ap()
        for n in WEIGHT_NAMES + EXTRA_LAYOUT:
            if n in self.skip_inputs:
                continue
            shp = list(WEIGHT_SHAPES[n])
            if shp[0] == 4:
                shp[0] = self.n_layers
            self.inp[n] = nc.dram_tensor(n, shp, F32, kind='ExternalInput').ap()
        for n, v in host_consts().items():
            self.inp[n] = nc.dram_tensor(n, list(v.shape), F32, kind='ExternalInput').ap()
        self.out = nc.dram_tensor('out', [T_LAT, D], F32, kind='ExternalOutput').ap()
        self.XS = self.dram('XS', [T, D], nsub=NT)
        self.MODS = self.dram('MODS', [2, 6 * D])
        self.PF = self.dram('PF', [1280, T])
        self.PT = self.dram('PT', [T, PT_W], nsub=NT)
        self.MO = self.dram('MO', [D, T])
        self.XB = self.dram('XB', [N_SLOT, D])
        self.YB = self.dram('YB', [N_SLOT, D])
        self.dbg = {}
        for name, shape in self.debug.items():
            self.dbg[name] = nc.dram_tensor('dbg_' + name, list(shape), F32, kind='ExternalOutput').ap()

    def bcast_row(self, ap_row, n):
        return ap_row.partition_broadcast(128)

    def phase_init(self):
        for tt in range(NT):
            self.dma("sp", self.XS[tt * 128:(tt + 1) * 128, :], self.inp['xs_in'][tt * 128:(tt + 1) * 128, :],
                     writes=self.XS.rk(tt))
        self.ident = self.sb('ident', [128, 128])
        self.dma("sp", self.ident[:], self.inp['ident'][:, :], writes=[self.ident])
        cT = self.sb('cT', [128, 16])
        self.dma("sp", cT[:], self.inp['cT_in'][:, :], writes=[cT])
        self.scT = self.sb('scT', [128, 8, 2], F32R)
        self.act(self.scT[:, :, 0], cT[:, 0:8], AF.Silu, [cT], [self.scT])
        self.act(self.scT[:, :, 1], cT[:, 8:16], AF.Silu, [cT], [self.scT])

    def phase_ada(self, l):
        self.push()
        wa = [self.sb(f'wa{i}', [128, 8, 512], F32R) for i in range(2)]
        ba = self.sb('ba', [2, 6 * D])
        mods = self.sb('mods', [2, 6 * D])
        pp = [self.ps(f'pada{i}', [2, 512]) for i in range(2)]
        for r in range(2):
            self.dma("sp", ba[r:r + 1, :], self.inp['b_ada'][l:l + 1, :], writes=[ba])
        wsrc = self.inp['w_ada'][l].rearrange("(kc p) n -> p kc n", p=128)
        for cg in range(12):
            w = wa[cg % 2]
            self.dma("pool", w[:], wsrc[:, :, cg * 512:(cg + 1) * 512], writes=[w])
            p = pp[cg % 2]
            for kc in range(8):
                self.mm(p[:], self.scT[:, kc, :], w[:, kc, :], kc == 0, kc == 7, [self.scT, w], [p])
            self.tt("dve", mods[:, cg * 512:(cg + 1) * 512], p[:], ba[:, cg * 512:(cg + 1) * 512], ALU.add,
                    [p, ba], [mods])
        self.dma("sp", self.MODS[:, :], mods[:], reads=[mods], writes=[self.MODS])
        self.pop()

    def load_mod(self, dst, which, r, g_name=None, l=0):
        self.dma("sp", dst[:], self.MODS[r:r + 1, which * D:(which + 1) * D].partition_broadcast(128),
                 reads=[self.MODS], writes=[dst])
        if g_name is not None:
            gb = self.sb('gb', [128, D])
            self.dma("sp", gb[:], self.inp[g_name][l:l + 1, :].partition_broadcast(128), writes=[gb])
            self.stt(dst[:], dst[:], 1.0, gb[:], ALU.add, ALU.mult, [dst, gb], [dst])

    def evac(self, i, out, in_, reads, writes):
        return self.copy("act" if i % 2 == 0 else "dve", out, in_, reads, writes)

    def norm_tile(self, xt, gm, sh, xn, junk, ss):
        self.act(junk[:], xt[:], AF.Square, [xt], [junk, ss], accum_out=ss[:, 0:1])
        self.act(ss[:, 1:2], ss[:, 0:1], AF.Sqrt, [ss], [ss], scale=1.0 / D, bias=self.eps_t[:, 0:1])
        self.op("dve", lambda e: e.reciprocal(out=ss[:, 2:3], in_=ss[:, 1:2]), [ss], [ss])
        self.stt(xn[:], xt[:], ss[:, 2:3], gm[:], ALU.mult, ALU.mult, [xt, ss, gm], [xn])
        if sh is not None:
            self.tt("pool", xn[:], xn[:], sh[:], ALU.add, [xn, sh], [xn])

    def phase_proj(self, l):
        self.push()
        win = self.sb('win', [128, 8, DIN], F32R)
        wsrc = self.inp['w_in'][l].rearrange("(kc p) n -> p kc n", p=128)
        for a, b in ((0, 1160), (1160, 2320)):
            self.dma("pool", win[:, :, a:b], wsrc[:, :, a:b], writes=[win])
        gm = [self.sb(f'gm{r}', [128, D]) for r in range(2)]
        sh = [self.sb(f'sh{r}', [128, D]) for r in range(2)]
        self.push()
        for r in range(2):
            self.load_mod(gm[r], 1, r, 'norm1_g', l)
            self.load_mod(sh[r], 0, r)
        self.pop()
        xt = [self.sb(f'xt{i}', [128, D]) for i in range(2)]
        xn = [self.sb(f'xn{i}', [128, D]) for i in range(2)]
        junk = self.sb('junk', [128, D])
        ss = [self.sb(f'ss{i}', [128, 4]) for i in range(2)]
        xnT = self.sb('xnT', [128, 8, 512], F32R, nsub=4)
        sfm = [self.sb(f'sfm{i}', [128, 512]) for i in range(2)]
        stm = [self.sb(f'stm{i}', [128, PT_W]) for i in range(2)]
        ptp = [self.ps(f'ptp{i}', [128, 512]) for i in range(2)]
        pfm = [self.ps(f'pfm{i}', [128, 512]) for i in range(2)]
        ptm = [self.ps('ptm0', [128, 272]), self.ps('ptm1', [128, 512]), self.ps('ptm2', [128, 256])]
        fm_groups = [(c0, PF_GDN + c0) for c0 in range(0, 768, 128)] + \
                    [(C_LX + c0, PF_LRU + c0) for c0 in range(0, 512, 128)]
        tm_groups = [(C_GG, 272, PT_GATE), (C_AQ, 512, PT_ATT), (C_AQ + 512, 256, PT_ATT + 512)]
        blocks = [(0, 2), (2, 4), (6, 4), (10, 4), (14, 4)]
        it = 0
        ig = 0
        for (t0, ntile) in blocks:
            r = 1 if t0 == 0 else 0
            for j in range(ntile):
                tt = t0 + j
                x_ = xt[it % 2]; xn_ = xn[it % 2]; ss_ = ss[it % 2]
                self.dma("sp", x_[:], self.XS[tt * 128:(tt + 1) * 128, :], reads=self.XS.rk(tt), writes=[x_])
                self.norm_tile(x_, gm[r], sh[r], xn_, junk, ss_)
                for half in range(2):
                    p = ptp[half]
                    for q in range(4):
                        kc = half * 4 + q
                        self.tr(p[:, q * 128:(q + 1) * 128], xn_[:, kc * 128:(kc + 1) * 128], self.ident[:],
                                [xn_, self.ident], [p])
                    self.evac(half, xnT[:, half * 4:(half + 1) * 4, j * 128:(j + 1) * 128],
                              p[:].rearrange("p (q t) -> p q t", q=4), [p], xnT.rk(j))
                st = stm[it % 2]
                for gi, (c0, n, d0) in enumerate(tm_groups):
                    p = ptm[gi]
                    for kc in range(8):
                        self.mm(p[:, 0:n], xnT[:, kc, j * 128:(j + 1) * 128], win[:, kc, c0:c0 + n], kc == 0, kc == 7,
                                [xnT.rk(j), win], [p])
                    self.evac(gi, st[:, d0:d0 + n], p[:, 0:n], [p], [st])
                self.dma("sp", self.PT[tt * 128:(tt + 1) * 128, :], st[:], reads=[st], writes=self.PT.rk(tt))
                it += 1
            ntok = ntile * 128
            for (c0, d0) in fm_groups:
                p = pfm[ig % 2]; s_ = sfm[ig % 2]
                for kc in range(8):
                    self.mm(p[:, 0:ntok], win[:, kc, c0:c0 + 128], xnT[:, kc, 0:ntok], kc == 0, kc == 7,
                            [win, xnT.rk(*range(ntile))], [p])
                self.evac(ig, s_[:, 0:ntok], p[:, 0:ntok], [p], [s_])
                self.dma("sp", self.PF[d0:d0 + 128, t0 * 128:t0 * 128 + ntok], s_[:, 0:ntok], reads=[s_], writes=[self.PF])
                ig += 1
        self.pop()

    def build(self, stop_after=None):
        self.stop = stop_after
        self.declare_io()
        self.eps_t = self.sb('eps', [128, 1])
        self.memset("pool", self.eps_t[:], EPS, [self.eps_t])
        self.phase_init()
        for l in range(self.n_layers):
            if l > 0:
                self.S.renew()
            self.phase_ada(l)
            self.phase_proj(l)
            if stop_after == 'proj':
                break
            if 'nogdn' not in self.flags:
                self.phase_gdn(l)
            if stop_after is not None and stop_after.startswith('gdn'):
                break
            self.phase_lru(l)
            if stop_after == 'lru':
                break
            self.phase_att(l)
            if stop_after == 'att':
                break
            self.phase_wout(l)
            if stop_after == 'wout':
                break
            self.phase_moe(l)
            if stop_after == 'moe':
                break
        if stop_after is None:
            self.phase_final()
        evs = []
        for name, ap in self.dbg.items():
            src = {'PF': self.PF, 'PT': self.PT, 'MODS': self.MODS, 'MO': self.MO, 'XS': self.XS}[name]
            self.S.barrier()
            evs.append(self.dma("sp", ap[:, :], src[:, :], reads=[src]))
        for ev in evs:
            self.S.wait_event("sp", ev)
        self.S.barrier()
        return self.nc


def make_in_maps(inputs, n_layers=DEPTH, skip=()):
    consts = host_consts()
    maps = []
    for b in range(8):
        m = {}
        m['xs_in'] = np.ascontiguousarray(np.concatenate([inputs['ctx'][b], inputs['x'][b]], axis=0), dtype=np.float32)
        cT = np.concatenate([np.asarray(inputs['c'][b]).reshape(8, 128).T,
                             np.asarray(inputs['c_ctx']).reshape(8, 128).T], axis=1)
        m['cT_in'] = np.ascontiguousarray(cT, dtype=np.float32)
        for n in WEIGHT_NAMES:
            if n in skip:
                continue
            a = np.asarray(inputs[n], dtype=np.float32).reshape(WEIGHT_SHAPES[n])
            if WEIGHT_SHAPES[n][0] == 4:
                a = a[:n_layers]
            m[n] = np.ascontiguousarray(a)
        m['gdn_conv_wT'] = np.ascontiguousarray(np.transpose(np.asarray(inputs['gdn_conv_w'], dtype=np.float32), (0, 2, 1))[:n_layers])
        m['lru_conv_wT'] = np.ascontiguousarray(np.transpose(np.asarray(inputs['lru_conv_w'], dtype=np.float32), (0, 2, 1))[:n_layers])
        m.update(consts)
        maps.append(m)
    return maps


def kernel(**inputs):
    kb = Kern()
    nc = kb.build()
    res = run_bass_kernel_spmd(nc, make_in_maps(inputs), core_ids=list(range(8)))
    return np.stack([np.asarray(r['out']) for r in res.results], axis=0)


G_NML, G_NMU, G_SML, G_SMU, G_L, G_U, G_ONE, G_NEG1, G_ID = range(9)


def _gdn_methods():
    def gc(self, k, n=256):
        return self.gconst[:, k * 256:k * 256 + n]

    def phase_gdn(self, l):
        self.push()
        S = self.S
        gconst = self.sb('gconst', [64, 9 * 256])
        self.gconst = gconst
        self.dma("sp", gconst[:], self.inp['gconst'][:, :], writes=[gconst])
        one_t = self.sb('one', [128, 1])
        self.memset("pool", one_t[:], 1.0, [one_t])
        qn = self.sb('qn', [64, 4, T], nsub=NCH)
        kn = self.sb('kn', [64, 4, T], nsub=NCH)
        vT = self.sb('vT', [128, 2, T], nsub=NCH)
        OF = self.dram(f'OF{l}', [T, 256], nsub=NCH)
        cwq = self.sb('cwq', [64, 8, 4])
        cwv = self.sb('cwv', [128, 2, 4])
        cw_src = self.inp['gdn_conv_wT'][l]
        self.dma("sp", cwq[:], cw_src[0:512, :].rearrange("(h p) m -> p h m", p=64), writes=[cwq])
        self.dma("sp", cwv[:], cw_src[512:768, :].rearrange("(h p) m -> p h m", p=128), writes=[cwv])
        chunks_all = list(range(NCH))

        self.push()
        raw = [self.sb(f'raw{i}', [128, 516]) for i in range(3)]
        lnb = self.sb('lnb', [64, 4, T])
        sqb = [self.sb(f'sqb{i}', [64, 512]) for i in range(2)]
        pss = [self.ps(f'pss{i}', [64, 512]) for i in range(2)]
        segs = [(0, T_CTX, [(0, 256)]), (T_CTX, T, [(T_CTX + i * 512, 512) for i in range(4)])]
        it = 0
        for kind in range(10):
            P = 64 if kind < 8 else 128
            row0 = kind * 64 if kind < 8 else 512 + (kind - 8) * 128
            cw = cwq[:, kind, :] if kind < 8 else cwv[:, kind - 8, :]
            cwt = cwq if kind < 8 else cwv
            for (s0, s1, blks) in segs:
                for (t0, blk) in blks:
                    rw = raw[it % 3]
                    lo = max(t0 - 1, s0); hi = min(t0 + blk + 2, s1)
                    if lo > t0 - 1:
                        self.memset("pool", rw[0:P, 0:1], 0.0, [rw])
                    if hi < t0 + blk + 2:
                        self.memset("pool", rw[0:P, hi - (t0 - 1):blk + 3], 0.0, [rw])
                    self.dma("sp", rw[0:P, lo - (t0 - 1):hi - (t0 - 1)], self.PF[row0:row0 + P, lo:hi],
                             reads=[self.PF], writes=[rw])
                    if kind >= 8:
                        dt_, dst = vT, vT[:, kind - 8, t0:t0 + blk]
                    elif kind < 4:
                        dt_, dst = qn, qn[:, kind, t0:t0 + blk]
                    else:
                        dt_, dst = kn, kn[:, kind - 4, t0:t0 + blk]
                    self.ts("dve", dst, rw[0:P, 0:blk], cw[:, 0:1], ALU.mult, [rw, cwt], [dt_])
                    for m in range(1, 4):
                        self.stt(dst, rw[0:P, m:m + blk], cw[:, m:m + 1], dst, ALU.mult, ALU.add, [rw, cwt, dt_], [dt_])
                    it += 1
        f2 = lambda t: t[:].rearrange("p h t -> p (h t)")
        for t_ in (qn, kn, vT):
            self.act(f2(t_), f2(t_), AF.Silu, [t_], [t_])
        blks_all = [(0, 256)] + [(T_CTX + i * 512, 512) for i in range(4)]
        it = 0
        for t_, sc in ((qn, 0.125), (kn, 1.0)):
            for h in range(4):
                for (t0, blk) in blks_all:
                    sq = sqb[it % 2]; ps_ = pss[it % 2]
                    self.tt("pool", sq[:, 0:blk], t_[:, h, t0:t0 + blk], t_[:, h, t0:t0 + blk], ALU.mult, [t_], [sq])
                    self.mm(ps_[:, 0:blk], self.gc(G_ONE, 64), sq[:, 0:blk], True, True, [gconst, sq], [ps_])
                    self.act(lnb[:, h, t0:t0 + blk], ps_[:, 0:blk], AF.Ln, [ps_], [lnb], bias=self.eps_t[0:64, 0:1])
                    it += 1
            self.act(f2(lnb), f2(lnb), AF.Exp, [lnb], [lnb], scale=-0.5)
            self.stt(f2(t_), f2(t_), sc, f2(lnb), ALU.mult, ALU.mult, [t_, lnb], [t_])
        self.pop()

        if self.stop == 'gdn_pre':
            self.dma("sp", self.MO[0:256, :].rearrange("(m p) t -> p m t", p=128), vT[:], reads=[vT], writes=[self.MO])
            self.dma("sp", self.MO[256:512, :].rearrange("(h p) t -> p h t", p=64), kn[:], reads=[kn], writes=[self.MO])
            self.dma("sp", self.MO[512:768, :].rearrange("(h p) t -> p h t", p=64), qn[:], reads=[qn], writes=[self.MO])
            self.pop()
            return
        abT = self.sb('abT', [64, NCH, 16])
        self.dma("sp", abT[:], self.PT[:, PT_AB:PT_AB + 16].rearrange("(c p) n -> p c n", p=64),
                 reads=[self.PT], writes=[abT])
        par = self.sb('par', [64, 16])
        self.dma("sp", par[:, 0:8], self.inp['gdn_a_log'][l:l + 1].rearrange("o d h -> o (d h)").partition_broadcast(64), writes=[par])
        self.dma("sp", par[:, 8:16], self.inp['gdn_dt_bias'][l:l + 1].rearrange("o d h -> o (d h)").partition_broadcast(64), writes=[par])
        negA = self.sb('negA', [64, 8])
        self.act(negA[:], par[:, 0:8], AF.Exp, [par], [negA])
        self.ts("dve", negA[:], negA[:], -1.0, ALU.mult, [negA], [negA])
        gall = self.sb('gall', [64, NCH, 8])
        beta = self.sb('beta', [64, NCH, 8])
        ball = self.sb('ball', [64, NCH, 8])
        eb = self.sb('eb', [64, NCH, 8])
        ebeta = self.sb('ebeta', [64, NCH, 8])
        ekd = self.sb('ekd', [64, NCH, 8])
        etot = self.sb('etot', [64, NCH, 8])
        tot = self.sb('tot', [64, NCH, 8])
        self.push()
        pb = [self.ps(f'pb{i}', [64, NCH * 8]) for i in range(3)]
        self.tt("dve", gall[:], abT[:, :, 0:8], par[:, 8:16].unsqueeze(1).to_broadcast([64, NCH, 8]), ALU.add, [abT, par], [gall])
        self.act(gall[:], gall[:], AF.Exp, [gall], [gall])
        self.act(gall[:], gall[:], AF.Ln, [gall], [gall], bias=one_t[0:64, 0:1])
        self.tt("dve", gall[:], gall[:], negA[:].unsqueeze(1).to_broadcast([64, NCH, 8]), ALU.mult, [gall, negA], [gall])
        self.act(beta[:], abT[:, :, 8:16], AF.Sigmoid, [abT], [beta])
        g2 = gall[:].rearrange("p c n -> p (c n)")
        self.mm(pb[0][:], self.gc(G_L, 64), g2, True, True, [gconst, gall], [pb[0]])
        self.mm(pb[1][:], self.gc(G_U, 64), g2, True, True, [gconst, gall], [pb[1]])
        self.mm(pb[2][:], self.gc(G_ONE, 64), g2, True, True, [gconst, gall], [pb[2]])
        v3 = lambda p: p[:].rearrange("p (c n) -> p c n", n=8)
        self.copy("dve", ball[:, :, 0:4], v3(pb[0])[:, :, 0:4], [pb[0]], [ball])
        self.copy("dve", ball[:, :, 4:8], v3(pb[1])[:, :, 4:8], [pb[1]], [ball])
        self.copy("dve", tot[:], v3(pb[2]), [pb[2]], [tot])
        self.act(eb[:], ball[:], AF.Exp, [ball], [eb])
        self.tt("dve", ebeta[:], eb[:], beta[:], ALU.mult, [eb, beta], [ebeta])
        self.tt("dve", ekd[:], tot[:], ball[:], ALU.subtract, [tot, ball], [ekd])
        self.act(ekd[:], ekd[:], AF.Exp, [ekd], [ekd])
        self.act(etot[:], tot[:], AF.Exp, [tot], [etot])
        self.pop()
        gng = self.sb('gng', [64, 4, 64])
        self.dma("sp", gng[:, 0, :], self.inp['gdn_norm_g'][l:l + 1, :].partition_broadcast(64), writes=[gng])
        for h in range(1, 4):
            self.copy("dve", gng[:, h, :], gng[:, 0, :], [gng], [gng])

        if self.stop == 'gdn_scal':
            for i_, t_ in enumerate((gall, beta, ball, eb, ebeta, ekd, etot, tot)):
                self.dma("sp", self.MO[i_ * 64:(i_ + 1) * 64, 0:NCH * 8], t_[:].rearrange("p c n -> p (c n)"), reads=[t_], writes=[self.MO])
            self.pop()
            return
        bk = [[self.ps(f'bk{j}{n}', [128, 512]) for n in range(3)] for j in range(2)]
        pG = self.ps('pG', [128, 512]); pH = self.ps('pH', [128, 512])
        H0 = slice(0, 256); H1 = slice(256, 512)
        W = [64, 256]
        LT = []
        for j in range(2):
            LT.append(dict(
                GLt=self.sb(f'GLt{j}', W), Em=self.sb(f'Em{j}', W), EmT=self.sb(f'EmT{j}', W), tA=self.sb(f'tA{j}', W),
                X=[self.sb(f'X{j}{i}', W) for i in range(2)], XT=[self.sb(f'XT{j}{i}', W) for i in range(2)],
                Pm=[self.sb(f'Pm{j}{i}', W) for i in range(2)], vb=self.sb(f'vb{j}', W), kbe=self.sb(f'kbe{j}', W)))
        NOB = 4
        u_ = [self.sb(f'u{i}', W) for i in range(NOB)]
        wT_ = [self.sb(f'wT{i}', W) for i in range(NOB)]
        KQm_ = [self.sb(f'KQm{i}', W) for i in range(NOB)]
        kd_ = [self.sb(f'kd{i}', W) for i in range(NOB)]
        Sst = [self.sb(f'S{i}', W) for i in range(2)]
        St = self.sb('St', W)
        vnew = self.sb('vnew', W)
        o2sb = self.sb('o2sb', W); osb = [self.sb(f'osb{i}', W) for i in range(2)]
        ofl = [self.sb(f'ofl{i}', W) for i in range(2)]
        gat = [self.sb(f'gat{i}', W) for i in range(2)]
        rs4 = self.sb('rs4', [64, 8]); ysb = self.sb('ysb', W); sqo = self.sb('sqo', W)
        rob = [self.sb(f'rob{i}', [128, 2, 64]) for i in range(2)]

        def bc4(t, c, d):
            return t[:, c, d * 4:(d + 1) * 4].unsqueeze(2).to_broadcast([64, 4, 64])

        def v4(ap):
            return ap.rearrange("p (h n) -> p h n", h=4)

        def local(c, d, i, j):
            L = LT[j]
            GLt, Em, EmT, tA, X, XT, Pm, vb, kbe = (L[k] for k in ('GLt', 'Em', 'EmT', 'tA', 'X', 'XT', 'Pm', 'vb', 'kbe'))
            b0, b1, b2 = bk[j]
            cs = slice(c * 64, (c + 1) * 64)
            NMa, NMb = (G_NML, G_NMU) if d == 0 else (G_NMU, G_NML)
            SM = G_SML if d == 0 else G_SMU
            self.tt("dve", v4(GLt[:]), v4(self.gc(G_ID)), bc4(ball, c, d), ALU.mult, [gconst, ball], [GLt])
            for h in range(4):
                hs = slice(h * 64, (h + 1) * 64)
                hs1 = slice(256 + h * 64, 256 + (h + 1) * 64)
                self.mm(b1[0:64, hs], kn[:, h, cs], kn[:, h, cs], True, True, kn.rk(c), [b1])
                self.mm(b1[0:64, hs1], kn[:, h, cs], qn[:, h, cs], True, True, [kn.rk(c), qn.rk(c)], [b1])
                self.mm(b2[0:64, hs], kn[:, h, cs], self.ident[0:64, 0:64], True, True, [kn.rk(c), self.ident], [b2])
            for m in range(2):
                self.mm(b2[0:64, 256 + m * 128:256 + (m + 1) * 128], vT[:, m, cs], self.ident[:, :], True, True, [vT.rk(c), self.ident], [b2])
            yield
            for h in range(4):
                hs = slice(h * 64, (h + 1) * 64)
                self.mm(b0[0:64, hs], self.gc(G_ONE, 64), GLt[:, hs], True, True, [GLt, gconst], [b0])
            self.tt("dve", tA[:], b1[0:64, H0], self.gc(SM), ALU.mult, [b1, gconst], [tA])
            self.tt("pool", v4(vb[:]), v4(vb[:]), v4(vb[:]), ALU.add, [vb], [vb]) if False else None
            yield
            self.tt("dve", v4(Em[:]), bc4(ball, c, d), v4(b0[0:64, H0]), ALU.subtract, [b0, ball], [Em])
            self.tt("dve", v4(EmT[:]), v4(b0[0:64, H0]), bc4(ball, c, d), ALU.subtract, [b0, ball], [EmT])
            yield
            self.tt("pool", Em[:], Em[:], self.gc(NMa), ALU.add, [Em, gconst], [Em])
            self.tt("pool", EmT[:], EmT[:], self.gc(NMb), ALU.add, [EmT, gconst], [EmT])
            self.tt("dve", v4(tA[:]), v4(tA[:]), bc4(beta, c, d), ALU.mult, [tA, beta], [tA])
            yield
            self.act(Em[:], Em[:], AF.Exp, [Em], [Em])
            self.act(EmT[:], EmT[:], AF.Exp, [EmT], [EmT])
            self.tt("dve", v4(vb[:]), v4(b2[0:64, H1]), bc4(beta, c, d), ALU.mult, [b2, beta], [vb])
            self.tt("dve", v4(kbe[:]), v4(b2[0:64, H0]), bc4(ebeta, c, d), ALU.mult, [b2, ebeta], [kbe])
            self.tt("dve", v4(kd_[i][:]), v4(b2[0:64, H0]), bc4(ekd, c, d), ALU.mult, [b2, ekd], [kd_[i]])
            yield
            self.tt("pool", XT[0][:], tA[:], Em[:], ALU.mult, [tA, Em], [XT[0]])
            self.tt("dve", KQm_[i][:], b1[0:64, H1], EmT[:], ALU.mult, [b1, EmT], [KQm_[i]])
            yield
            for h in range(4):
                hs = slice(h * 64, (h + 1) * 64)
                self.mm(b1[0:64, 256 + h * 64:256 + (h + 1) * 64], XT[0][:, hs], self.ident[0:64, 0:64], True, True, [XT[0], self.ident], [b1])
            yield
            self.copy("act", X[0][:], b1[0:64, H1], [b1], [X[0]])
            yield
            self.tt("pool", Pm[0][:], X[0][:], self.gc(G_ID), ALU.add, [X[0], gconst], [Pm[0]])
            cur = 0
            pc = 0

            def p_update(xt_tile, pc):
                for h in range(4):
                    hs = slice(h * 64, (h + 1) * 64)
                    self.mm(b1[0:64, hs], xt_tile[:, hs], Pm[pc][:, hs], True, True, [xt_tile, Pm[pc]], [b1])
                return 1 - pc

            def p_add(pc_old):
                self.tt("dve", Pm[1 - pc_old][:], b1[0:64, H0], Pm[pc_old][:], ALU.add, [b1, Pm[pc_old]], [Pm[1 - pc_old]])

            for k in range(5):
                nxt = 1 - cur
                last = (k == 4)
                for h in range(4):
                    hs = slice(h * 64, (h + 1) * 64)
                    hs1 = slice(256 + h * 64, 256 + (h + 1) * 64)
                    if not last:
                        self.mm(b0[0:64, hs], XT[cur][:, hs], X[cur][:, hs], True, True, [XT[cur], X[cur]], [b0])
                    self.mm(b0[0:64, hs1], X[cur][:, hs], XT[cur][:, hs], True, True, [XT[cur], X[cur]], [b0])
                pc_old = pc
                if k >= 1:
                    pc = p_update(XT[cur], pc)
                yield
                if not last:
                    self.copy("act", X[nxt][:], b0[0:64, H0], [b0], [X[nxt]])
                self.copy("dve", XT[nxt][:], b0[0:64, H1], [b0], [XT[nxt]])
                if k >= 1:
                    p_add(pc_old)
                cur = nxt
                yield
            pc_old = pc
            pc = p_update(XT[cur], pc)
            yield
            p_add(pc_old)
            assert pc == 1
            TT = Pm[1]
            yield
            for h in range(4):
                hs = slice(h * 64, (h + 1) * 64)
                hs1 = slice(256 + h * 64, 256 + (h + 1) * 64)
                self.mm(b0[0:64, hs], TT[:, hs], vb[:, hs], True, True, [TT, vb], [b0])
                self.mm(b0[0:64, hs1], kbe[:, hs], TT[:, hs], True, True, [TT, kbe], [b0])
            yield
            self.copy("act", u_[i][:], b0[0:64, H0], [b0], [u_[i]])
            self.copy("dve", wT_[i][:], b0[0:64, H1], [b0], [wT_[i]])

        def seq(c, d, i, si, step):
            cs = slice(c * 64, (c + 1) * 64)
            Sc = Sst[si]; Sn = Sst[1 - si]
            for h in range(4):
                hs = slice(h * 64, (h + 1) * 64)
                self.mm(pG[0:64, hs], wT_[i][:, hs], Sc[:, hs], True, True, [wT_[i], Sc], [pG])
            self.tt("dve", vnew[:], u_[i][:], pG[0:64, H0], ALU.subtract, [u_[i], pG], [vnew])
            for h in range(4):
                hs = slice(h * 64, (h + 1) * 64)
                hs1 = slice(256 + h * 64, 256 + (h + 1) * 64)
                self.mm(pG[0:64, hs1], kd_[i][:, hs], vnew[:, hs], True, True, [kd_[i], vnew], [pG])
            self.tt("dve", v4(St[:]), v4(Sc[:]), bc4(etot, c, d), ALU.mult, [Sc, etot], [St])
            self.tt("dve", Sn[:], St[:], pG[0:64, H1], ALU.add, [St, pG], [Sn])
            for h in range(4):
                hs = slice(h * 64, (h + 1) * 64)
                hs1 = slice(256 + h * 64, 256 + (h + 1) * 64)
                self.mm(pH[0:64, hs], qn[:, h, cs], Sc[:, hs], True, True, [qn.rk(c), Sc], [pH])
                self.mm(pH[0:64, hs1], KQm_[i][:, hs], vnew[:, hs], True, True, [KQm_[i], vnew], [pH])
            ob = osb[step % 2]
            self.copy("act", o2sb[:], pH[0:64, H1], [pH], [o2sb])
            self.tt("dve", v4(ob[:]), v4(pH[0:64, H0]), bc4(eb, c, d), ALU.mult, [pH, eb], [ob])
            self.tt("pool", ob[:], ob[:], o2sb[:], ALU.add, [ob, o2sb], [ob])
            if d == 0:
                self.dma("sp", OF[cs, :], ob[:], reads=[ob], writes=OF.rk(c))
            else:
                of = ofl[step % 2]; ga = gat[step % 2]
                self.dma("sp", of[:], OF[cs, :], reads=OF.rk(c), writes=[of])
                self.dma("sp", ga[:], self.PT[cs, PT_GATE:PT_GATE + 256], reads=[self.PT], writes=[ga])
                self.tt("pool", ob[:], ob[:], of[:], ALU.add, [ob, of], [ob])
                self.tt("pool", sqo[:], ob[:], ob[:], ALU.mult, [ob], [sqo])
                self.op("dve", lambda e: e.tensor_reduce(out=rs4[:, 0:4], in_=v4(sqo[:]), axis=AX.X, op=ALU.add), [sqo], [rs4])
                self.act(rs4[:, 4:8], rs4[:, 0:4], AF.Sqrt, [rs4], [rs4], scale=1.0 / 64, bias=self.eps_t[0:64, 0:1])
                self.op("dve", lambda e: e.reciprocal(out=rs4[:, 0:4], in_=rs4[:, 4:8]), [rs4], [rs4])
                self.tt("dve", v4(ysb[:]), v4(ob[:]), rs4[:, 0:4].unsqueeze(2).to_broadcast([64, 4, 64]), ALU.mult, [ob, rs4], [ysb])
                self.tt("pool", ysb[:], ysb[:], gng[:].rearrange("p h n -> p (h n)"), ALU.mult, [ysb, gng], [ysb])
                self.act(ga[:], ga[:], AF.Silu, [ga], [ga])
                self.tt("dve", ysb[:], ysb[:], ga[:], ALU.mult, [ysb, ga], [ysb])
                for m in range(2):
                    self.mm(pH[:, m * 64:(m + 1) * 64], ysb[:, m * 128:(m + 1) * 128], self.ident[0:64, 0:64], True, True, [ysb, self.ident], [pH])
                rb_ = rob[step % 2]
                self.copy("act", rb_[:], pH[:, 0:128].rearrange("p (m t) -> p m t", m=2), [pH], [rb_])
                self.dma("sp", self.MO[0:256, cs].rearrange("(m p) t -> p m t", p=128), rb_[:], reads=[rb_], writes=[self.MO])

        def run_interleaved(gens, extra=()):
            gens = list(gens)
            extra = list(extra)
            rnd = 0
            while gens:
                for g_ in list(gens):
                    try:
                        next(g_)
                    except StopIteration:
                        gens.remove(g_)
                if extra and rnd in (1, 12):
                    extra.pop(0)()
                rnd += 1
            for f_ in extra:
                f_()

        for d in range(2):
            order = list(range(NCH)) if d == 0 else [3, 2, 1, 0] + list(range(NCH - 1, 3, -1))
            self.memset("pool", Sst[0][:], 0.0, [Sst[0]])
            n = len(order)
            run_interleaved([local(order[0], d, 0, 0), local(order[1], d, 1, 1)])
            for s0 in range(0, n, 2):
                gens = []
                for q_ in range(2):
                    s2 = s0 + 2 + q_
                    if s2 < n:
                        gens.append(local(order[s2], d, s2 % NOB, q_))
                extra = []
                for q_ in range(2):
                    s1 = s0 + q_
                    if s1 < n:
                        extra.append(lambda s1=s1: seq(order[s1], d, s1 % NOB, s1 % 2, s1))
                run_interleaved(gens, extra)
        self.pop()

    return dict(gc=gc, phase_gdn=phase_gdn)


for _k, _v in _gdn_methods().items():
    setattr(Kern, _k, _v)


def _lru_att_methods():
    def rev_ap(self, ap2d):
        n = ap2d.shape[1]
        last = ap2d[:, n - 1:n]
        return bass.AP(last.tensor, last.offset, [list(last.ap[0]), [-1, n]])

    def phase_lru(self, l):
        self.push()
        one_t = self.sb('one', [128, 1])
        self.memset("pool", one_t[:], 1.0, [one_t])
        xr = self.sb('xr', [128, 2, T])
        yg = self.sb('yg', [128, 2, T])
        A = self.sb('A', [128, 2, T])
        BX = self.sb('BX', [128, 2, T])
        H = [self.sb(f'H{i}', [128, 2, T]) for i in range(2)]
        cw = self.sb('cwl', [128, 2, 4])
        cb = self.sb('cbl', [128, 2])
        self.dma("sp", cw[:], self.inp['lru_conv_wT'][l].rearrange("(m p) k -> p m k", p=128), writes=[cw])
        for m in range(2):
            self.dma("sp", cb[:, m:m + 1], self.inp['lru_conv_b'][l, m * 128:(m + 1) * 128].rearrange("(p o) -> p o", o=1), writes=[cb])
        WB = self.sb('WB', [128, 8, 128])
        self.memset("pool", WB[:], 0.0, [WB])
        bri = self.sb('bri', [128, 8])
        lam = self.sb('lam', [128, 4])
        for d in range(2):
            for gi, (wn, bn) in enumerate((('lru_w_r', 'lru_b_r'), ('lru_w_i', 'lru_b_i'))):
                for m in range(2):
                    idx = (d * 2 + gi) * 2 + m
                    for hb in range(2):
                        self.dma("sp", WB[hb * 64:(hb + 1) * 64, idx, hb * 64:(hb + 1) * 64],
                                 self.inp[wn][l, d, 2 * m + hb], writes=[WB])
                    self.dma("sp", bri[:, idx:idx + 1],
                             self.inp[bn][l, d, m * 128:(m + 1) * 128].rearrange("(p o) -> p o", o=1), writes=[bri])
            for m in range(2):
                self.dma("sp", lam[:, d * 2 + m:d * 2 + m + 1],
                         self.inp['lru_lambda'][l, d, m * 128:(m + 1) * 128].rearrange("(p o) -> p o", o=1), writes=[lam])
        cch = self.sb('cch', [128, 4])
        self.act(cch[:], lam[:], AF.Exp, [lam], [cch], scale=-1.0)
        self.act(cch[:], cch[:], AF.Ln, [cch], [cch], bias=one_t[:, 0:1])
        self.ts("dve", cch[:], cch[:], -8.0, ALU.mult, [cch], [cch])
        self.push()
        raw = [self.sb(f'raw{i}', [128, 516]) for i in range(2)]
        t1 = self.sb('t1', [128, 512]); t2 = self.sb('t2', [128, 512])
        segs = [(0, T_CTX, [(0, 256)]), (T_CTX, T, [(T_CTX + i * 512, 512) for i in range(4)])]
        it = 0
        for m in range(2):
            row0 = PF_LRU + m * 128
            for (s0, s1, blks) in segs:
                for (t0, blk) in blks:
                    rw = raw[it % 2]
                    lo = max(t0 - 1, s0); hi = min(t0 + blk + 2, s1)
                    if lo > t0 - 1:
                        self.memset("pool", rw[:, 0:1], 0.0, [rw])
                    if hi < t0 + blk + 2:
                        self.memset("pool", rw[:, hi - (t0 - 1):blk + 3], 0.0, [rw])
                    self.dma("sp", rw[:, lo - (t0 - 1):hi - (t0 - 1)], self.PF[row0:row0 + 128, lo:hi], reads=[self.PF], writes=[rw])
                    dst = xr[:, m, t0:t0 + blk]
                    self.ts("dve", dst, rw[:, 0:blk], cw[:, m, 0:1], ALU.mult, [rw, cw, cb], [xr], s2=cb[:, m:m + 1], op1=ALU.add)
                    for k in range(1, 4):
                        self.stt(dst, rw[:, k:k + blk], cw[:, m, k:k + 1], dst, ALU.mult, ALU.add, [rw, cw, xr], [xr])
                    it += 1
                    rg = raw[it % 2]
                    self.dma("sp", rg[:, 0:blk], self.PF[row0 + 256:row0 + 384, t0:t0 + blk], reads=[self.PF], writes=[rg])
                    self.tt("pool", t1[:, 0:blk], rg[:, 0:blk], rg[:, 0:blk], ALU.mult, [rg], [t1])
                    self.ts("dve", t1[:, 0:blk], t1[:, 0:blk], 0.044715, ALU.mult, [t1], [t1], s2=1.0, op1=ALU.add)
                    self.tt("pool", t1[:, 0:blk], t1[:, 0:blk], rg[:, 0:blk], ALU.mult, [t1, rg], [t1])
                    self.act(t1[:, 0:blk], t1[:, 0:blk], AF.Tanh, [t1], [t1], scale=0.7978845608028654)
                    self.ts("dve", t2[:, 0:blk], rg[:, 0:blk], 0.5, ALU.mult, [rg], [t2])
                    self.stt(yg[:, m, t0:t0 + blk], t1[:, 0:blk], 1.0, t2[:, 0:blk], ALU.add, ALU.mult, [t1, t2], [yg])
                    it += 1
        self.pop()
        pr = [self.ps(f'pr{i}', [128, 512]) for i in range(2)]
        pi = [self.ps(f'pi{i}', [128, 512]) for i in range(2)]
        rt = self.sb('rt', [128, 512]); itl = self.sb('itl', [128, 512]); mt = self.sb('mt', [128, 512])
        blocks = [(0, 256)] + [(T_CTX + i * 512, 512) for i in range(4)]
        it = 0
        for d in range(2):
            for m in range(2):
                for (t0, blk) in blocks:
                    p1 = pr[it % 2]; p2 = pi[it % 2]
                    src = xr[:, m, t0:t0 + blk]
                    ir = (d * 2 + 0) * 2 + m; ii = (d * 2 + 1) * 2 + m
                    self.mm(p1[:, 0:blk], WB[:, ir, :], src, True, True, [WB, xr], [p1])
                    self.mm(p2[:, 0:blk], WB[:, ii, :], src, True, True, [WB, xr], [p2])
                    self.act(rt[:, 0:blk], p1[:, 0:blk], AF.Sigmoid, [p1, bri], [rt], bias=bri[:, ir:ir + 1])
                    self.act(itl[:, 0:blk], p2[:, 0:blk], AF.Sigmoid, [p2, bri], [itl], bias=bri[:, ii:ii + 1])
                    a_ = A[:, m, t0:t0 + blk]
                    self.act(a_, rt[:, 0:blk], AF.Exp, [rt, cch], [A], scale=cch[:, d * 2 + m:d * 2 + m + 1])
                    self.tt("pool", mt[:, 0:blk], a_, a_, ALU.mult, [A], [mt])
                    self.ts("dve", mt[:, 0:blk], mt[:, 0:blk], -1.0, ALU.mult, [mt], [mt], s2=1.0, op1=ALU.add)
                    self.act(mt[:, 0:blk], mt[:, 0:blk], AF.Sqrt, [mt], [mt])
                    self.tt("dve", mt[:, 0:blk], mt[:, 0:blk], itl[:, 0:blk], ALU.mult, [mt, itl], [mt])
                    self.tt("pool", BX[:, m, t0:t0 + blk], mt[:, 0:blk], src, ALU.mult, [mt, xr], [BX])
                    it += 1
                if d == 0:
                    self.op("dve", lambda e: e.tensor_tensor_scan(out=H[0][:, m, :], data0=A[:, m, :], data1=BX[:, m, :],
                                                                   initial=0.0, op0=ALU.mult, op1=ALU.add), [A, BX], [H[0]])
                else:
                    rv = self.rev_ap
                    self.op("dve", lambda e: e.tensor_tensor_scan(out=rv(H[1][:, m, 0:T_CTX]), data0=rv(A[:, m, 0:T_CTX]),
                                                                   data1=rv(BX[:, m, 0:T_CTX]), initial=0.0,
                                                                   op0=ALU.mult, op1=ALU.add), [A, BX], [H[1]])
                    self.op("dve", lambda e: e.tensor_tensor_scan(out=rv(H[1][:, m, T_CTX:T]), data0=rv(A[:, m, T_CTX:T]),
                                                                   data1=rv(BX[:, m, T_CTX:T]), initial=H[1][:, m, 0:1],
                                                                   op0=ALU.mult, op1=ALU.add), [A, BX, H[1]], [H[1]])
        for m in range(2):
            self.tt("pool", H[0][:, m, :], H[0][:, m, :], H[1][:, m, :], ALU.add, [H[0], H[1]], [H[0]])
            self.tt("dve", H[0][:, m, :], H[0][:, m, :], yg[:, m, :], ALU.mult, [H[0], yg], [H[0]])
        self.dma("sp", self.MO[256:512, :].rearrange("(m p) t -> p m t", p=128), H[0][:], reads=[H[0]], writes=[self.MO])
        self.pop()

    def phase_att(self, l):
        self.push()
        qT = self.sb('qT', [64, 8, T], F32R)
        kT = self.sb('kT', [64, 2, T], F32R)
        V = self.sb('V', [128, NT, 2, 65], F32R)
        onesf = self.sb('onesf', [128, NT * 2])
        self.memset("pool", onesf[:], 1.0, [onesf])
        self.copy("dve", V[:, :, :, 64], onesf[:].rearrange("p (t g) -> p t g", g=2), [onesf], [V])
        gbc = self.sb('gbc', [128, 10, 64])
        self.dma("sp", gbc[:, 0, :], self.inp['attn_q_norm_g'][l:l + 1, :].partition_broadcast(128), writes=[gbc])
        self.dma("sp", gbc[:, 8, :], self.inp['attn_k_norm_g'][l:l + 1, :].partition_broadcast(128), writes=[gbc])
        negc = self.sb('negc', [128, 4])
        self.op("dve", lambda e: e.tensor_reduce(out=negc[:, 0:1], in_=gbc[:, 0, :], axis=AX.X, op=ALU.max, apply_absolute_value=True), [gbc], [negc])
        self.op("dve", lambda e: e.tensor_reduce(out=negc[:, 1:2], in_=gbc[:, 8, :], axis=AX.X, op=ALU.max, apply_absolute_value=True), [gbc], [negc])
        self.tt("dve", negc[:, 2:3], negc[:, 0:1], negc[:, 1:2], ALU.mult, [negc], [negc])
        self.ts("dve", negc[:, 3:4], negc[:, 2:3], -8.0, ALU.mult, [negc], [negc])
        self.ts("dve", gbc[:, 0, :], gbc[:, 0, :], 0.125, ALU.mult, [gbc], [gbc])
        for h in range(1, 8):
            self.copy("dve", gbc[:, h, :], gbc[:, 0, :], [gbc], [gbc])
        self.copy("dve", gbc[:, 9, :], gbc[:, 8, :], [gbc], [gbc])
        self.push()
        at = [self.sb(f'at{i}', [128, 768]) for i in range(2)]
        an = [self.sb(f'an{i}', [128, 640]) for i in range(2)]
        sqa = self.sb('sqa', [128, 640])
        ssa = self.sb('ssa', [128, 20])
        cs_t = [self.sb(f'cs{i}', [128, 64]) for i in range(2)]
        r1 = self.sb('r1', [128, 10, 2, 16]); r2 = self.sb('r2', [128, 10, 2, 16])
        r3 = self.sb('r3', [128, 10, 2, 16]); r4 = self.sb('r4', [128, 10, 2, 16])
        ptq = [self.ps(f'ptq{i}', [128, 512]) for i in range(3)]
        for tt in range(NT):
            a_ = at[tt % 2]; n_ = an[tt % 2]
            self.dma("sp", a_[:], self.PT[tt * 128:(tt + 1) * 128, PT_ATT:PT_ATT + 768], reads=self.PT.rk(tt), writes=[a_])
            self.tt("pool", sqa[:], a_[:, 0:640], a_[:, 0:640], ALU.mult, [a_], [sqa])
            self.op("dve", lambda e: e.tensor_reduce(out=ssa[:, 0:10], in_=sqa[:].rearrange("p (h n) -> p h n", h=10), axis=AX.X, op=ALU.add), [sqa], [ssa])
            self.act(ssa[:, 10:20], ssa[:, 0:10], AF.Sqrt, [ssa], [ssa], scale=1.0 / 64, bias=self.eps_t[:, 0:1])
            self.op("dve", lambda e: e.reciprocal(out=ssa[:, 0:10], in_=ssa[:, 10:20]), [ssa], [ssa])
            n3 = n_[:].rearrange("p (h n) -> p h n", h=10)
            self.tt("dve", n3, a_[:, 0:640].rearrange("p (h n) -> p h n", h=10), ssa[:, 0:10].unsqueeze(2).to_broadcast([128, 10, 64]), ALU.mult, [a_, ssa], [n_])
            self.tt("pool", n_[:], n_[:], gbc[:].rearrange("p h n -> p (h n)"), ALU.mult, [n_, gbc], [n_])
            if tt >= 2:
                c_ = cs_t[tt % 2]
                lt = tt - 2
                self.dma("sp", c_[:], self.inp['rope_cs'][lt * 128:(lt + 1) * 128, :], writes=[c_])
                n5 = n_[:].rearrange("p (h a f n) -> p h a f n", h=10, a=2, f=2)
                x1 = n5[:, :, :, 0, :]; x2 = n5[:, :, :, 1, :]
                cosb = c_[:, 0:32].rearrange("p (a n) -> p a n", a=2).unsqueeze(1).to_broadcast([128, 10, 2, 16])
                sinb = c_[:, 32:64].rearrange("p (a n) -> p a n", a=2).unsqueeze(1).to_broadcast([128, 10, 2, 16])
                self.tt("dve", r1[:], x1, cosb, ALU.mult, [n_, c_], [r1])
                self.tt("pool", r2[:], x2, sinb, ALU.mult, [n_, c_], [r2])
                self.tt("dve", r3[:], x1, sinb, ALU.mult, [n_, c_], [r3])
                self.tt("pool", r4[:], x2, cosb, ALU.mult, [n_, c_], [r4])
                self.tt("dve", x1, r1[:], r2[:], ALU.subtract, [r1, r2], [n_])
                self.tt("pool", x2, r3[:], r4[:], ALU.add, [r3, r4], [n_])
            ts_ = slice(tt * 128, (tt + 1) * 128)
            for grp in range(3):
                p = ptq[grp]
                hs = range(4) if grp < 2 else range(2)
                for j in hs:
                    h = grp * 4 + j
                    self.mm(p[0:64, j * 128:(j + 1) * 128], n_[:, h * 64:(h + 1) * 64], self.ident[:, :], True, True, [n_, self.ident], [p])
                if grp < 2:
                    self.evac(grp, qT[:, grp * 4:(grp + 1) * 4, ts_], p[0:64, :].rearrange("p (h t) -> p h t", h=4), [p], [qT])
                else:
                    self.evac(grp, kT[:, :, ts_], p[0:64, 0:256].rearrange("p (h t) -> p h t", h=2), [p], [kT])
            self.copy("dve", V[:, tt, :, 0:64], a_[:, 640:768].rearrange("p (g n) -> p g n", g=2), [a_], [V])
        self.pop()
        pS = [self.ps(f'pS{i}', [128, 512]) for i in range(2)]
        pO = [self.ps(f'pO{i}', [128, 512]) for i in range(2)]
        pBc = self.ps('pBc', [128, 512])
        ones64 = self.sb('ones64', [128, 64])
        self.memset("pool", ones64[:], 1.0, [ones64])
        Pt = [self.sb(f'Pt{i}', [128, 512], F32R) for i in range(3)]
        rsb = self.sb('rsb', [128, 512]); bcs = self.sb('bcs', [64, 512])
        osg = [self.sb(f'osg{i}', [64, 512]) for i in range(2)]
        qblocks = [(0, 256, [0, 1])] + [(T_CTX + i * 512, 512, list(range(NT))) for i in range(4)]
        ip = 0; io = 0
        for (q0, qn_, ktiles) in qblocks:
            for h in range(8):
                g = h // 4
                po = pO[io % 2]
                pend = None
                nk = len(ktiles)
                for ki, kt in enumerate(ktiles):
                    ps_ = pS[ip % 2]; pt = Pt[ip % 3]
                    self.mm(ps_[:, 0:qn_], kT[:, g, kt * 128:(kt + 1) * 128], qT[:, h, q0:q0 + qn_], True, True, [kT, qT], [ps_])
                    self.act(pt[:, 0:qn_], ps_[:, 0:qn_], AF.Exp, [ps_, negc], [pt], bias=negc[:, 3:4])
                    if pend is not None:
                        pkt, ppt, pki = pend
                        self.mm(po[0:65, 0:qn_], V[:, pkt, g, :], ppt[:, 0:qn_], pki == 0, pki == nk - 1, [V, ppt], [po])
                    pend = (kt, pt, ki)
                    ip += 1
                pkt, ppt, pki = pend
                self.mm(po[0:65, 0:qn_], V[:, pkt, g, :], ppt[:, 0:qn_], pki == 0, pki == nk - 1, [V, ppt], [po])
                self.op("dve", lambda e: e.reciprocal(out=rsb[64:65, 0:qn_], in_=po[64:65, 0:qn_]), [po], [rsb])
                self.mm(pBc[0:64, 0:qn_], onesf[64:65, 0:64 - 28] if False else ones64[64:65, 0:64], rsb[64:65, 0:qn_], True, True, [ones64, rsb], [pBc])
                self.copy("act", bcs[:, 0:qn_], pBc[0:64, 0:qn_], [pBc], [bcs])
                og = osg[io % 2]
                self.tt("dve", og[:, 0:qn_], po[0:64, 0:qn_], bcs[:, 0:qn_], ALU.mult, [po, bcs], [og])
                self.dma("sp", self.MO[512 + h * 64:512 + (h + 1) * 64, q0:q0 + qn_], og[:, 0:qn_], reads=[og], writes=[self.MO])
                io += 1
        self.pop()

    return dict(rev_ap=rev_ap, phase_lru=phase_lru, phase_att=phase_att)


for _k, _v in _lru_att_methods().items():
    setattr(Kern, _k, _v)


N_OV = 36
N_SLOT = (64 + N_OV) * 128


def _tail_methods():
    def phase_wout(self, l):
        self.push()
        wo = self.sb('wo', [128, 8, D], F32R)
        self.dma("pool", wo[:], self.inp['w_out'][l].rearrange("(kc p) n -> p kc n", p=128), writes=[wo])
        g1 = [self.sb(f'g1{r}', [128, D]) for r in range(2)]
        for r in range(2):
            self.load_mod(g1[r], 2, r)
        mo = [self.sb(f'mo{i}', [128, 8, 512], F32R) for i in range(2)]
        xt = [self.sb(f'xw{i}', [128, D]) for i in range(2)]
        tw = [self.sb(f'tw{i}', [128, D]) for i in range(2)]
        pw = [self.ps(f'pw{i}', [128, 512]) for i in range(4)]
        blocks = [(0, 2), (2, 4), (6, 4), (10, 4), (14, 4)]
        msrc = self.MO[:, :].rearrange("(kc p) t -> p kc t", p=128)
        it = 0
        for bi, (t0, ntile) in enumerate(blocks):
            r = 1 if t0 == 0 else 0
            m_ = mo[bi % 2]
            ntok = ntile * 128
            self.dma("pool", m_[:, :, 0:ntok], msrc[:, :, t0 * 128:t0 * 128 + ntok], reads=[self.MO], writes=[m_])
            for j in range(ntile):
                tt = t0 + j
                x_ = xt[it % 2]; t_ = tw[it % 2]
                self.dma("sp", x_[:], self.XS[tt * 128:(tt + 1) * 128, :], reads=self.XS.rk(tt), writes=[x_])
                for half in range(2):
                    p = pw[(it % 2) * 2 + half]
                    for kc in range(8):
                        self.mm(p[:], m_[:, kc, j * 128:(j + 1) * 128], wo[:, kc, half * 512:(half + 1) * 512], kc == 0, kc == 7, [m_, wo], [p])
                    hs = slice(half * 512, (half + 1) * 512)
                    self.tt("dve", t_[:, hs], p[:], g1[r][:, hs], ALU.mult, [p, g1[r]], [t_])
                self.tt("pool", x_[:], x_[:], t_[:], ALU.add, [x_, t_], [x_])
                self.dma("sp", self.XS[tt * 128:(tt + 1) * 128, :], x_[:], reads=[x_], writes=self.XS.rk(tt))
                it += 1
        self.pop()

    def phase_moe(self, l):
        self.push()
        mc = self.sb('mc', [128, 256])
        self.dma("sp", mc[:], self.inp['moe_const'][:, :], writes=[mc])
        iota64 = mc[:, 0:64]; thr = mc[:, 64:100]; bvals = mc[:, 100:136]; pidx = mc[:, 136:137]; ones64 = mc[:, 137:201]
        ustr = self.sb('ustr', [128, 256])
        self.dma("sp", ustr[:], self.inp['moe_tri'][:, :], writes=[ustr])
        OH = self.sb('OH', [128, NT, 2, 64])
        gates = self.sb('gates', [128, NT, 2])
        dest = self.sb('dest', [128, NT, 2], I32)
        wix = self.sb('wix', [128, 2, N_OV], I32)
        XB, YB = self.XB, self.YB
        self.push()
        gm = [self.sb(f'gm2{r}', [128, D]) for r in range(2)]
        sh = [self.sb(f'sh2{r}', [128, D]) for r in range(2)]
        self.push()
        for r in range(2):
            self.load_mod(gm[r], 4, r, 'norm2_g', l)
            self.load_mod(sh[r], 3, r)
        self.pop()
        hbuf = self.sb('hbuf', [128, NT, D], nsub=NT)
        wr = self.sb('wr', [128, 8, 72])
        self.dma("sp", wr[:, :, 0:8], self.inp['moe_w_group'][l].rearrange("(kc p) n -> p kc n", p=128), writes=[wr])
        self.dma("sp", wr[:, :, 8:72], self.inp['moe_w_expert'][l].rearrange("(kc p) n -> p kc n", p=128), writes=[wr])
        rb = self.sb('rb', [128, 72])
        self.dma("sp", rb[:, 0:8], self.inp['moe_b_group'][l:l + 1, :].partition_broadcast(128), writes=[rb])
        self.dma("sp", rb[:, 8:72], self.inp['moe_b_expert'][l:l + 1, :].partition_broadcast(128), writes=[rb])
        xt = [self.sb(f'xm{i}', [128, D]) for i in range(2)]
        junk = self.sb('junk', [128, D])
        ss = [self.sb(f'ssm{i}', [128, 4]) for i in range(2)]
        hT = self.sb('hT', [128, 8, 128])
        ptp = [self.ps(f'ptp{i}', [128, 512]) for i in range(2)]
        plg = self.ps('plg', [128, 72])
        prk = self.ps('prk', [128, 64]); pcs = self.ps('pcs', [128, 64])
        lg = self.sb('lg', [128, 72]); sm = self.sb('sm', [128, 16])
        ohg = self.sb('ohg', [128, 8]); tmp64 = self.sb('tmp64', [128, 64]); le = self.sb('le', [128, 8]); le2 = self.sb('le2', [128, 8])
        oh1 = self.sb('oh1', [128, 8]); oh2 = self.sb('oh2', [128, 8]); eg = self.sb('eg', [128, 8])
        for tt in range(NT):
            r = 1 if tt < 2 else 0
            x_ = xt[tt % 2]; ss_ = ss[tt % 2]
            self.dma("sp", x_[:], self.XS[tt * 128:(tt + 1) * 128, :], reads=self.XS.rk(tt), writes=[x_])
            hcur = Tl(hbuf[:, tt, :], 'hcur'); hcur.rs = hbuf.rk(tt)
            self.norm_tile(x_, gm[r], sh[r], hcur, junk, ss_)
            for half in range(2):
                p = ptp[half]
                for q in range(4):
                    kc = half * 4 + q
                    self.tr(p[:, q * 128:(q + 1) * 128], hbuf[:, tt, kc * 128:(kc + 1) * 128], self.ident[:], [hbuf.rk(tt), self.ident], [p])
                self.evac(half, hT[:, half * 4:(half + 1) * 4, :], p[:].rearrange("p (q t) -> p q t", q=4), [p], [hT])
            for kc in range(8):
                self.mm(plg[:], hT[:, kc, :], wr[:, kc, :], kc == 0, kc == 7, [hT, wr], [plg])
            self.tt("dve", lg[:], plg[:], rb[:], ALU.add, [plg, rb], [lg])
            self.op("dve", lambda e: e.tensor_reduce(out=sm[:, 0:1], in_=lg[:, 0:8], axis=AX.X, op=ALU.max), [lg], [sm])
            self.ts("dve", ohg[:], lg[:, 0:8], sm[:, 0:1], ALU.is_equal, [lg, sm], [ohg])
            self.ts("dve", sm[:, 1:2], sm[:, 0:1], -1.0, ALU.mult, [sm], [sm])
            self.act(eg[:], lg[:, 0:8], AF.Exp, [lg, sm], [eg, sm], bias=sm[:, 1:2], accum_out=sm[:, 2:3])
            self.op("dve", lambda e: e.reciprocal(out=sm[:, 3:4], in_=sm[:, 2:3]), [sm], [sm])
            self.tt("dve", tmp64[:].rearrange("p (g e) -> p g e", g=8), lg[:, 8:72].rearrange("p (g e) -> p g e", g=8),
                    ohg[:].unsqueeze(2).to_broadcast([128, 8, 8]), ALU.mult, [lg, ohg], [tmp64])
            self.op("dve", lambda e: e.tensor_reduce(out=le[:], in_=tmp64[:].rearrange("p (g e) -> p e g", g=8), axis=AX.X, op=ALU.add), [tmp64], [le])
            self.op("dve", lambda e: e.tensor_reduce(out=sm[:, 4:5], in_=le[:], axis=AX.X, op=ALU.max), [le], [sm])
            self.ts("dve", oh1[:], le[:], sm[:, 4:5], ALU.is_equal, [le, sm], [oh1])
            self.stt(le2[:], oh1[:], -1.0e30, le[:], ALU.mult, ALU.add, [oh1, le], [le2])
            self.op("dve", lambda e: e.tensor_reduce(out=sm[:, 5:6], in_=le2[:], axis=AX.X, op=ALU.max), [le2], [sm])
            self.ts("dve", oh2[:], le2[:], sm[:, 5:6], ALU.is_equal, [le2, sm], [oh2])
            self.tt("dve", sm[:, 6:7], sm[:, 5:6], sm[:, 4:5], ALU.subtract, [sm], [sm])
            self.act(sm[:, 7:8], sm[:, 6:7], AF.Exp, [sm], [sm])
            self.ts("dve", sm[:, 8:9], sm[:, 7:8], 1.0, ALU.add, [sm], [sm])
            self.op("dve", lambda e: e.reciprocal(out=sm[:, 9:10], in_=sm[:, 8:9]), [sm], [sm])
            self.tt("dve", gates[:, tt, 0:1], sm[:, 3:4], sm[:, 9:10], ALU.mult, [sm], [gates])
            self.tt("dve", gates[:, tt, 1:2], gates[:, tt, 0:1], sm[:, 7:8], ALU.mult, [sm, gates], [gates])
            for k, ohk in enumerate((oh1, oh2)):
                self.tt("dve", OH[:, tt, k, :].rearrange("p (g e) -> p g e", g=8), ohg[:].unsqueeze(2).to_broadcast([128, 8, 8]),
                        ohk[:].unsqueeze(1).to_broadcast([128, 8, 8]), ALU.mult, [ohg, ohk], [OH])
        Osum = self.sb('Osum', [128, NT, 64])
        self.tt("dve", Osum[:], OH[:, :, 0, :], OH[:, :, 1, :], ALU.add, [OH], [Osum])
        rank = self.sb('rank', [128, NT, 64])
        pref = self.sb('pref', [128, 64])
        self.memset("pool", pref[:], 0.0, [pref])
        for tt in range(NT):
            self.mm(prk[:], ustr[:, 0:128], Osum[:, tt, :], True, True, [ustr, Osum], [prk])
            self.mm(pcs[:], ustr[:, 128:256], Osum[:, tt, :], True, True, [ustr, Osum], [pcs])
            self.tt("dve", rank[:, tt, :], prk[:], pref[:], ALU.add, [prk, pref], [rank])
            self.tt("dve", pref[:], pcs[:], pref[:], ALU.add, [pcs, pref], [pref])
        cmpb = self.sb('cmpb', [128, 64, 36])
        nblk = self.sb('nblk', [128, 64]); ovf = self.sb('ovf', [128, 64]); incl = self.sb('incl', [128, 64]); delta = self.sb('delta', [128, 64])
        self.tt("dve", cmpb[:], pref[:].unsqueeze(2).to_broadcast([128, 64, 36]), thr.unsqueeze(1).to_broadcast([128, 64, 36]), ALU.is_gt, [pref, mc], [cmpb])
        self.op("dve", lambda e: e.tensor_reduce(out=nblk[:], in_=cmpb[:], axis=AX.X, op=ALU.add), [cmpb], [nblk])
        self.ts("dve", ovf[:], nblk[:], -1.0, ALU.add, [nblk], [ovf], s2=0.0, op1=ALU.max)
        self.op("dve", lambda e: e.tensor_tensor_scan(out=incl[:], data0=ones64, data1=ovf[:], initial=0.0, op0=ALU.mult, op1=ALU.add), [mc, ovf], [incl])
        self.tt("dve", delta[:], incl[:], ovf[:], ALU.subtract, [incl, ovf], [delta])
        self.tt("dve", delta[:], delta[:], iota64, ALU.subtract, [delta, mc], [delta])
        self.ts("dve", delta[:], delta[:], 128.0, ALU.mult, [delta], [delta], s2=8064.0, op1=ALU.add)
        base1 = self.sb('base1', [128, 64])
        self.ts("dve", base1[:], iota64, 128.0, ALU.mult, [mc], [base1])
        dall = self.sb('dall', [128, NT, 64]); ge = self.sb('ge', [128, NT, 64]); destf = self.sb('destf', [128, NT, 2])
        self.ts("dve", ge[:], rank[:], 128.0, ALU.is_ge, [rank], [ge])
        self.tt("dve", ge[:], ge[:], delta[:].unsqueeze(1).to_broadcast([128, NT, 64]), ALU.mult, [ge, delta], [ge])
        self.tt("dve", dall[:], rank[:], base1[:].unsqueeze(1).to_broadcast([128, NT, 64]), ALU.add, [rank, base1], [dall])
        self.tt("dve", dall[:], dall[:], ge[:], ALU.add, [dall, ge], [dall])
        for k in range(2):
            self.tt("dve", ge[:], OH[:, :, k, :], dall[:], ALU.mult, [OH, dall], [ge])
            self.op("dve", lambda e: e.tensor_reduce(out=destf[:, :, k], in_=ge[:], axis=AX.X, op=ALU.add), [ge], [destf])
        self.copy("dve", dest[:], destf[:], [destf], [dest])
        cmp2 = self.sb('cmp2', [128, 36, 64]); bke = self.sb('bke', [128, 36]); wixf = self.sb('wixf', [128, 2, 36])
        self.tt("dve", cmp2[:], incl[:].unsqueeze(1).to_broadcast([128, 36, 64]), bvals.unsqueeze(2).to_broadcast([128, 36, 64]), ALU.is_le, [incl, mc], [cmp2])
        self.op("dve", lambda e: e.tensor_reduce(out=bke[:], in_=cmp2[:], axis=AX.X, op=ALU.add), [cmp2], [bke])
        p2 = self.sb('p2', [128, 1])
        self.ts("dve", p2[:], pidx, 2.0, ALU.mult, [mc], [p2])
        self.ts("dve", wixf[:, 0, :], bke[:], 256.0, ALU.mult, [bke, p2], [wixf], s2=p2[:, 0:1], op1=ALU.add)
        self.ts("dve", wixf[:, 1, :], bke[:], 256.0, ALU.mult, [bke, p2], [wixf], s2=p2[:, 0:1], op1=ALU.add)
        self.copy("dve", wix[:], wixf[:], [wixf], [wix])
        for tt in range(NT):
            for k in range(2):
                self.S.dma("pool", lambda e, tt=tt, k=k: e.indirect_dma_start(
                    out=XB[:, :], out_offset=bass.IndirectOffsetOnAxis(ap=dest[:, tt, k:k + 1], axis=0),
                    in_=hbuf[:, tt, :], in_offset=None), _flat([hbuf.rk(tt), dest]), _flat([XB]))
        self.pop()
        self.push()
        NWB = 3
        w1b = [self.sb(f'w1b{i}', [128, 8, 512], F32R) for i in range(NWB)]
        w3b = [self.sb(f'w3b{i}', [128, 8, 512], F32R) for i in range(NWB)]
        w2b = [self.sb(f'w2b{i}', [128, 4, D], F32R) for i in range(NWB)]
        xb = [self.sb(f'xb{i}', [128, D]) for i in range(2)]
        xbT = self.sb('xbT', [128, 8, 128], F32R)
        a1 = self.sb('a1', [128, 512]); hh = self.sb('hh', [128, 512])
        hhT = self.sb('hhT', [128, 4, 128], F32R)
        yb = [self.sb(f'yb{i}', [128, D]) for i in range(2)]
        ptp = [self.ps(f'ptq{i}', [128, 512]) for i in range(2)]
        ph1 = self.ps('ph1', [128, 512]); ph3 = self.ps('ph3', [128, 512]); pht = self.ps('pht', [128, 512])
        py = [self.ps(f'py{i}', [128, 512]) for i in range(2)]
        w1v = self.inp['moe_w1'].rearrange("l e (r kc) n -> (l e r) (kc n)", kc=4)
        w3v = self.inp['moe_w3'].rearrange("l e (r kc) n -> (l e r) (kc n)", kc=4)
        w2v = self.inp['moe_w2'].rearrange("l e (r c) n -> (l e r) (c n)", c=2)
        loff = l * 64 * 1024 * 512
        if not hasattr(self, 'reg_b13'):
            self.reg_b13 = self.nc.gpsimd.to_reg(16383)
            self.reg_b2 = self.nc.gpsimd.to_reg(32767)
        reg_b13, reg_b2 = self.reg_b13, self.reg_b2
        order = []
        nxt2 = 0
        for e_ in range(64):
            order.append(e_)
            while nxt2 < N_OV and (nxt2 + 1) * 64 <= (e_ + 1) * N_OV:
                order.append(64 + nxt2)
                nxt2 += 1
        assert len(order) == 64 + N_OV and nxt2 == N_OV
        def load_w(pos, blk):
            iw = pos % NWB
            W1, W3, W2 = w1b[iw], w3b[iw], w2b[iw]
            if blk < 64:
                self.dma("pool", W1[:], self.inp['moe_w1'][l, blk].rearrange("(p kc) n -> p kc n", kc=8), writes=[W1], max_dma_last_dim=8192)
                self.dma("pool", W3[:], self.inp['moe_w3'][l, blk].rearrange("(p kc) n -> p kc n", kc=8), writes=[W3], max_dma_last_dim=8192)
                self.dma("pool", W2[:], self.inp['moe_w2'][l, blk].rearrange("(p kc) n -> p kc n", kc=4), writes=[W2], max_dma_last_dim=8192)
            else:
                b = blk - 64
                for (Wt, wv) in ((W1, w1v), (W3, w3v), (W2, w2v)):
                    w2d = Wt[:].rearrange("p k n -> p (k n)")
                    for half in range(2):
                        self.S.dma("pool", lambda e, w2d=w2d, wv=wv, half=half, b=b: e.indirect_dma_start(
                            out=w2d[:, half * 2048:(half + 1) * 2048], out_offset=None, in_=wv[:, :],
                            in_offset=bass.IndirectOffsetOnAxis(ap=wix[:, 0, b:b + 1], axis=0),
                            element_offset=loff + half * 2048, bounds_check=reg_b13, oob_is_err=False), _flat([wix]), _flat([Wt]))

        def stage1(pos, blk):
            x_ = xb[pos % 2]
            self.dma("sp", x_[:], XB[blk * 128:(blk + 1) * 128, :], reads=[XB], writes=[x_])
            for half in range(2):
                p = ptp[half]
                for q in range(4):
                    kc = half * 4 + q
                    self.tr(p[:, q * 128:(q + 1) * 128], x_[:, kc:D:8], self.ident[:], [x_, self.ident], [p])
                self.evac(half, xbT[:, half * 4:(half + 1) * 4, :], p[:].rearrange("p (q t) -> p q t", q=4), [p], [xbT])

        def stage2(pos, blk):
            iw = pos % NWB
            W1, W3 = w1b[iw], w3b[iw]
            for kc in range(8):
                self.mm(ph1[:], xbT[:, kc, :], W1[:, kc, :], kc == 0, kc == 7, [xbT, W1], [ph1])
            for kc in range(8):
                self.mm(ph3[:], xbT[:, kc, :], W3[:, kc, :], kc == 0, kc == 7, [xbT, W3], [ph3])
            self.act(a1[:], ph1[:], AF.Silu, [ph1], [a1])
            self.tt("dve", hh[:], a1[:], ph3[:], ALU.mult, [a1, ph3], [hh])

        def stage34(pos, blk):
            iw = pos % NWB
            W2 = w2b[iw]
            for q in range(4):
                self.tr(pht[:, q * 128:(q + 1) * 128], hh[:, q:512:4], self.ident[:], [hh, self.ident], [pht])
            self.copy("act", hhT[:], pht[:].rearrange("p (q t) -> p q t", q=4), [pht], [hhT])
            y_ = yb[pos % 2]
            for half in range(2):
                p = py[half]
                for c in range(4):
                    self.mm(p[:], hhT[:, c, :], W2[:, c, half * 512:(half + 1) * 512], c == 0, c == 3, [hhT, W2], [p])
                self.evac(half, y_[:, half * 512:(half + 1) * 512], p[:], [p], [y_])
            self.dma("sp", YB[blk * 128:(blk + 1) * 128, :], y_[:], reads=[y_], writes=[YB])

        nb = len(order)
        load_w(0, order[0])
        if nb > 1:
            load_w(1, order[1])
        stage1(0, order[0])
        for pos, blk in enumerate(order):
            if pos + 2 < nb:
                load_w(pos + 2, order[pos + 2])
            stage2(pos, blk)
            if pos + 1 < nb:
                stage1(pos + 1, order[pos + 1])
            stage34(pos, blk)
        self.pop()
        self.push()
        g2 = [self.sb(f'g2{r}', [128, D]) for r in range(2)]
        for r in range(2):
            self.load_mod(g2[r], 5, r)
        Y0 = [self.sb(f'Y0{i}', [128, D]) for i in range(2)]
        Y1 = [self.sb(f'Y1{i}', [128, D]) for i in range(2)]
        xt = [self.sb(f'xf{i}', [128, D]) for i in range(2)]
        for tt in range(NT):
            r = 1 if tt < 2 else 0
            i = tt % 2
            for k, Yk in enumerate((Y0[i], Y1[i])):
                self.S.dma("pool", lambda e, Yk=Yk, tt=tt, k=k: e.indirect_dma_start(
                    out=Yk[:, :], out_offset=None, in_=YB[:, :],
                    in_offset=bass.IndirectOffsetOnAxis(ap=dest[:, tt, k:k + 1], axis=0)), _flat([YB, dest]), _flat([Yk]))
            x_ = xt[i]
            self.dma("sp", x_[:], self.XS[tt * 128:(tt + 1) * 128, :], reads=self.XS.rk(tt), writes=[x_])
            self.ts("dve", Y0[i][:], Y0[i][:], gates[:, tt, 0:1], ALU.mult, [Y0[i], gates], [Y0[i]])
            self.stt(Y0[i][:], Y1[i][:], gates[:, tt, 1:2], Y0[i][:], ALU.mult, ALU.add, [Y1[i], Y0[i], gates], [Y0[i]])
            self.tt("pool", Y0[i][:], Y0[i][:], g2[r][:], ALU.mult, [Y0[i], g2[r]], [Y0[i]])
            self.tt("dve", x_[:], x_[:], Y0[i][:], ALU.add, [x_, Y0[i]], [x_])
            self.dma("sp", self.XS[tt * 128:(tt + 1) * 128, :], x_[:], reads=[x_], writes=self.XS.rk(tt))
        self.pop()
        self.pop()

    def phase_final(self):
        self.push()
        fg = self.sb('fg', [128, D])
        self.dma("sp", fg[:], self.inp['final_norm_g'][0:1, :].partition_broadcast(128), writes=[fg])
        xt = [self.sb(f'xo{i}', [128, D]) for i in range(2)]
        xn = [self.sb(f'xq{i}', [128, D]) for i in range(2)]
        junk = self.sb('junk', [128, D])
        ss = [self.sb(f'sso{i}', [128, 4]) for i in range(2)]
        evs = []
        for tt in range(2, NT):
            i = tt % 2
            self.dma("sp", xt[i][:], self.XS[tt * 128:(tt + 1) * 128, :], reads=self.XS.rk(tt), writes=[xt[i]])
            self.norm_tile(xt[i], fg, None, xn[i], junk, ss[i])
            evs.append(self.dma("sp", self.out[(tt - 2) * 128:(tt - 1) * 128, :], xn[i][:], reads=[xn[i]]))
        for ev in evs:
            self.S.wait_event("sp", ev)
        self.pop()

    return dict(phase_wout=phase_wout, phase_moe=phase_moe, phase_final=phase_final)


for _k, _v in _tail_methods().items():
    setattr(Kern, _k, _v)
```

```python
import contextlib
import numpy as np
import concourse.bass as bass
import concourse.mybir as mybir
from concourse.bass_utils import run_bass_kernel_spmd

F32 = mybir.dt.float32
F32R = mybir.dt.float32r
BF16 = mybir.dt.bfloat16
I32 = mybir.dt.int32
AF = mybir.ActivationFunctionType
ALU = mybir.AluOpType
AX = mybir.AxisListType

D = 1024
T_CTX = 256
T_LAT = 2048
T = T_CTX + T_LAT
NT = T // 128
NCH = T // 64
DEPTH = 4
DIN = 2320
EPS = 1e-6
NEG = -30000.0


class Res:
    __slots__ = ("name", "writer", "readers", "excl")

    def __init__(self, name):
        self.name = name
        self.writer = None
        self.readers = []
        self.excl = False


class Sched:
    ENG = ("pe", "dve", "act", "pool", "sp")

    def __init__(self, nc, n_dma_sems=8):
        self.nc = nc
        self.obj = {"pe": nc.tensor, "dve": nc.vector, "act": nc.scalar,
                    "pool": nc.gpsimd, "sp": nc.sync}
        self.sem = {}
        self.count = {}
        self.waited = {e: {} for e in self.ENG}
        self._ctx = []
        for e in self.ENG:
            cm = nc.semaphore("s_" + e)
            self.sem[e] = cm.__enter__()
            self._ctx.append(cm)
            self.count[e] = 0
        self.dma_sems = {}
        self.dma_rr = {}
        self.gen = 0
        self.n_dma_sems = n_dma_sems
        for q in ("sp", "pool"):
            lst = []
            for i in range(n_dma_sems):
                cm = nc.semaphore(f"d_{q}{i}")
                lst.append([cm.__enter__(), 0])
                self._ctx.append(cm)
            self.dma_sems[q] = lst
            self.dma_rr[q] = 0
        self.n_wait = 0
        self.n_inst = 0

    def renew(self):
        self.barrier()
        self.gen += 1
        nc = self.nc
        for e in self.ENG:
            cm = nc.semaphore(f"s_{e}_g{self.gen}")
            self.sem[e] = cm.__enter__()
            self._ctx.append(cm)
            self.count[e] = 0
        for q in ("sp", "pool"):
            lst = []
            for i in range(self.n_dma_sems):
                cm = nc.semaphore(f"d_{q}{i}_g{self.gen}")
                lst.append([cm.__enter__(), 0])
                self._ctx.append(cm)
            self.dma_sems[q] = lst
            self.dma_rr[q] = 0
        self.waited = {e: {} for e in self.ENG}

    def _need(self, eng, ev, same_ok_dist=None):
        if ev is None:
            return None
        if len(ev) > 4 and ev[4] != self.gen:
            return None
        key, sem, val, src = ev[:4]
        if src == eng and src is not None:
            if same_ok_dist is None:
                return None
            if self.count[eng] - val >= same_ok_dist:
                return None
        if self.waited[eng].get(key, 0) >= val:
            return None
        return ev

    def _emit_waits(self, eng, evs):
        best = {}
        for ev in evs:
            if ev is None:
                continue
            key = ev[0]
            if key not in best or best[key][2] < ev[2]:
                best[key] = ev
        for key, evb in best.items():
            sem, val = evb[1], evb[2]
            self.obj[eng].wait_ge(sem, val)
            self.waited[eng][key] = val
            self.n_wait += 1

    def deps(self, eng, reads, writes):
        evs = []
        for r in reads:
            evs.append(self._need(eng, r.writer, same_ok_dist=4))
        for w in writes:
            evs.append(self._need(eng, w.writer, same_ok_dist=None))
            for rd in w.readers:
                evs.append(self._need(eng, rd, same_ok_dist=None))
        return evs

    def _commit(self, ev, reads, writes):
        for r in reads:
            r.readers.append(ev)
            if len(r.readers) > 48:
                best = {}
                for e in r.readers:
                    if e[4] != self.gen:
                        continue
                    if e[0] not in best or best[e[0]][2] < e[2]:
                        best[e[0]] = e
                r.readers = list(best.values())
        for w in writes:
            w.writer = ev
            w.readers = []

    def op(self, eng, fn, reads=(), writes=()):
        ex = [r for r in reads if r.excl]
        if ex:
            writes = list(writes) + ex
        self._emit_waits(eng, self.deps(eng, reads, writes))
        ins = fn(self.obj[eng])
        self.count[eng] += 1
        ins.then_inc(self.sem[eng], 1)
        ev = (eng, self.sem[eng], self.count[eng], eng, self.gen)
        self._commit(ev, reads, writes)
        self.n_inst += 1
        return ev

    def dma(self, q, fn, reads=(), writes=()):
        evs = self.deps(q, reads, writes)
        idx = self.dma_rr[q]
        slot = self.dma_sems[q][idx]
        self.dma_rr[q] = (idx + 1) % len(self.dma_sems[q])
        key = f"d_{q}{idx}"
        if slot[1] > 0:
            evs.append(self._need(q, (key, slot[0], slot[1], None, self.gen)))
        self._emit_waits(q, evs)
        ins = fn(self.obj[q])
        slot[1] += 16
        ins.then_inc(slot[0], 16)
        ev = (key, slot[0], slot[1], None, self.gen)
        self._commit(ev, reads, writes)
        self.n_inst += 1
        return ev

    def wait_event(self, eng, ev):
        self._emit_waits(eng, [self._need(eng, ev)])

    def barrier(self):
        evs = []
        for e in self.ENG:
            if self.count[e] > 0:
                evs.append((e, self.sem[e], self.count[e], e, self.gen))
        for q, lst in self.dma_sems.items():
            for i, (sem, val) in enumerate(lst):
                if val > 0:
                    evs.append((f"d_{q}{i}", sem, val, None, self.gen))
        for e in self.ENG:
            need = []
            for ev in evs:
                if ev[3] == e:
                    continue
                if self.waited[e].get(ev[0], 0) >= ev[2]:
                    continue
                need.append(ev)
            self._emit_waits(e, need)


class Tl:
    def __init__(self, ap, name, nsub=0):
        self.ap = ap
        self.name = name
        self.rs = [Res(f"{name}.{i}") for i in range(max(1, nsub))]

    def __getitem__(self, key):
        return self.ap[key]

    @property
    def r(self):
        return self.rs

    def rk(self, *ks):
        n = len(self.rs)
        return [self.rs[min(k, n - 1)] for k in ks]


def _flat(lst):
    out = []
    for x in lst:
        if isinstance(x, Tl):
            out.extend(x.rs)
        elif isinstance(x, (list, tuple)):
            out.extend(_flat(x))
        elif x is not None:
            out.append(x)
    return out


class Builder:
    def __init__(self, n_layers=DEPTH, debug=None, skip_inputs=(), flags=()):
        self.flags = set(flags)
        self.skip_inputs = set(skip_inputs)
        self.n_layers = n_layers
        self.debug = debug or {}
        self.nc = bass.Bass("TRN2", target_bir_lowering=False)
        self.S = Sched(self.nc)
        self.stack = [contextlib.ExitStack()]
        self._uid = 0
        self.outputs = []

    def push(self):
        self.stack.append(contextlib.ExitStack())

    def pop(self):
        self.S.barrier()
        self.stack.pop().close()

    def sb(self, name, shape, dt=F32, nsub=0):
        self._uid += 1
        t = self.stack[-1].enter_context(self.nc.sbuf_tensor(f"{name}_{self._uid}", list(shape), dt))
        return Tl(t, name, nsub)

    def ps(self, name, shape, dt=F32, nsub=0):
        self._uid += 1
        t = self.stack[-1].enter_context(self.nc.psum_tensor(f"{name}_{self._uid}", list(shape), dt))
        tl = Tl(t, name, nsub)
        for r in tl.rs:
            r.excl = True
        return tl

    def dram(self, name, shape, dt=F32, kind="Internal", nsub=0):
        t = self.nc.dram_tensor(name, list(shape), dt, kind=kind).ap()
        return Tl(t, name, nsub)

    op_limit = None
    op_cnt = 0

    def op(self, eng, fn, reads=(), writes=()):
        if self.op_limit is not None:
            self.op_cnt += 1
            if self.op_cnt > self.op_limit:
                return None
            if self.op_cnt == self.op_limit:
                import inspect
                fr = inspect.stack()
                print("LAST OP:", eng, [f"{f.function}:{f.lineno}" for f in fr[1:5]])
        return self.S.op(eng, fn, _flat(reads), _flat(writes))

    def dma(self, q, out, in_, reads=(), writes=(), **kw):
        return self.S.dma(q, lambda e: e.dma_start(out=out, in_=in_, **kw), _flat(reads), _flat(writes))

    def mm(self, out, lhsT, rhs, start, stop, reads, writes):
        return self.op("pe", lambda e: e.matmul(out, lhsT=lhsT, rhs=rhs, start=start, stop=stop), reads, writes)

    def tr(self, out, in_, ident, reads, writes):
        return self.op("pe", lambda e: e.transpose(out, in_, ident), reads, writes)

    def act(self, out, in_, func, reads, writes, eng="act", **kw):
        return self.op("act", lambda e: e.activation(out=out, in_=in_, func=func, **kw), reads, writes)

    def tt(self, eng, out, in0, in1, op, reads, writes):
        return self.op(eng, lambda e: e.tensor_tensor(out=out, in0=in0, in1=in1, op=op), reads, writes)

    def ts(self, eng, out, in0, s1, op0, reads, writes, s2=None, op1=None):
        if op1 is None:
            return self.op(eng, lambda e: e.tensor_scalar(out=out, in0=in0, scalar1=s1, scalar2=None, op0=op0), reads, writes)
        return self.op(eng, lambda e: e.tensor_scalar(out=out, in0=in0, scalar1=s1, scalar2=s2, op0=op0, op1=op1), reads, writes)

    def stt(self, out, in0, scalar, in1, op0, op1, reads, writes):
        return self.op("dve", lambda e: e.scalar_tensor_tensor(out=out, in0=in0, scalar=scalar, in1=in1, op0=op0, op1=op1), reads, writes)

    def copy(self, eng, out, in_, reads, writes):
        if eng == "act":
            return self.op("act", lambda e: e.copy(out=out, in_=in_), reads, writes)
        return self.op(eng, lambda e: e.tensor_copy(out=out, in_=in_), reads, writes)

    def memset(self, eng, ap, val, writes):
        return self.op(eng, lambda e: e.memset(ap, val), (), writes)


C_GQ, C_GK, C_GV, C_GG, C_GAB = 0, 256, 512, 768, 1024
C_LX, C_LY = 1040, 1296
C_AQ, C_AK, C_AV = 1552, 2064, 2192
PF_GDN, PF_LRU = 0, 768
PT_GATE, PT_AB, PT_ATT = 0, 256, 272
PT_W = 1040

EXTRA_LAYOUT = ['gdn_conv_wT', 'lru_conv_wT']
WEIGHT_NAMES = ['w_ada', 'b_ada', 'norm1_g', 'norm2_g', 'w_in', 'w_out', 'gdn_conv_w', 'gdn_a_log',
                'gdn_dt_bias', 'gdn_norm_g', 'lru_conv_w', 'lru_conv_b', 'lru_w_r', 'lru_b_r', 'lru_w_i',
                'lru_b_i', 'lru_lambda', 'attn_q_norm_g', 'attn_k_norm_g', 'moe_w_group', 'moe_b_group',
                'moe_w_expert', 'moe_b_expert', 'moe_w1', 'moe_w3', 'moe_w2', 'final_norm_g']
WEIGHT_SHAPES = {
    'w_ada': [4, 1024, 6144], 'b_ada': [4, 6144], 'norm1_g': [4, 1024], 'norm2_g': [4, 1024],
    'w_in': [4, 1024, 2320], 'w_out': [4, 1024, 1024], 'gdn_conv_w': [4, 4, 768], 'gdn_a_log': [4, 2, 4],
    'gdn_dt_bias': [4, 2, 4], 'gdn_norm_g': [4, 64], 'lru_conv_w': [4, 4, 256], 'lru_conv_b': [4, 256],
    'lru_w_r': [4, 2, 4, 64, 64], 'lru_b_r': [4, 2, 256], 'lru_w_i': [4, 2, 4, 64, 64], 'lru_b_i': [4, 2, 256],
    'lru_lambda': [4, 2, 256], 'attn_q_norm_g': [4, 64], 'attn_k_norm_g': [4, 64], 'moe_w_group': [4, 1024, 8],
    'moe_b_group': [4, 8], 'moe_w_expert': [4, 1024, 64], 'moe_b_expert': [4, 64],
    'moe_w1': [4, 64, 1024, 512], 'moe_w3': [4, 64, 1024, 512], 'moe_w2': [4, 64, 512, 1024],
    'final_norm_g': [1, 1024], 'gdn_conv_wT': [4, 768, 4], 'lru_conv_wT': [4, 256, 4]}


def host_consts():
    c = {}
    c['ident'] = np.eye(128, dtype=np.float32)
    r = np.arange(64)[:, None]
    q = np.arange(64)[None, :]
    def t4(m):
        return np.tile(m.astype(np.float32), (1, 4))
    g = np.zeros((64, 9, 256), np.float32)
    g[:, 0] = t4(np.where(r >= q, 0.0, NEG))
    g[:, 1] = t4(np.where(r <= q, 0.0, NEG))
    g[:, 2] = t4(np.where(r > q, -1.0, 0.0))
    g[:, 3] = t4(np.where(r < q, -1.0, 0.0))
    g[:, 4] = t4(np.where(r <= q, 1.0, 0.0))
    g[:, 5] = t4(np.where(r >= q, 1.0, 0.0))
    g[:, 6] = 1.0
    g[:, 7] = -1.0
    g[:, 8] = t4(np.eye(64))
    c['gconst'] = g.reshape(64, 9 * 256)
    pos = np.arange(T_LAT)
    row = (pos // 64).astype(np.float64); col = (pos % 64).astype(np.float64)
    inv = 10000.0 ** (-np.arange(0, 32, 2, dtype=np.float64) / 32)
    ang = np.concatenate([row[:, None] * inv, col[:, None] * inv], axis=-1)
    c['rope_cs'] = np.concatenate([np.cos(ang), np.sin(ang)], axis=-1).astype(np.float32)
    mc = np.zeros((128, 256), np.float32)
    mc[:, 0:64] = np.arange(64)[None, :]
    mc[:, 64:100] = 128.0 * np.arange(36)[None, :]
    mc[:, 100:136] = np.arange(36)[None, :]
    mc[:, 136] = np.arange(128)
    mc[:, 137:201] = 1.0
    c['moe_const'] = mc
    tri = np.zeros((128, 256), np.float32)
    tri[:, 0:128] = (np.arange(128)[:, None] < np.arange(128)[None, :])
    tri[:, 128:256] = 1.0
    c['moe_tri'] = tri
    return c


class Kern(Builder):
    def declare_io(self):
        nc = self.nc
        self.inp = {}
        self.inp['xs_in'] = nc.dram_tensor('xs_in', [T, D], F32, kind='ExternalInput').ap()
        self.inp['cT_in'] = nc.dram_tensor('cT_in', [128, 16], F32, kind='ExternalInput').ap()
        for n in WEIGHT_NAMES + EXTRA_LAYOUT:
            if n in self.skip_inputs:
                continue
            shp = list(WEIGHT_SHAPES[n])
            if shp[0] == 4:
                shp[0] = self.n_layers
            self.inp[n] = nc.dram_tensor(n, shp, F32, kind='ExternalInput').ap()
        for n, v in host_consts().items():
            self.inp[n] = nc.dram_tensor(n, list(v.shape), F32, kind='ExternalInput').ap()
        self.out = nc.dram_tensor('out', [T_LAT, D], F32, kind='ExternalOutput').ap()
        self.XS = self.dram('XS', [T, D], nsub=NT)
        self.MODS = self.dram('MODS', [2, 6 * D])
        self.PF = self.dram('PF', [1280, T])
        self.PT = self.dram('PT', [T, PT_W], nsub=NT)
        self.MO = self.dram('MO', [D, T])
        self.XB = self.dram('XB', [N_SLOT, D])
        self.YB = self.dram('YB', [N_SLOT, D])
        self.dbg = {}
        for name, shape in self.debug.items():
            self.dbg[name] = nc.dram_tensor('dbg_' + name, list(shape), F32, kind='ExternalOutput').ap()

    def bcast_row(self, ap_row, n):
        return ap_row.partition_broadcast(128)

    def phase_init(self):
        for tt in range(NT):
            self.dma("sp", self.XS[tt * 128:(tt + 1) * 128, :], self.inp['xs_in'][tt * 128:(tt + 1) * 128, :],
                     writes=self.XS.rk(tt))
        self.ident = self.sb('ident', [128, 128])
        self.dma("sp", self.ident[:], self.inp['ident'][:, :], writes=[self.ident])
        cT = self.sb('cT', [128, 16])
        self.dma("sp", cT[:], self.inp['cT_in'][:, :], writes=[cT])
        self.scT = self.sb('scT', [128, 8, 2], F32R)
        self.act(self.scT[:, :, 0], cT[:, 0:8], AF.Silu, [cT], [self.scT])
        self.act(self.scT[:, :, 1], cT[:, 8:16], AF.Silu, [cT], [self.scT])

    def phase_ada(self, l):
        self.push()
        wa = [self.sb(f'wa{i}', [128, 8, 512], F32R) for i in range(2)]
        ba = self.sb('ba', [2, 6 * D])
        mods = self.sb('mods', [2, 6 * D])
        pp = [self.ps(f'pada{i}', [2, 512]) for i in range(2)]
        for r in range(2):
            self.dma("sp", ba[r:r + 1, :], self.inp['b_ada'][l:l + 1, :], writes=[ba])
        wsrc = self.inp['w_ada'][l].rearrange("(kc p) n -> p kc n", p=128)
        for cg in range(12):
            w = wa[cg % 2]
            self.dma("pool", w[:], wsrc[:, :, cg * 512:(cg + 1) * 512], writes=[w])
            p = pp[cg % 2]
            for kc in range(8):
                self.mm(p[:], self.scT[:, kc, :], w[:, kc, :], kc == 0, kc == 7, [self.scT, w], [p])
            self.tt("dve", mods[:, cg * 512:(cg + 1) * 512], p[:], ba[:, cg * 512:(cg + 1) * 512], ALU.add,
                    [p, ba], [mods])
        self.dma("sp", self.MODS[:, :], mods[:], reads=[mods], writes=[self.MODS])
        self.pop()

    def load_mod(self, dst, which, r, g_name=None, l=0):
        self.dma("sp", dst[:], self.MODS[r:r + 1, which * D:(which + 1) * D].partition_broadcast(128),
                 reads=[self.MODS], writes=[dst])
        if g_name is not None:
            gb = self.sb('gb', [128, D])
            self.dma("sp", gb[:], self.inp[g_name][l:l + 1, :].partition_broadcast(128), writes=[gb])
            self.stt(dst[:], dst[:], 1.0, gb[:], ALU.add, ALU.mult, [dst, gb], [dst])

    def evac(self, i, out, in_, reads, writes):
        return self.copy("act" if i % 2 == 0 else "dve", out, in_, reads, writes)

    def norm_tile(self, xt, gm, sh, xn, junk, ss):
        self.act(junk[:], xt[:], AF.Square, [xt], [junk, ss], accum_out=ss[:, 0:1])
        self.act(ss[:, 1:2], ss[:, 0:1], AF.Sqrt, [ss], [ss], scale=1.0 / D, bias=self.eps_t[:, 0:1])
        self.op("dve", lambda e: e.reciprocal(out=ss[:, 2:3], in_=ss[:, 1:2]), [ss], [ss])
        self.stt(xn[:], xt[:], ss[:, 2:3], gm[:], ALU.mult, ALU.mult, [xt, ss, gm], [xn])
        if sh is not None:
            self.tt("pool", xn[:], xn[:], sh[:], ALU.add, [xn, sh], [xn])

    def phase_proj(self, l):
        self.push()
        win = self.sb('win', [128, 8, DIN], F32R)
        wsrc = self.inp['w_in'][l].rearrange("(kc p) n -> p kc n", p=128)
        for a, b in ((0, 1160), (1160, 2320)):
            self.dma("pool", win[:, :, a:b], wsrc[:, :, a:b], writes=[win])
        gm = [self.sb(f'gm{r}', [128, D]) for r in range(2)]
        sh = [self.sb(f'sh{r}', [128, D]) for r in range(2)]
        self.push()
        for r in range(2):
            self.load_mod(gm[r], 1, r, 'norm1_g', l)
            self.load_mod(sh[r], 0, r)
        self.pop()
        xt = [self.sb(f'xt{i}', [128, D]) for i in range(2)]
        xn = [self.sb(f'xn{i}', [128, D]) for i in range(2)]
        junk = self.sb('junk', [128, D])
        ss = [self.sb(f'ss{i}', [128, 4]) for i in range(2)]
        xnT = self.sb('xnT', [128, 8, 512], F32R, nsub=4)
        sfm = [self.sb(f'sfm{i}', [128, 512]) for i in range(2)]
        stm = [self.sb(f'stm{i}', [128, PT_W]) for i in range(2)]
        ptp = [self.ps(f'ptp{i}', [128, 512]) for i in range(2)]
        pfm = [self.ps(f'pfm{i}', [128, 512]) for i in range(2)]
        ptm = [self.ps('ptm0', [128, 272]), self.ps('ptm1', [128, 512]), self.ps('ptm2', [128, 256])]
        fm_groups = [(c0, PF_GDN + c0) for c0 in range(0, 768, 128)] + \
                    [(C_LX + c0, PF_LRU + c0) for c0 in range(0, 512, 128)]
        tm_groups = [(C_GG, 272, PT_GATE), (C_AQ, 512, PT_ATT), (C_AQ + 512, 256, PT_ATT + 512)]
        blocks = [(0, 2), (2, 4), (6, 4), (10, 4), (14, 4)]
        it = 0
        ig = 0
        for (t0, ntile) in blocks:
            r = 1 if t0 == 0 else 0
            for j in range(ntile):
                tt = t0 + j
                x_ = xt[it % 2]; xn_ = xn[it % 2]; ss_ = ss[it % 2]
                self.dma("sp", x_[:], self.XS[tt * 128:(tt + 1) * 128, :], reads=self.XS.rk(tt), writes=[x_])
                self.norm_tile(x_, gm[r], sh[r], xn_, junk, ss_)
                for half in range(2):
                    p = ptp[half]
                    for q in range(4):
                        kc = half * 4 + q
                        self.tr(p[:, q * 128:(q + 1) * 128], xn_[:, kc * 128:(kc + 1) * 128], self.ident[:],
                                [xn_, self.ident], [p])
                    self.evac(half, xnT[:, half * 4:(half + 1) * 4, j * 128:(j + 1) * 128],
                              p[:].rearrange("p (q t) -> p q t", q=4), [p], xnT.rk(j))
                st = stm[it % 2]
                for gi, (c0, n, d0) in enumerate(tm_groups):
                    p = ptm[gi]
                    for kc in range(8):
                        self.mm(p[:, 0:n], xnT[:, kc, j * 128:(j + 1) * 128], win[:, kc, c0:c0 + n], kc == 0, kc == 7,
                                [xnT.rk(j), win], [p])
                    self.evac(gi, st[:, d0:d0 + n], p[:, 0:n], [p], [st])
                self.dma("sp", self.PT[tt * 128:(tt + 1) * 128, :], st[:], reads=[st], writes=self.PT.rk(tt))
                it += 1
            ntok = ntile * 128
            for (c0, d0) in fm_groups:
                p = pfm[ig % 2]; s_ = sfm[ig % 2]
                for kc in range(8):
                    self.mm(p[:, 0:ntok], win[:, kc, c0:c0 + 128], xnT[:, kc, 0:ntok], kc == 0, kc == 7,
                            [win, xnT.rk(*range(ntile))], [p])
                self.evac(ig, s_[:, 0:ntok], p[:, 0:ntok], [p], [s_])
                self.dma("sp", self.PF[d0:d0 + 128, t0 * 128:t0 * 128 + ntok], s_[:, 0:ntok], reads=[s_], writes=[self.PF])
                ig += 1
        self.pop()

    def build(self, stop_after=None):
        self.stop = stop_after
        self.declare_io()
        self.eps_t = self.sb('eps', [128, 1])
        self.memset("pool", self.eps_t[:], EPS, [self.eps_t])
        self.phase_init()
        for l in range(self.n_layers):
            if l > 0:
                self.S.renew()
            self.phase_ada(l)
            self.phase_proj(l)
            if stop_after == 'proj':
                break
            if 'nogdn' not in self.flags:
                self.phase_gdn(l)
            if stop_after is not None and stop_after.startswith('gdn'):
                break
            self.phase_lru(l)
            if stop_after == 'lru':
                break
            self.phase_att(l)
            if stop_after == 'att':
                break
            self.phase_wout(l)
            if stop_after == 'wout':
                break
            self.phase_moe(l)
            if stop_after == 'moe':
                break
        if stop_after is None:
            self.phase_final()
        evs = []
        for name, ap in self.dbg.items():
            src = {'PF': self.PF, 'PT': self.PT, 'MODS': self.MODS, 'MO': self.MO, 'XS': self.XS}[name]
            self.S.barrier()
            evs.append(self.dma("sp", ap[:, :], src[:, :], reads=[src]))
        for ev in evs:
            self.S.wait_event("sp", ev)
        self.S.barrier()
        return self.nc


def make_in_maps(inputs, n_layers=DEPTH, skip=()):
    consts = host_consts()
    maps = []
    for b in range(8):
        m = {}
        m['xs_in'] = np.ascontiguousarray(np.concatenate([inputs['ctx'][b], inputs['x'][b]], axis=0), dtype=np.float32)
        cT = np.concatenate([np.asarray(inputs['c'][b]).reshape(8, 128).T,
                             np.asarray(inputs['c_ctx']).reshape(8, 128).T], axis=1)
        m['cT_in'] = np.ascontiguousarray(cT, dtype=np.float32)
        for n in WEIGHT_NAMES:
            if n in skip:
                continue
            a = np.asarray(inputs[n], dtype=np.float32).reshape(WEIGHT_SHAPES[n])
            if WEIGHT_SHAPES[n][0] == 4:
                a = a[:n_layers]
            m[n] = np.ascontiguousarray(a)
        m['gdn_conv_wT'] = np.ascontiguousarray(np.transpose(np.asarray(inputs['gdn_conv_w'], dtype=np.float32), (0, 2, 1))[:n_layers])
        m['lru_conv_wT'] = np.ascontiguousarray(np.transpose(np.asarray(inputs['lru_conv_w'], dtype=np.float32), (0, 2, 1))[:n_layers])
        m.update(consts)
        maps.append(m)
    return maps


def kernel(**inputs):
    kb = Kern()
    nc = kb.build()
    res = run_bass_kernel_spmd(nc, make_in_maps(inputs), core_ids=list(range(8)))
    return np.stack([np.asarray(r['out']) for r in res.results], axis=0)


G_NML, G_NMU, G_SML, G_SMU, G_L, G_U, G_ONE, G_NEG1, G_ID = range(9)


def _gdn_methods():
    def gc(self, k, n=256):
        return self.gconst[:, k * 256:k * 256 + n]

    def phase_gdn(self, l):
        self.push()
        S = self.S
        gconst = self.sb('gconst', [64, 9 * 256])
        self.gconst = gconst
        self.dma("sp", gconst[:], self.inp['gconst'][:, :], writes=[gconst])
        one_t = self.sb('one', [128, 1])
        self.memset("pool", one_t[:], 1.0, [one_t])
        qn = self.sb('qn', [64, 4, T], nsub=NCH)
        kn = self.sb('kn', [64, 4, T], nsub=NCH)
        vT = self.sb('vT', [128, 2, T], nsub=NCH)
        OF = self.dram(f'OF{l}', [T, 256], nsub=NCH)
        cwq = self.sb('cwq', [64, 8, 4])
        cwv = self.sb('cwv', [128, 2, 4])
        cw_src = self.inp['gdn_conv_wT'][l]
        self.dma("sp", cwq[:], cw_src[0:512, :].rearrange("(h p) m -> p h m", p=64), writes=[cwq])
        self.dma("sp", cwv[:], cw_src[512:768, :].rearrange("(h p) m -> p h m", p=128), writes=[cwv])
        chunks_all = list(range(NCH))

        self.push()
        raw = [self.sb(f'raw{i}', [128, 516]) for i in range(3)]
        lnb = self.sb('lnb', [64, 4, T])
        sqb = [self.sb(f'sqb{i}', [64, 512]) for i in range(2)]
        pss = [self.ps(f'pss{i}', [64, 512]) for i in range(2)]
        segs = [(0, T_CTX, [(0, 256)]), (T_CTX, T, [(T_CTX + i * 512, 512) for i in range(4)])]
        it = 0
        for kind in range(10):
            P = 64 if kind < 8 else 128
            row0 = kind * 64 if kind < 8 else 512 + (kind - 8) * 128
            cw = cwq[:, kind, :] if kind < 8 else cwv[:, kind - 8, :]
            cwt = cwq if kind < 8 else cwv
            for (s0, s1, blks) in segs:
                for (t0, blk) in blks:
                    rw = raw[it % 3]
                    lo = max(t0 - 1, s0); hi = min(t0 + blk + 2, s1)
                    if lo > t0 - 1:
                        self.memset("pool", rw[0:P, 0:1], 0.0, [rw])
                    if hi < t0 + blk + 2:
                        self.memset("pool", rw[0:P, hi - (t0 - 1):blk + 3], 0.0, [rw])
                    self.dma("sp", rw[0:P, lo - (t0 - 1):hi - (t0 - 1)], self.PF[row0:row0 + P, lo:hi],
                             reads=[self.PF], writes=[rw])
                    if kind >= 8:
                        dt_, dst = vT, vT[:, kind - 8, t0:t0 + blk]
                    elif kind < 4:
                        dt_, dst = qn, qn[:, kind, t0:t0 + blk]
                    else:
                        dt_, dst = kn, kn[:, kind - 4, t0:t0 + blk]
                    self.ts("dve", dst, rw[0:P, 0:blk], cw[:, 0:1], ALU.mult, [rw, cwt], [dt_])
                    for m in range(1, 4):
                        self.stt(dst, rw[0:P, m:m + blk], cw[:, m:m + 1], dst, ALU.mult, ALU.add, [rw, cwt, dt_], [dt_])
                    it += 1
        f2 = lambda t: t[:].rearrange("p h t -> p (h t)")
        for t_ in (qn, kn, vT):
            self.act(f2(t_), f2(t_), AF.Silu, [t_], [t_])
        blks_all = [(0, 256)] + [(T_CTX + i * 512, 512) for i in range(4)]
        it = 0
        for t_, sc in ((qn, 0.125), (kn, 1.0)):
            for h in range(4):
                for (t0, blk) in blks_all:
                    sq = sqb[it % 2]; ps_ = pss[it % 2]
                    self.tt("pool", sq[:, 0:blk], t_[:, h, t0:t0 + blk], t_[:, h, t0:t0 + blk], ALU.mult, [t_], [sq])
                    self.mm(ps_[:, 0:blk], self.gc(G_ONE, 64), sq[:, 0:blk], True, True, [gconst, sq], [ps_])
                    self.act(lnb[:, h, t0:t0 + blk], ps_[:, 0:blk], AF.Ln, [ps_], [lnb], bias=self.eps_t[0:64, 0:1])
                    it += 1
            self.act(f2(lnb), f2(lnb), AF.Exp, [lnb], [lnb], scale=-0.5)
            self.stt(f2(t_), f2(t_), sc, f2(lnb), ALU.mult, ALU.mult, [t_, lnb], [t_])
        self.pop()

        if self.stop == 'gdn_pre':
            self.dma("sp", self.MO[0:256, :].rearrange("(m p) t -> p m t", p=128), vT[:], reads=[vT], writes=[self.MO])
            self.dma("sp", self.MO[256:512, :].rearrange("(h p) t -> p h t", p=64), kn[:], reads=[kn], writes=[self.MO])
            self.dma("sp", self.MO[512:768, :].rearrange("(h p) t -> p h t", p=64), qn[:], reads=[qn], writes=[self.MO])
            self.pop()
            return
        abT = self.sb('abT', [64, NCH, 16])
        self.dma("sp", abT[:], self.PT[:, PT_AB:PT_AB + 16].rearrange("(c p) n -> p c n", p=64),
                 reads=[self.PT], writes=[abT])
        par = self.sb('par', [64, 16])
        self.dma("sp", par[:, 0:8], self.inp['gdn_a_log'][l:l + 1].rearrange("o d h -> o (d h)").partition_broadcast(64), writes=[par])
        self.dma("sp", par[:, 8:16], self.inp['gdn_dt_bias'][l:l + 1].rearrange("o d h -> o (d h)").partition_broadcast(64), writes=[par])
        negA = self.sb('negA', [64, 8])
        self.act(negA[:], par[:, 0:8], AF.Exp, [par], [negA])
        self.ts("dve", negA[:], negA[:], -1.0, ALU.mult, [negA], [negA])
        gall = self.sb('gall', [64, NCH, 8])
        beta = self.sb('beta', [64, NCH, 8])
        ball = self.sb('ball', [64, NCH, 8])
        eb = self.sb('eb', [64, NCH, 8])
        ebeta = self.sb('ebeta', [64, NCH, 8])
        ekd = self.sb('ekd', [64, NCH, 8])
        etot = self.sb('etot', [64, NCH, 8])
        tot = self.sb('tot', [64, NCH, 8])
        self.push()
        pb = [self.ps(f'pb{i}', [64, NCH * 8]) for i in range(3)]
        self.tt("dve", gall[:], abT[:, :, 0:8], par[:, 8:16].unsqueeze(1).to_broadcast([64, NCH, 8]), ALU.add, [abT, par], [gall])
        self.act(gall[:], gall[:], AF.Exp, [gall], [gall])
        self.act(gall[:], gall[:], AF.Ln, [gall], [gall], bias=one_t[0:64, 0:1])
        self.tt("dve", gall[:], gall[:], negA[:].unsqueeze(1).to_broadcast([64, NCH, 8]), ALU.mult, [gall, negA], [gall])
        self.act(beta[:], abT[:, :, 8:16], AF.Sigmoid, [abT], [beta])
        g2 = gall[:].rearrange("p c n -> p (c n)")
        self.mm(pb[0][:], self.gc(G_L, 64), g2, True, True, [gconst, gall], [pb[0]])
        self.mm(pb[1][:], self.gc(G_U, 64), g2, True, True, [gconst, gall], [pb[1]])
        self.mm(pb[2][:], self.gc(G_ONE, 64), g2, True, True, [gconst, gall], [pb[2]])
        v3 = lambda p: p[:].rearrange("p (c n) -> p c n", n=8)
        self.copy("dve", ball[:, :, 0:4], v3(pb[0])[:, :, 0:4], [pb[0]], [ball])
        self.copy("dve", ball[:, :, 4:8], v3(pb[1])[:, :, 4:8], [pb[1]], [ball])
        self.copy("dve", tot[:], v3(pb[2]), [pb[2]], [tot])
        self.act(eb[:], ball[:], AF.Exp, [ball], [eb])
        self.tt("dve", ebeta[:], eb[:], beta[:], ALU.mult, [eb, beta], [ebeta])
        self.tt("dve", ekd[:], tot[:], ball[:], ALU.subtract, [tot, ball], [ekd])
        self.act(ekd[:], ekd[:], AF.Exp, [ekd], [ekd])
        self.act(etot[:], tot[:], AF.Exp, [tot], [etot])
        self.pop()
        gng = self.sb('gng', [64, 4, 64])
        self.dma("sp", gng[:, 0, :], self.inp['gdn_norm_g'][l:l + 1, :].partition_broadcast(64), writes=[gng])
        for h in range(1, 4):
            self.copy("dve", gng[:, h, :], gng[:, 0, :], [gng], [gng])

        if self.stop == 'gdn_scal':
            for i_, t_ in enumerate((gall, beta, ball, eb, ebeta, ekd, etot, tot)):
                self.dma("sp", self.MO[i_ * 64:(i_ + 1) * 64, 0:NCH * 8], t_[:].rearrange("p c n -> p (c n)"), reads=[t_], writes=[self.MO])
            self.pop()
            return
        bk = [[self.ps(f'bk{j}{n}', [128, 512]) for n in range(3)] for j in range(2)]
        pG = self.ps('pG', [128, 512]); pH = self.ps('pH', [128, 512])
        H0 = slice(0, 256); H1 = slice(256, 512)
        W = [64, 256]
        LT = []
        for j in range(2):
            LT.append(dict(
                GLt=self.sb(f'GLt{j}', W), Em=self.sb(f'Em{j}', W), EmT=self.sb(f'EmT{j}', W), tA=self.sb(f'tA{j}', W),
                X=[self.sb(f'X{j}{i}', W) for i in range(2)], XT=[self.sb(f'XT{j}{i}', W) for i in range(2)],
                Pm=[self.sb(f'Pm{j}{i}', W) for i in range(2)], vb=self.sb(f'vb{j}', W), kbe=self.sb(f'kbe{j}', W)))
        NOB = 4
        u_ = [self.sb(f'u{i}', W) for i in range(NOB)]
        wT_ = [self.sb(f'wT{i}', W) for i in range(NOB)]
        KQm_ = [self.sb(f'KQm{i}', W) for i in range(NOB)]
        kd_ = [self.sb(f'kd{i}', W) for i in range(NOB)]
        Sst = [self.sb(f'S{i}', W) for i in range(2)]
        St = self.sb('St', W)
        vnew = self.sb('vnew', W)
        o2sb = self.sb('o2sb', W); osb = [self.sb(f'osb{i}', W) for i in range(2)]
        ofl = [self.sb(f'ofl{i}', W) for i in range(2)]
        gat = [self.sb(f'gat{i}', W) for i in range(2)]
        rs4 = self.sb('rs4', [64, 8]); ysb = self.sb('ysb', W); sqo = self.sb('sqo', W)
        rob = [self.sb(f'rob{i}', [128, 2, 64]) for i in range(2)]

        def bc4(t, c, d):
            return t[:, c, d * 4:(d + 1) * 4].unsqueeze(2).to_broadcast([64, 4, 64])

        def v4(ap):
            return ap.rearrange("p (h n) -> p h n", h=4)

        def local(c, d, i, j):
            L = LT[j]
            GLt, Em, EmT, tA, X, XT, Pm, vb, kbe = (L[k] for k in ('GLt', 'Em', 'EmT', 'tA', 'X', 'XT', 'Pm', 'vb', 'kbe'))
            b0, b1, b2 = bk[j]
            cs = slice(c * 64, (c + 1) * 64)
            NMa, NMb = (G_NML, G_NMU) if d == 0 else (G_NMU, G_NML)
            SM = G_SML if d == 0 else G_SMU
            self.tt("dve", v4(GLt[:]), v4(self.gc(G_ID)), bc4(ball, c, d), ALU.mult, [gconst, ball], [GLt])
            for h in range(4):
                hs = slice(h * 64, (h + 1) * 64)
                hs1 = slice(256 + h * 64, 256 + (h + 1) * 64)
                self.mm(b1[0:64, hs], kn[:, h, cs], kn[:, h, cs], True, True, kn.rk(c), [b1])
                self.mm(b1[0:64, hs1], kn[:, h, cs], qn[:, h, cs], True, True, [kn.rk(c), qn.rk(c)], [b1])
                self.mm(b2[0:64, hs], kn[:, h, cs], self.ident[0:64, 0:64], True, True, [kn.rk(c), self.ident], [b2])
            for m in range(2):
                self.mm(b2[0:64, 256 + m * 128:256 + (m + 1) * 128], vT[:, m, cs], self.ident[:, :], True, True, [vT.rk(c), self.ident], [b2])
            yield
            for h in range(4):
                hs = slice(h * 64, (h + 1) * 64)
                self.mm(b0[0:64, hs], self.gc(G_ONE, 64), GLt[:, hs], True, True, [GLt, gconst], [b0])
            self.tt("dve", tA[:], b1[0:64, H0], self.gc(SM), ALU.mult, [b1, gconst], [tA])
            self.tt("pool", v4(vb[:]), v4(vb[:]), v4(vb[:]), ALU.add, [vb], [vb]) if False else None
            yield
            self.tt("dve", v4(Em[:]), bc4(ball, c, d), v4(b0[0:64, H0]), ALU.subtract, [b0, ball], [Em])
            self.tt("dve", v4(EmT[:]), v4(b0[0:64, H0]), bc4(ball, c, d), ALU.subtract, [b0, ball], [EmT])
            yield
            self.tt("pool", Em[:], Em[:], self.gc(NMa), ALU.add, [Em, gconst], [Em])
            self.tt("pool", EmT[:], EmT[:], self.gc(NMb), ALU.add, [EmT, gconst], [EmT])
            self.tt("dve", v4(tA[:]), v4(tA[:]), bc4(beta, c, d), ALU.mult, [tA, beta], [tA])
            yield
            self.act(Em[:], Em[:], AF.Exp, [Em], [Em])
            self.act(EmT[:], EmT[:], AF.Exp, [EmT], [EmT])
            self.tt("dve", v4(vb[:]), v4(b2[0:64, H1]), bc4(beta, c, d), ALU.mult, [b2, beta], [vb])
            self.tt("dve", v4(kbe[:]), v4(b2[0:64, H0]), bc4(ebeta, c, d), ALU.mult, [b2, ebeta], [kbe])
            self.tt("dve", v4(kd_[i][:]), v4(b2[0:64, H0]), bc4(ekd, c, d), ALU.mult, [b2, ekd], [kd_[i]])
            yield
            self.tt("pool", XT[0][:], tA[:], Em[:], ALU.mult, [tA, Em], [XT[0]])
            self.tt("dve", KQm_[i][:], b1[0:64, H1], EmT[:], ALU.mult, [b1, EmT], [KQm_[i]])
            yield
            for h in range(4):
                hs = slice(h * 64, (h + 1) * 64)
                self.mm(b1[0:64, 256 + h * 64:256 + (h + 1) * 64], XT[0][:, hs], self.ident[0:64, 0:64], True, True, [XT[0], self.ident], [b1])
            yield
            self.copy("act", X[0][:], b1[0:64, H1], [b1], [X[0]])
            yield
            self.tt("pool", Pm[0][:], X[0][:], self.gc(G_ID), ALU.add, [X[0], gconst], [Pm[0]])
            cur = 0
            pc = 0

            def p_update(xt_tile, pc):
                for h in range(4):
                    hs = slice(h * 64, (h + 1) * 64)
                    self.mm(b1[0:64, hs], xt_tile[:, hs], Pm[pc][:, hs], True, True, [xt_tile, Pm[pc]], [b1])
                return 1 - pc

            def p_add(pc_old):
                self.tt("dve", Pm[1 - pc_old][:], b1[0:64, H0], Pm[pc_old][:], ALU.add, [b1, Pm[pc_old]], [Pm[1 - pc_old]])

            for k in range(5):
                nxt = 1 - cur
                last = (k == 4)
                for h in range(4):
                    hs = slice(h * 64, (h + 1) * 64)
                    hs1 = slice(256 + h * 64, 256 + (h + 1) * 64)
                    if not last:
                        self.mm(b0[0:64, hs], XT[cur][:, hs], X[cur][:, hs], True, True, [XT[cur], X[cur]], [b0])
                    self.mm(b0[0:64, hs1], X[cur][:, hs], XT[cur][:, hs], True, True, [XT[cur], X[cur]], [b0])
                pc_old = pc
                if k >= 1:
                    pc = p_update(XT[cur], pc)
                yield
                if not last:
                    self.copy("act", X[nxt][:], b0[0:64, H0], [b0], [X[nxt]])
                self.copy("dve", XT[nxt][:], b0[0:64, H1], [b0], [XT[nxt]])
                if k >= 1:
                    p_add(pc_old)
                cur = nxt
                yield
            pc_old = pc
            pc = p_update(XT[cur], pc)
            yield
            p_add(pc_old)
            assert pc == 1
            TT = Pm[1]
            yield
            for h in range(4):
                hs = slice(h * 64, (h + 1) * 64)
                hs1 = slice(256 + h * 64, 256 + (h + 1) * 64)
                self.mm(b0[0:64, hs], TT[:, hs], vb[:, hs], True, True, [TT, vb], [b0])
                self.mm(b0[0:64, hs1], kbe[:, hs], TT[:, hs], True, True, [TT, kbe], [b0])
            yield
            self.copy("act", u_[i][:], b0[0:64, H0], [b0], [u_[i]])
            self.copy("dve", wT_[i][:], b0[0:64, H1], [b0], [wT_[i]])

        def seq(c, d, i, si, step):
            cs = slice(c * 64, (c + 1) * 64)
            Sc = Sst[si]; Sn = Sst[1 - si]
            for h in range(4):
                hs = slice(h * 64, (h + 1) * 64)
                self.mm(pG[0:64, hs], wT_[i][:, hs], Sc[:, hs], True, True, [wT_[i], Sc], [pG])
            self.tt("dve", vnew[:], u_[i][:], pG[0:64, H0], ALU.subtract, [u_[i], pG], [vnew])
            for h in range(4):
                hs = slice(h * 64, (h + 1) * 64)
                hs1 = slice(256 + h * 64, 256 + (h + 1) * 64)
                self.mm(pG[0:64, hs1], kd_[i][:, hs], vnew[:, hs], True, True, [kd_[i], vnew], [pG])
            self.tt("dve", v4(St[:]), v4(Sc[:]), bc4(etot, c, d), ALU.mult, [Sc, etot], [St])
            self.tt("dve", Sn[:], St[:], pG[0:64, H1], ALU.add, [St, pG], [Sn])
            for h in range(4):
                hs = slice(h * 64, (h + 1) * 64)
                hs1 = slice(256 + h * 64, 256 + (h + 1) * 64)
                self.mm(pH[0:64, hs], qn[:, h, cs], Sc[:, hs], True, True, [qn.rk(c), Sc], [pH])
                self.mm(pH[0:64, hs1], KQm_[i][:, hs], vnew[:, hs], True, True, [KQm_[i], vnew], [pH])
            ob = osb[step % 2]
            self.copy("act", o2sb[:], pH[0:64, H1], [pH], [o2sb])
            self.tt("dve", v4(ob[:]), v4(pH[0:64, H0]), bc4(eb, c, d), ALU.mult, [pH, eb], [ob])
            self.tt("pool", ob[:], ob[:], o2sb[:], ALU.add, [ob, o2sb], [ob])
            if d == 0:
                self.dma("sp", OF[cs, :], ob[:], reads=[ob], writes=OF.rk(c))
            else:
                of = ofl[step % 2]; ga = gat[step % 2]
                self.dma("sp", of[:], OF[cs, :], reads=OF.rk(c), writes=[of])
                self.dma("sp", ga[:], self.PT[cs, PT_GATE:PT_GATE + 256], reads=[self.PT], writes=[ga])
                self.tt("pool", ob[:], ob[:], of[:], ALU.add, [ob, of], [ob])
                self.tt("pool", sqo[:], ob[:], ob[:], ALU.mult, [ob], [sqo])
                self.op("dve", lambda e: e.tensor_reduce(out=rs4[:, 0:4], in_=v4(sqo[:]), axis=AX.X, op=ALU.add), [sqo], [rs4])
                self.act(rs4[:, 4:8], rs4[:, 0:4], AF.Sqrt, [rs4], [rs4], scale=1.0 / 64, bias=self.eps_t[0:64, 0:1])
                self.op("dve", lambda e: e.reciprocal(out=rs4[:, 0:4], in_=rs4[:, 4:8]), [rs4], [rs4])
                self.tt("dve", v4(ysb[:]), v4(ob[:]), rs4[:, 0:4].unsqueeze(2).to_broadcast([64, 4, 64]), ALU.mult, [ob, rs4], [ysb])
                self.tt("pool", ysb[:], ysb[:], gng[:].rearrange("p h n -> p (h n)"), ALU.mult, [ysb, gng], [ysb])
                self.act(ga[:], ga[:], AF.Silu, [ga], [ga])
                self.tt("dve", ysb[:], ysb[:], ga[:], ALU.mult, [ysb, ga], [ysb])
                for m in range(2):
                    self.mm(pH[:, m * 64:(m + 1) * 64], ysb[:, m * 128:(m + 1) * 128], self.ident[0:64, 0:64], True, True, [ysb, self.ident], [pH])
                rb_ = rob[step % 2]
                self.copy("act", rb_[:], pH[:, 0:128].rearrange("p (m t) -> p m t", m=2), [pH], [rb_])
                self.dma("sp", self.MO[0:256, cs].rearrange("(m p) t -> p m t", p=128), rb_[:], reads=[rb_], writes=[self.MO])

        def run_interleaved(gens, extra=()):
            gens = list(gens)
            extra = list(extra)
            rnd = 0
            while gens:
                for g_ in list(gens):
                    try:
                        next(g_)
                    except StopIteration:
                        gens.remove(g_)
                if extra and rnd in (1, 12):
                    extra.pop(0)()
                rnd += 1
            for f_ in extra:
                f_()

        for d in range(2):
            order = list(range(NCH)) if d == 0 else [3, 2, 1, 0] + list(range(NCH - 1, 3, -1))
            self.memset("pool", Sst[0][:], 0.0, [Sst[0]])
            n = len(order)
            run_interleaved([local(order[0], d, 0, 0), local(order[1], d, 1, 1)])
            for s0 in range(0, n, 2):
                gens = []
                for q_ in range(2):
                    s2 = s0 + 2 + q_
                    if s2 < n:
                        gens.append(local(order[s2], d, s2 % NOB, q_))
                extra = []
                for q_ in range(2):
                    s1 = s0 + q_
                    if s1 < n:
                        extra.append(lambda s1=s1: seq(order[s1], d, s1 % NOB, s1 % 2, s1))
                run_interleaved(gens, extra)
        self.pop()

    return dict(gc=gc, phase_gdn=phase_gdn)


for _k, _v in _gdn_methods().items():
    setattr(Kern, _k, _v)


def _lru_att_methods():
    def rev_ap(self, ap2d):
        n = ap2d.shape[1]
        last = ap2d[:, n - 1:n]
        return bass.AP(last.tensor, last.offset, [list(last.ap[0]), [-1, n]])

    def phase_lru(self, l):
        self.push()
        one_t = self.sb('one', [128, 1])
        self.memset("pool", one_t[:], 1.0, [one_t])
        xr = self.sb('xr', [128, 2, T])
        yg = self.sb('yg', [128, 2, T])
        A = self.sb('A', [128, 2, T])
        BX = self.sb('BX', [128, 2, T])
        H = [self.sb(f'H{i}', [128, 2, T]) for i in range(2)]
        cw = self.sb('cwl', [128, 2, 4])
        cb = self.sb('cbl', [128, 2])
        self.dma("sp", cw[:], self.inp['lru_conv_wT'][l].rearrange("(m p) k -> p m k", p=128), writes=[cw])
        for m in range(2):
            self.dma("sp", cb[:, m:m + 1], self.inp['lru_conv_b'][l, m * 128:(m + 1) * 128].rearrange("(p o) -> p o", o=1), writes=[cb])
        WB = self.sb('WB', [128, 8, 128])
        self.memset("pool", WB[:], 0.0, [WB])
        bri = self.sb('bri', [128, 8])
        lam = self.sb('lam', [128, 4])
        for d in range(2):
            for gi, (wn, bn) in enumerate((('lru_w_r', 'lru_b_r'), ('lru_w_i', 'lru_b_i'))):
                for m in range(2):
                    idx = (d * 2 + gi) * 2 + m
                    for hb in range(2):
                        self.dma("sp", WB[hb * 64:(hb + 1) * 64, idx, hb * 64:(hb + 1) * 64],
                                 self.inp[wn][l, d, 2 * m + hb], writes=[WB])
                    self.dma("sp", bri[:, idx:idx + 1],
                             self.inp[bn][l, d, m * 128:(m + 1) * 128].rearrange("(p o) -> p o", o=1), writes=[bri])
            for m in range(2):
                self.dma("sp", lam[:, d * 2 + m:d * 2 + m + 1],
                         self.inp['lru_lambda'][l, d, m * 128:(m + 1) * 128].rearrange("(p o) -> p o", o=1), writes=[lam])
        cch = self.sb('cch', [128, 4])
        self.act(cch[:], lam[:], AF.Exp, [lam], [cch], scale=-1.0)
        self.act(cch[:], cch[:], AF.Ln, [cch], [cch], bias=one_t[:, 0:1])
        self.ts("dve", cch[:], cch[:], -8.0, ALU.mult, [cch], [cch])
        self.push()
        raw = [self.sb(f'raw{i}', [128, 516]) for i in range(2)]
        t1 = self.sb('t1', [128, 512]); t2 = self.sb('t2', [128, 512])
        segs = [(0, T_CTX, [(0, 256)]), (T_CTX, T, [(T_CTX + i * 512, 512) for i in range(4)])]
        it = 0
        for m in range(2):
            row0 = PF_LRU + m * 128
            for (s0, s1, blks) in segs:
                for (t0, blk) in blks:
                    rw = raw[it % 2]
                    lo = max(t0 - 1, s0); hi = min(t0 + blk + 2, s1)
                    if lo > t0 - 1:
                        self.memset("pool", rw[:, 0:1], 0.0, [rw])
                    if hi < t0 + blk + 2:
                        self.memset("pool", rw[:, hi - (t0 - 1):blk + 3], 0.0, [rw])
                    self.dma("sp", rw[:, lo - (t0 - 1):hi - (t0 - 1)], self.PF[row0:row0 + 128, lo:hi], reads=[self.PF], writes=[rw])
                    dst = xr[:, m, t0:t0 + blk]
                    self.ts("dve", dst, rw[:, 0:blk], cw[:, m, 0:1], ALU.mult, [rw, cw, cb], [xr], s2=cb[:, m:m + 1], op1=ALU.add)
                    for k in range(1, 4):
                        self.stt(dst, rw[:, k:k + blk], cw[:, m, k:k + 1], dst, ALU.mult, ALU.add, [rw, cw, xr], [xr])
                    it += 1
                    rg = raw[it % 2]
                    self.dma("sp", rg[:, 0:blk], self.PF[row0 + 256:row0 + 384, t0:t0 + blk], reads=[self.PF], writes=[rg])
                    self.tt("pool", t1[:, 0:blk], rg[:, 0:blk], rg[:, 0:blk], ALU.mult, [rg], [t1])
                    self.ts("dve", t1[:, 0:blk], t1[:, 0:blk], 0.044715, ALU.mult, [t1], [t1], s2=1.0, op1=ALU.add)
                    self.tt("pool", t1[:, 0:blk], t1[:, 0:blk], rg[:, 0:blk], ALU.mult, [t1, rg], [t1])
                    self.act(t1[:, 0:blk], t1[:, 0:blk], AF.Tanh, [t1], [t1], scale=0.7978845608028654)
                    self.ts("dve", t2[:, 0:blk], rg[:, 0:blk], 0.5, ALU.mult, [rg], [t2])
                    self.stt(yg[:, m, t0:t0 + blk], t1[:, 0:blk], 1.0, t2[:, 0:blk], ALU.add, ALU.mult, [t1, t2], [yg])
                    it += 1
        self.pop()
        pr = [self.ps(f'pr{i}', [128, 512]) for i in range(2)]
        pi = [self.ps(f'pi{i}', [128, 512]) for i in range(2)]
        rt = self.sb('rt', [128, 512]); itl = self.sb('itl', [128, 512]); mt = self.sb('mt', [128, 512])
        blocks = [(0, 256)] + [(T_CTX + i * 512, 512) for i in range(4)]
        it = 0
        for d in range(2):
            for m in range(2):
                for (t0, blk) in blocks:
                    p1 = pr[it % 2]; p2 = pi[it % 2]
                    src = xr[:, m, t0:t0 + blk]
                    ir = (d * 2 + 0) * 2 + m; ii = (d * 2 + 1) * 2 + m
                    self.mm(p1[:, 0:blk], WB[:, ir, :], src, True, True, [WB, xr], [p1])
                    self.mm(p2[:, 0:blk], WB[:, ii, :], src, True, True, [WB, xr], [p2])
                    self.act(rt[:, 0:blk], p1[:, 0:blk], AF.Sigmoid, [p1, bri], [rt], bias=bri[:, ir:ir + 1])
                    self.act(itl[:, 0:blk], p2[:, 0:blk], AF.Sigmoid, [p2, bri], [itl], bias=bri[:, ii:ii + 1])
                    a_ = A[:, m, t0:t0 + blk]
                    self.act(a_, rt[:, 0:blk], AF.Exp, [rt, cch], [A], scale=cch[:, d * 2 + m:d * 2 + m + 1])
                    self.tt("pool", mt[:, 0:blk], a_, a_, ALU.mult, [A], [mt])
                    self.ts("dve", mt[:, 0:blk], mt[:, 0:blk], -1.0, ALU.mult, [mt], [mt], s2=1.0, op1=ALU.add)
                    self.act(mt[:, 0:blk], mt[:, 0:blk], AF.Sqrt, [mt], [mt])
                    self.tt("dve", mt[:, 0:blk], mt[:, 0:blk], itl[:, 0:blk], ALU.mult, [mt, itl], [mt])
                    self.tt("pool", BX[:, m, t0:t0 + blk], mt[:, 0:blk], src, ALU.mult, [mt, xr], [BX])
                    it += 1
                if d == 0:
                    self.op("dve", lambda e: e.tensor_tensor_scan(out=H[0][:, m, :], data0=A[:, m, :], data1=BX[:, m, :],
                                                                   initial=0.0, op0=ALU.mult, op1=ALU.add), [A, BX], [H[0]])
                else:
                    rv = self.rev_ap
                    self.op("dve", lambda e: e.tensor_tensor_scan(out=rv(H[1][:, m, 0:T_CTX]), data0=rv(A[:, m, 0:T_CTX]),
                                                                   data1=rv(BX[:, m, 0:T_CTX]), initial=0.0,
                                                                   op0=ALU.mult, op1=ALU.add), [A, BX], [H[1]])
                    self.op("dve", lambda e: e.tensor_tensor_scan(out=rv(H[1][:, m, T_CTX:T]), data0=rv(A[:, m, T_CTX:T]),
                                                                   data1=rv(BX[:, m, T_CTX:T]), initial=H[1][:, m, 0:1],
                                                                   op0=ALU.mult, op1=ALU.add), [A, BX, H[1]], [H[1]])
        for m in range(2):
            self.tt("pool", H[0][:, m, :], H[0][:, m, :], H[1][:, m, :], ALU.add, [H[0], H[1]], [H[0]])
            self.tt("dve", H[0][:, m, :], H[0][:, m, :], yg[:, m, :], ALU.mult, [H[0], yg], [H[0]])
        self.dma("sp", self.MO[256:512, :].rearrange("(m p) t -> p m t", p=128), H[0][:], reads=[H[0]], writes=[self.MO])
        self.pop()

    def phase_att(self, l):
        self.push()
        qT = self.sb('qT', [64, 8, T], F32R)
        kT = self.sb('kT', [64, 2, T], F32R)
        V = self.sb('V', [128, NT, 2, 65], F32R)
        onesf = self.sb('onesf', [128, NT * 2])
        self.memset("pool", onesf[:], 1.0, [onesf])
        self.copy("dve", V[:, :, :, 64], onesf[:].rearrange("p (t g) -> p t g", g=2), [onesf], [V])
        gbc = self.sb('gbc', [128, 10, 64])
        self.dma("sp", gbc[:, 0, :], self.inp['attn_q_norm_g'][l:l + 1, :].partition_broadcast(128), writes=[gbc])
        self.dma("sp", gbc[:, 8, :], self.inp['attn_k_norm_g'][l:l + 1, :].partition_broadcast(128), writes=[gbc])
        negc = self.sb('negc', [128, 4])
        self.op("dve", lambda e: e.tensor_reduce(out=negc[:, 0:1], in_=gbc[:, 0, :], axis=AX.X, op=ALU.max, apply_absolute_value=True), [gbc], [negc])
        self.op("dve", lambda e: e.tensor_reduce(out=negc[:, 1:2], in_=gbc[:, 8, :], axis=AX.X, op=ALU.max, apply_absolute_value=True), [gbc], [negc])
        self.tt("dve", negc[:, 2:3], negc[:, 0:1], negc[:, 1:2], ALU.mult, [negc], [negc])
        self.ts("dve", negc[:, 3:4], negc[:, 2:3], -8.0, ALU.mult, [negc], [negc])
        self.ts("dve", gbc[:, 0, :], gbc[:, 0, :], 0.125, ALU.mult, [gbc], [gbc])
        for h in range(1, 8):
            self.copy("dve", gbc[:, h, :], gbc[:, 0, :], [gbc], [gbc])
        self.copy("dve", gbc[:, 9, :], gbc[:, 8, :], [gbc], [gbc])
        self.push()
        at = [self.sb(f'at{i}', [128, 768]) for i in range(2)]
        an = [self.sb(f'an{i}', [128, 640]) for i in range(2)]
        sqa = self.sb('sqa', [128, 640])
        ssa = self.sb('ssa', [128, 20])
        cs_t = [self.sb(f'cs{i}', [128, 64]) for i in range(2)]
        r1 = self.sb('r1', [128, 10, 2, 16]); r2 = self.sb('r2', [128, 10, 2, 16])
        r3 = self.sb('r3', [128, 10, 2, 16]); r4 = self.sb('r4', [128, 10, 2, 16])
        ptq = [self.ps(f'ptq{i}', [128, 512]) for i in range(3)]
        for tt in range(NT):
            a_ = at[tt % 2]; n_ = an[tt % 2]
            self.dma("sp", a_[:], self.PT[tt * 128:(tt + 1) * 128, PT_ATT:PT_ATT + 768], reads=self.PT.rk(tt), writes=[a_])
            self.tt("pool", sqa[:], a_[:, 0:640], a_[:, 0:640], ALU.mult, [a_], [sqa])
            self.op("dve", lambda e: e.tensor_reduce(out=ssa[:, 0:10], in_=sqa[:].rearrange("p (h n) -> p h n", h=10), axis=AX.X, op=ALU.add), [sqa], [ssa])
            self.act(ssa[:, 10:20], ssa[:, 0:10], AF.Sqrt, [ssa], [ssa], scale=1.0 / 64, bias=self.eps_t[:, 0:1])
            self.op("dve", lambda e: e.reciprocal(out=ssa[:, 0:10], in_=ssa[:, 10:20]), [ssa], [ssa])
            n3 = n_[:].rearrange("p (h n) -> p h n", h=10)
            self.tt("dve", n3, a_[:, 0:640].rearrange("p (h n) -> p h n", h=10), ssa[:, 0:10].unsqueeze(2).to_broadcast([128, 10, 64]), ALU.mult, [a_, ssa], [n_])
            self.tt("pool", n_[:], n_[:], gbc[:].rearrange("p h n -> p (h n)"), ALU.mult, [n_, gbc], [n_])
            if tt >= 2:
                c_ = cs_t[tt % 2]
                lt = tt - 2
                self.dma("sp", c_[:], self.inp['rope_cs'][lt * 128:(lt + 1) * 128, :], writes=[c_])
                n5 = n_[:].rearrange("p (h a f n) -> p h a f n", h=10, a=2, f=2)
                x1 = n5[:, :, :, 0, :]; x2 = n5[:, :, :, 1, :]
                cosb = c_[:, 0:32].rearrange("p (a n) -> p a n", a=2).unsqueeze(1).to_broadcast([128, 10, 2, 16])
                sinb = c_[:, 32:64].rearrange("p (a n) -> p a n", a=2).unsqueeze(1).to_broadcast([128, 10, 2, 16])
                self.tt("dve", r1[:], x1, cosb, ALU.mult, [n_, c_], [r1])
                self.tt("pool", r2[:], x2, sinb, ALU.mult, [n_, c_], [r2])
                self.tt("dve", r3[:], x1, sinb, ALU.mult, [n_, c_], [r3])
                self.tt("pool", r4[:], x2, cosb, ALU.mult, [n_, c_], [r4])
                self.tt("dve", x1, r1[:], r2[:], ALU.subtract, [r1, r2], [n_])
                self.tt("pool", x2, r3[:], r4[:], ALU.add, [r3, r4], [n_])
            ts_ = slice(tt * 128, (tt + 1) * 128)
            for grp in range(3):
                p = ptq[grp]
                hs = range(4) if grp < 2 else range(2)
                for j in hs:
                    h = grp * 4 + j
                    self.mm(p[0:64, j * 128:(j + 1) * 128], n_[:, h * 64:(h + 1) * 64], self.ident[:, :], True, True, [n_, self.ident], [p])
                if grp < 2:
                    self.evac(grp, qT[:, grp * 4:(grp + 1) * 4, ts_], p[0:64, :].rearrange("p (h t) -> p h t", h=4), [p], [qT])
                else:
                    self.evac(grp, kT[:, :, ts_], p[0:64, 0:256].rearrange("p (h t) -> p h t", h=2), [p], [kT])
            self.copy("dve", V[:, tt, :, 0:64], a_[:, 640:768].rearrange("p (g n) -> p g n", g=2), [a_], [V])
        self.pop()
        pS = [self.ps(f'pS{i}', [128, 512]) for i in range(3)]
        pO = [self.ps(f'pO{i}', [128, 512]) for i in range(2)]
        pBc = self.ps('pBc', [128, 512])
        ones64 = self.sb('ones64', [128, 64])
        self.memset("pool", ones64[:], 1.0, [ones64])
        Pt = [self.sb(f'Pt{i}', [128, 512], F32R) for i in range(4)]
        rsb = self.sb('rsb', [128, 512]); bcs = self.sb('bcs', [64, 512])
        osg = [self.sb(f'osg{i}', [64, 512]) for i in range(2)]
        qblocks = [(0, 256, [0, 1])] + [(T_CTX + i * 512, 512, list(range(NT))) for i in range(4)]
        ip = 0; io = 0
        for (q0, qn_, ktiles) in qblocks:
            for h in range(8):
                g = h // 4
                po = pO[io % 2]
                pend = []
                nk = len(ktiles)

                def pv(item):
                    pkt, ppt, pki = item
                    self.mm(po[0:65, 0:qn_], V[:, pkt, g, :], ppt[:, 0:qn_], pki == 0, pki == nk - 1, [V, ppt], [po])

                for ki, kt in enumerate(ktiles):
                    ps_ = pS[ip % 3]; pt = Pt[ip % 4]
                    self.mm(ps_[:, 0:qn_], kT[:, g, kt * 128:(kt + 1) * 128], qT[:, h, q0:q0 + qn_], True, True, [kT, qT], [ps_])
                    self.act(pt[:, 0:qn_], ps_[:, 0:qn_], AF.Exp, [ps_, negc], [pt], bias=negc[:, 3:4])
                    pend.append((kt, pt, ki))
                    if len(pend) > 2:
                        pv(pend.pop(0))
                    ip += 1
                while pend:
                    pv(pend.pop(0))
                self.op("dve", lambda e: e.reciprocal(out=rsb[64:65, 0:qn_], in_=po[64:65, 0:qn_]), [po], [rsb])
                self.mm(pBc[0:64, 0:qn_], onesf[64:65, 0:64 - 28] if False else ones64[64:65, 0:64], rsb[64:65, 0:qn_], True, True, [ones64, rsb], [pBc])
                self.copy("act", bcs[:, 0:qn_], pBc[0:64, 0:qn_], [pBc], [bcs])
                og = osg[io % 2]
                self.tt("dve", og[:, 0:qn_], po[0:64, 0:qn_], bcs[:, 0:qn_], ALU.mult, [po, bcs], [og])
                self.dma("sp", self.MO[512 + h * 64:512 + (h + 1) * 64, q0:q0 + qn_], og[:, 0:qn_], reads=[og], writes=[self.MO])
                io += 1
        self.pop()

    return dict(rev_ap=rev_ap, phase_lru=phase_lru, phase_att=phase_att)


for _k, _v in _lru_att_methods().items():
    setattr(Kern, _k, _v)


N_OV = 36
N_SLOT = (64 + N_OV) * 128


def _tail_methods():
    def phase_wout(self, l):
        self.push()
        wo = self.sb('wo', [128, 8, D], F32R)
        self.dma("pool", wo[:], self.inp['w_out'][l].rearrange("(kc p) n -> p kc n", p=128), writes=[wo])
        g1 = [self.sb(f'g1{r}', [128, D]) for r in range(2)]
        for r in range(2):
            self.load_mod(g1[r], 2, r)
        mo = [self.sb(f'mo{i}', [128, 8, 512], F32R) for i in range(2)]
        xt = [self.sb(f'xw{i}', [128, D]) for i in range(2)]
        tw = [self.sb(f'tw{i}', [128, D]) for i in range(2)]
        pw = [self.ps(f'pw{i}', [128, 512]) for i in range(4)]
        blocks = [(0, 2), (2, 4), (6, 4), (10, 4), (14, 4)]
        msrc = self.MO[:, :].rearrange("(kc p) t -> p kc t", p=128)
        it = 0
        for bi, (t0, ntile) in enumerate(blocks):
            r = 1 if t0 == 0 else 0
            m_ = mo[bi % 2]
            ntok = ntile * 128
            self.dma("pool", m_[:, :, 0:ntok], msrc[:, :, t0 * 128:t0 * 128 + ntok], reads=[self.MO], writes=[m_])
            for j in range(ntile):
                tt = t0 + j
                x_ = xt[it % 2]; t_ = tw[it % 2]
                self.dma("sp", x_[:], self.XS[tt * 128:(tt + 1) * 128, :], reads=self.XS.rk(tt), writes=[x_])
                for half in range(2):
                    p = pw[(it % 2) * 2 + half]
                    for kc in range(8):
                        self.mm(p[:], m_[:, kc, j * 128:(j + 1) * 128], wo[:, kc, half * 512:(half + 1) * 512], kc == 0, kc == 7, [m_, wo], [p])
                    hs = slice(half * 512, (half + 1) * 512)
                    self.tt("dve", t_[:, hs], p[:], g1[r][:, hs], ALU.mult, [p, g1[r]], [t_])
                self.tt("pool", x_[:], x_[:], t_[:], ALU.add, [x_, t_], [x_])
                self.dma("sp", self.XS[tt * 128:(tt + 1) * 128, :], x_[:], reads=[x_], writes=self.XS.rk(tt))
                it += 1
        self.pop()

    def phase_moe(self, l):
        self.push()
        mc = self.sb('mc', [128, 256])
        self.dma("sp", mc[:], self.inp['moe_const'][:, :], writes=[mc])
        iota64 = mc[:, 0:64]; thr = mc[:, 64:100]; bvals = mc[:, 100:136]; pidx = mc[:, 136:137]; ones64 = mc[:, 137:201]
        ustr = self.sb('ustr', [128, 256])
        self.dma("sp", ustr[:], self.inp['moe_tri'][:, :], writes=[ustr])
        OH = self.sb('OH', [128, NT, 2, 64])
        gates = self.sb('gates', [128, NT, 2])
        dest = self.sb('dest', [128, NT, 2], I32)
        wix = self.sb('wix', [128, 2, N_OV], I32)
        XB, YB = self.XB, self.YB
        self.push()
        gm = [self.sb(f'gm2{r}', [128, D]) for r in range(2)]
        sh = [self.sb(f'sh2{r}', [128, D]) for r in range(2)]
        self.push()
        for r in range(2):
            self.load_mod(gm[r], 4, r, 'norm2_g', l)
            self.load_mod(sh[r], 3, r)
        self.pop()
        hbuf = self.sb('hbuf', [128, NT, D], nsub=NT)
        wr = self.sb('wr', [128, 8, 72])
        self.dma("sp", wr[:, :, 0:8], self.inp['moe_w_group'][l].rearrange("(kc p) n -> p kc n", p=128), writes=[wr])
        self.dma("sp", wr[:, :, 8:72], self.inp['moe_w_expert'][l].rearrange("(kc p) n -> p kc n", p=128), writes=[wr])
        rb = self.sb('rb', [128, 72])
        self.dma("sp", rb[:, 0:8], self.inp['moe_b_group'][l:l + 1, :].partition_broadcast(128), writes=[rb])
        self.dma("sp", rb[:, 8:72], self.inp['moe_b_expert'][l:l + 1, :].partition_broadcast(128), writes=[rb])
        xt = [self.sb(f'xm{i}', [128, D]) for i in range(2)]
        junk = self.sb('junk', [128, D])
        ss = [self.sb(f'ssm{i}', [128, 4]) for i in range(2)]
        hT = self.sb('hT', [128, 8, 128])
        ptp = [self.ps(f'ptp{i}', [128, 512]) for i in range(2)]
        plg = self.ps('plg', [128, 72])
        prk = self.ps('prk', [128, 64]); pcs = self.ps('pcs', [128, 64])
        lg = self.sb('lg', [128, 72]); sm = self.sb('sm', [128, 16])
        ohg = self.sb('ohg', [128, 8]); tmp64 = self.sb('tmp64', [128, 64]); le = self.sb('le', [128, 8]); le2 = self.sb('le2', [128, 8])
        oh1 = self.sb('oh1', [128, 8]); oh2 = self.sb('oh2', [128, 8]); eg = self.sb('eg', [128, 8])
        for tt in range(NT):
            r = 1 if tt < 2 else 0
            x_ = xt[tt % 2]; ss_ = ss[tt % 2]
            self.dma("sp", x_[:], self.XS[tt * 128:(tt + 1) * 128, :], reads=self.XS.rk(tt), writes=[x_])
            hcur = Tl(hbuf[:, tt, :], 'hcur'); hcur.rs = hbuf.rk(tt)
            self.norm_tile(x_, gm[r], sh[r], hcur, junk, ss_)
            for half in range(2):
                p = ptp[half]
                for q in range(4):
                    kc = half * 4 + q
                    self.tr(p[:, q * 128:(q + 1) * 128], hbuf[:, tt, kc * 128:(kc + 1) * 128], self.ident[:], [hbuf.rk(tt), self.ident], [p])
                self.evac(half, hT[:, half * 4:(half + 1) * 4, :], p[:].rearrange("p (q t) -> p q t", q=4), [p], [hT])
            for kc in range(8):
                self.mm(plg[:], hT[:, kc, :], wr[:, kc, :], kc == 0, kc == 7, [hT, wr], [plg])
            self.tt("dve", lg[:], plg[:], rb[:], ALU.add, [plg, rb], [lg])
            self.op("dve", lambda e: e.tensor_reduce(out=sm[:, 0:1], in_=lg[:, 0:8], axis=AX.X, op=ALU.max), [lg], [sm])
            self.ts("dve", ohg[:], lg[:, 0:8], sm[:, 0:1], ALU.is_equal, [lg, sm], [ohg])
            self.ts("dve", sm[:, 1:2], sm[:, 0:1], -1.0, ALU.mult, [sm], [sm])
            self.act(eg[:], lg[:, 0:8], AF.Exp, [lg, sm], [eg, sm], bias=sm[:, 1:2], accum_out=sm[:, 2:3])
            self.op("dve", lambda e: e.reciprocal(out=sm[:, 3:4], in_=sm[:, 2:3]), [sm], [sm])
            self.tt("dve", tmp64[:].rearrange("p (g e) -> p g e", g=8), lg[:, 8:72].rearrange("p (g e) -> p g e", g=8),
                    ohg[:].unsqueeze(2).to_broadcast([128, 8, 8]), ALU.mult, [lg, ohg], [tmp64])
            self.op("dve", lambda e: e.tensor_reduce(out=le[:], in_=tmp64[:].rearrange("p (g e) -> p e g", g=8), axis=AX.X, op=ALU.add), [tmp64], [le])
            self.op("dve", lambda e: e.tensor_reduce(out=sm[:, 4:5], in_=le[:], axis=AX.X, op=ALU.max), [le], [sm])
            self.ts("dve", oh1[:], le[:], sm[:, 4:5], ALU.is_equal, [le, sm], [oh1])
            self.stt(le2[:], oh1[:], -1.0e30, le[:], ALU.mult, ALU.add, [oh1, le], [le2])
            self.op("dve", lambda e: e.tensor_reduce(out=sm[:, 5:6], in_=le2[:], axis=AX.X, op=ALU.max), [le2], [sm])
            self.ts("dve", oh2[:], le2[:], sm[:, 5:6], ALU.is_equal, [le2, sm], [oh2])
            self.tt("dve", sm[:, 6:7], sm[:, 5:6], sm[:, 4:5], ALU.subtract, [sm], [sm])
            self.act(sm[:, 7:8], sm[:, 6:7], AF.Exp, [sm], [sm])
            self.ts("dve", sm[:, 8:9], sm[:, 7:8], 1.0, ALU.add, [sm], [sm])
            self.op("dve", lambda e: e.reciprocal(out=sm[:, 9:10], in_=sm[:, 8:9]), [sm], [sm])
            self.tt("dve", gates[:, tt, 0:1], sm[:, 3:4], sm[:, 9:10], ALU.mult, [sm], [gates])
            self.tt("dve", gates[:, tt, 1:2], gates[:, tt, 0:1], sm[:, 7:8], ALU.mult, [sm, gates], [gates])
            for k, ohk in enumerate((oh1, oh2)):
                self.tt("dve", OH[:, tt, k, :].rearrange("p (g e) -> p g e", g=8), ohg[:].unsqueeze(2).to_broadcast([128, 8, 8]),
                        ohk[:].unsqueeze(1).to_broadcast([128, 8, 8]), ALU.mult, [ohg, ohk], [OH])
        Osum = self.sb('Osum', [128, NT, 64])
        self.tt("dve", Osum[:], OH[:, :, 0, :], OH[:, :, 1, :], ALU.add, [OH], [Osum])
        rank = self.sb('rank', [128, NT, 64])
        pref = self.sb('pref', [128, 64])
        self.memset("pool", pref[:], 0.0, [pref])
        for tt in range(NT):
            self.mm(prk[:], ustr[:, 0:128], Osum[:, tt, :], True, True, [ustr, Osum], [prk])
            self.mm(pcs[:], ustr[:, 128:256], Osum[:, tt, :], True, True, [ustr, Osum], [pcs])
            self.tt("dve", rank[:, tt, :], prk[:], pref[:], ALU.add, [prk, pref], [rank])
            self.tt("dve", pref[:], pcs[:], pref[:], ALU.add, [pcs, pref], [pref])
        cmpb = self.sb('cmpb', [128, 64, 36])
        nblk = self.sb('nblk', [128, 64]); ovf = self.sb('ovf', [128, 64]); incl = self.sb('incl', [128, 64]); delta = self.sb('delta', [128, 64])
        self.tt("dve", cmpb[:], pref[:].unsqueeze(2).to_broadcast([128, 64, 36]), thr.unsqueeze(1).to_broadcast([128, 64, 36]), ALU.is_gt, [pref, mc], [cmpb])
        self.op("dve", lambda e: e.tensor_reduce(out=nblk[:], in_=cmpb[:], axis=AX.X, op=ALU.add), [cmpb], [nblk])
        self.ts("dve", ovf[:], nblk[:], -1.0, ALU.add, [nblk], [ovf], s2=0.0, op1=ALU.max)
        self.op("dve", lambda e: e.tensor_tensor_scan(out=incl[:], data0=ones64, data1=ovf[:], initial=0.0, op0=ALU.mult, op1=ALU.add), [mc, ovf], [incl])
        self.tt("dve", delta[:], incl[:], ovf[:], ALU.subtract, [incl, ovf], [delta])
        self.tt("dve", delta[:], delta[:], iota64, ALU.subtract, [delta, mc], [delta])
        self.ts("dve", delta[:], delta[:], 128.0, ALU.mult, [delta], [delta], s2=8064.0, op1=ALU.add)
        base1 = self.sb('base1', [128, 64])
        self.ts("dve", base1[:], iota64, 128.0, ALU.mult, [mc], [base1])
        dall = self.sb('dall', [128, NT, 64]); ge = self.sb('ge', [128, NT, 64]); destf = self.sb('destf', [128, NT, 2])
        self.ts("dve", ge[:], rank[:], 128.0, ALU.is_ge, [rank], [ge])
        self.tt("dve", ge[:], ge[:], delta[:].unsqueeze(1).to_broadcast([128, NT, 64]), ALU.mult, [ge, delta], [ge])
        self.tt("dve", dall[:], rank[:], base1[:].unsqueeze(1).to_broadcast([128, NT, 64]), ALU.add, [rank, base1], [dall])
        self.tt("dve", dall[:], dall[:], ge[:], ALU.add, [dall, ge], [dall])
        for k in range(2):
            self.tt("dve", ge[:], OH[:, :, k, :], dall[:], ALU.mult, [OH, dall], [ge])
            self.op("dve", lambda e: e.tensor_reduce(out=destf[:, :, k], in_=ge[:], axis=AX.X, op=ALU.add), [ge], [destf])
        self.copy("dve", dest[:], destf[:], [destf], [dest])
        cmp2 = self.sb('cmp2', [128, 36, 64]); bke = self.sb('bke', [128, 36]); wixf = self.sb('wixf', [128, 2, 36])
        self.tt("dve", cmp2[:], incl[:].unsqueeze(1).to_broadcast([128, 36, 64]), bvals.unsqueeze(2).to_broadcast([128, 36, 64]), ALU.is_le, [incl, mc], [cmp2])
        self.op("dve", lambda e: e.tensor_reduce(out=bke[:], in_=cmp2[:], axis=AX.X, op=ALU.add), [cmp2], [bke])
        p2 = self.sb('p2', [128, 1])
        self.ts("dve", p2[:], pidx, 2.0, ALU.mult, [mc], [p2])
        self.ts("dve", wixf[:, 0, :], bke[:], 256.0, ALU.mult, [bke, p2], [wixf], s2=p2[:, 0:1], op1=ALU.add)
        self.ts("dve", wixf[:, 1, :], bke[:], 256.0, ALU.mult, [bke, p2], [wixf], s2=p2[:, 0:1], op1=ALU.add)
        self.copy("dve", wix[:], wixf[:], [wixf], [wix])
        for tt in range(NT):
            for k in range(2):
                self.S.dma("pool", lambda e, tt=tt, k=k: e.indirect_dma_start(
                    out=XB[:, :], out_offset=bass.IndirectOffsetOnAxis(ap=dest[:, tt, k:k + 1], axis=0),
                    in_=hbuf[:, tt, :], in_offset=None), _flat([hbuf.rk(tt), dest]), _flat([XB]))
        self.pop()
        self.push()
        NWB = 3
        w1b = [self.sb(f'w1b{i}', [128, 8, 512], F32R) for i in range(NWB)]
        w3b = [self.sb(f'w3b{i}', [128, 8, 512], F32R) for i in range(NWB)]
        w2b = [self.sb(f'w2b{i}', [128, 4, D], F32R) for i in range(NWB)]
        xb = [self.sb(f'xb{i}', [128, D]) for i in range(2)]
        xbT = self.sb('xbT', [128, 8, 128], F32R)
        a1 = self.sb('a1', [128, 512]); hh = self.sb('hh', [128, 512])
        hhT = self.sb('hhT', [128, 4, 128], F32R)
        yb = [self.sb(f'yb{i}', [128, D]) for i in range(2)]
        ptp = [self.ps(f'ptq{i}', [128, 512]) for i in range(2)]
        ph1 = self.ps('ph1', [128, 512]); ph3 = self.ps('ph3', [128, 512]); pht = self.ps('pht', [128, 512])
        py = [self.ps(f'py{i}', [128, 512]) for i in range(2)]
        w1v = self.inp['moe_w1'].rearrange("l e (r kc) n -> (l e r) (kc n)", kc=4)
        w3v = self.inp['moe_w3'].rearrange("l e (r kc) n -> (l e r) (kc n)", kc=4)
        w2v = self.inp['moe_w2'].rearrange("l e (r c) n -> (l e r) (c n)", c=2)
        loff = l * 64 * 1024 * 512
        if not hasattr(self, 'reg_b13'):
            self.reg_b13 = self.nc.gpsimd.to_reg(16383)
            self.reg_b2 = self.nc.gpsimd.to_reg(32767)
        reg_b13, reg_b2 = self.reg_b13, self.reg_b2
        order = []
        nxt2 = 0
        for e_ in range(64):
            order.append(e_)
            while nxt2 < N_OV and (nxt2 + 1) * 64 <= (e_ + 1) * N_OV:
                order.append(64 + nxt2)
                nxt2 += 1
        assert len(order) == 64 + N_OV and nxt2 == N_OV
        def load_w(pos, blk):
            iw = pos % NWB
            W1, W3, W2 = w1b[iw], w3b[iw], w2b[iw]
            if blk < 64:
                self.dma("pool", W1[:], self.inp['moe_w1'][l, blk].rearrange("(p kc) n -> p kc n", kc=8), writes=[W1], max_dma_last_dim=8192)
                self.dma("pool", W3[:], self.inp['moe_w3'][l, blk].rearrange("(p kc) n -> p kc n", kc=8), writes=[W3], max_dma_last_dim=8192)
                self.dma("pool", W2[:], self.inp['moe_w2'][l, blk].rearrange("(p kc) n -> p kc n", kc=4), writes=[W2], max_dma_last_dim=8192)
            else:
                b = blk - 64
                for (Wt, wv) in ((W1, w1v), (W3, w3v), (W2, w2v)):
                    w2d = Wt[:].rearrange("p k n -> p (k n)")
                    for half in range(2):
                        self.S.dma("pool", lambda e, w2d=w2d, wv=wv, half=half, b=b: e.indirect_dma_start(
                            out=w2d[:, half * 2048:(half + 1) * 2048], out_offset=None, in_=wv[:, :],
                            in_offset=bass.IndirectOffsetOnAxis(ap=wix[:, 0, b:b + 1], axis=0),
                            element_offset=loff + half * 2048, bounds_check=reg_b13, oob_is_err=False), _flat([wix]), _flat([Wt]))

        def stage1(pos, blk):
            x_ = xb[pos % 2]
            self.dma("sp", x_[:], XB[blk * 128:(blk + 1) * 128, :], reads=[XB], writes=[x_])
            for half in range(2):
                p = ptp[half]
                for q in range(4):
                    kc = half * 4 + q
                    self.tr(p[:, q * 128:(q + 1) * 128], x_[:, kc:D:8], self.ident[:], [x_, self.ident], [p])
                self.evac(half, xbT[:, half * 4:(half + 1) * 4, :], p[:].rearrange("p (q t) -> p q t", q=4), [p], [xbT])

        def stage2(pos, blk):
            iw = pos % NWB
            W1, W3 = w1b[iw], w3b[iw]
            for kc in range(8):
                self.mm(ph1[:], xbT[:, kc, :], W1[:, kc, :], kc == 0, kc == 7, [xbT, W1], [ph1])
            for kc in range(8):
                self.mm(ph3[:], xbT[:, kc, :], W3[:, kc, :], kc == 0, kc == 7, [xbT, W3], [ph3])
            self.act(a1[:], ph1[:], AF.Silu, [ph1], [a1])
            self.tt("dve", hh[:], a1[:], ph3[:], ALU.mult, [a1, ph3], [hh])

        def stage34(pos, blk):
            iw = pos % NWB
            W2 = w2b[iw]
            for q in range(4):
                self.tr(pht[:, q * 128:(q + 1) * 128], hh[:, q:512:4], self.ident[:], [hh, self.ident], [pht])
            self.copy("act", hhT[:], pht[:].rearrange("p (q t) -> p q t", q=4), [pht], [hhT])
            y_ = yb[pos % 2]
            for half in range(2):
                p = py[half]
                for c in range(4):
                    self.mm(p[:], hhT[:, c, :], W2[:, c, half * 512:(half + 1) * 512], c == 0, c == 3, [hhT, W2], [p])
                self.evac(half, y_[:, half * 512:(half + 1) * 512], p[:], [p], [y_])
            self.dma("sp", YB[blk * 128:(blk + 1) * 128, :], y_[:], reads=[y_], writes=[YB])

        nb = len(order)
        load_w(0, order[0])
        if nb > 1:
            load_w(1, order[1])
        stage1(0, order[0])
        for pos, blk in enumerate(order):
            if pos + 2 < nb:
                load_w(pos + 2, order[pos + 2])
            stage2(pos, blk)
            if pos + 1 < nb:
                stage1(pos + 1, order[pos + 1])
            stage34(pos, blk)
        self.pop()
        self.push()
        g2 = [self.sb(f'g2{r}', [128, D]) for r in range(2)]
        for r in range(2):
            self.load_mod(g2[r], 5, r)
        Y0 = [self.sb(f'Y0{i}', [128, D]) for i in range(2)]
        Y1 = [self.sb(f'Y1{i}', [128, D]) for i in range(2)]
        xt = [self.sb(f'xf{i}', [128, D]) for i in range(2)]
        for tt in range(NT):
            r = 1 if tt < 2 else 0
            i = tt % 2
            for k, Yk in enumerate((Y0[i], Y1[i])):
                self.S.dma("pool", lambda e, Yk=Yk, tt=tt, k=k: e.indirect_dma_start(
                    out=Yk[:, :], out_offset=None, in_=YB[:, :],
                    in_offset=bass.IndirectOffsetOnAxis(ap=dest[:, tt, k:k + 1], axis=0)), _flat([YB, dest]), _flat([Yk]))
            x_ = xt[i]
            self.dma("sp", x_[:], self.XS[tt * 128:(tt + 1) * 128, :], reads=self.XS.rk(tt), writes=[x_])
            self.ts("dve", Y0[i][:], Y0[i][:], gates[:, tt, 0:1], ALU.mult, [Y0[i], gates], [Y0[i]])
            self.stt(Y0[i][:], Y1[i][:], gates[:, tt, 1:2], Y0[i][:], ALU.mult, ALU.add, [Y1[i], Y0[i], gates], [Y0[i]])
            self.tt("pool", Y0[i][:], Y0[i][:], g2[r][:], ALU.mult, [Y0[i], g2[r]], [Y0[i]])
            self.tt("dve", x_[:], x_[:], Y0[i][:], ALU.add, [x_, Y0[i]], [x_])
            self.dma("sp", self.XS[tt * 128:(tt + 1) * 128, :], x_[:], reads=[x_], writes=self.XS.rk(tt))
        self.pop()
        self.pop()

    def phase_final(self):
        self.push()
        fg = self.sb('fg', [128, D])
        self.dma("sp", fg[:], self.inp['final_norm_g'][0:1, :].partition_broadcast(128), writes=[fg])
        xt = [self.sb(f'xo{i}', [128, D]) for i in range(2)]
        xn = [self.sb(f'xq{i}', [128, D]) for i in range(2)]
        junk = self.sb('junk', [128, D])
        ss = [self.sb(f'sso{i}', [128, 4]) for i in range(2)]
        evs = []
        for tt in range(2, NT):
            i = tt % 2
            self.dma("sp", xt[i][:], self.XS[tt * 128:(tt + 1) * 128, :], reads=self.XS.rk(tt), writes=[xt[i]])
            self.norm_tile(xt[i], fg, None, xn[i], junk, ss[i])
            evs.append(self.dma("sp", self.out[(tt - 2) * 128:(tt - 1) * 128, :], xn[i][:], reads=[xn[i]]))
        for ev in evs:
            self.S.wait_event("sp", ev)
        self.pop()

    return dict(phase_wout=phase_wout, phase_moe=phase_moe, phase_final=phase_final)


for _k, _v in _tail_methods().items():
    setattr(Kern, _k, _v)
```
